# Optimizing a Trainium2 kernel written in Bass

```python
import jax, jax.numpy as jnp
from jax import lax
import numpy as np

D_MODEL = 1024
BATCH = 8
SEQ = 8192
DEPTH = 2

GRID_W = 64
CTX_LEN = 256
D_MIX = D_MODEL
D_CONV = D_MIX // 2
CONV_W = 3
D_MLSTM = D_MIX - D_CONV
N_HEADS = 4
HEAD_DIM = D_MLSTM // N_HEADS
CHUNK = 128
N_EXPERTS = 16
CAPACITY_FACTOR = 2
D_EXPERT = 1408
N_MOD = 6
EPS = 1e-6
D_PROJ = 3 * D_CONV + 4 * D_MLSTM + 4 * N_HEADS

kernel_name = "hybrid_conv_mlstm_ecmoe_dit"


def rmsnorm(x, g):
    xf = x.astype(jnp.float32)
    y = xf * lax.rsqrt(jnp.mean(xf * xf, axis=-1, keepdims=True) + EPS)
    return (y * g.astype(jnp.float32)).astype(x.dtype)


def split_mod(m):
    m = m.reshape(m.shape[0], N_MOD, D_MODEL)
    return [m[:, j, None, :] for j in range(N_MOD)]


def modulate(h, shift, scale):
    return h * (1 + scale) + shift


def split_proj(u):
    pts = [D_CONV, 2 * D_CONV, 3 * D_CONV,
           3 * D_CONV + D_MLSTM, 3 * D_CONV + 2 * D_MLSTM,
           3 * D_CONV + 3 * D_MLSTM, 3 * D_CONV + 4 * D_MLSTM]
    return jnp.split(u, pts, axis=-1)


def dwconv3(u, w, b):
    ch = u.shape[-1]
    y = lax.conv_general_dilated(u, w[:, None, :].astype(u.dtype), window_strides=(1,),
                                 padding=((CONV_W // 2, CONV_W // 2),),
                                 dimension_numbers=('NWC', 'WIO', 'NWC'), feature_group_count=ch)
    return y + b.astype(u.dtype)


def conv_rows(u, w, b):
    bn, n, ch = u.shape
    rows = n // GRID_W
    return dwconv3(u.reshape(bn * rows, GRID_W, ch), w, b).reshape(bn, n, ch)


def mlstm_inputs(q, k, v, gates, gate_b):
    bn, n, _ = q.shape
    def heads(t):
        return t.reshape(bn, n, N_HEADS, HEAD_DIM).transpose(0, 2, 1, 3).astype(jnp.float32)
    g = (gates.astype(jnp.float32) + gate_b.reshape(-1).astype(jnp.float32))
    g = g.reshape(bn, n, 4, N_HEADS).transpose(2, 0, 3, 1)
    return heads(q) * HEAD_DIM ** -0.5, heads(k), heads(v), g


def mlstm_scan(q, k, v, i_pre, log_f, state):
    bn, nh, n, dh = q.shape
    nc = n // CHUNK
    def chunks(t):
        return jnp.moveaxis(t.reshape(bn, nh, nc, CHUNK, *t.shape[3:]), 2, 0)
    incl = jnp.tril(jnp.ones((CHUNK, CHUNK), dtype=bool))

    def body(carry, xs):
        C, nv, m = carry
        qb, kb, vb, ib, fb = xs
        b = jnp.cumsum(fb, axis=-1)
        a = b + m[..., None]
        dmat = jnp.where(incl, b[..., :, None] - b[..., None, :] + ib[..., None, :], -jnp.inf)
        m_t = jnp.maximum(a, jnp.max(dmat, axis=-1))
        w_intra = jnp.exp(dmat - m_t[..., None])
        w_inter = jnp.exp(a - m_t)
        s = jnp.einsum('bhtk,bhsk->bhts', qb, kb) * w_intra
        num = w_inter[..., None] * jnp.einsum('bhtk,bhkv->bhtv', qb, C) + jnp.einsum('bhts,bhsv->bhtv', s, vb)
        den = w_inter * jnp.einsum('bhtk,bhk->bht', qb, nv) + jnp.sum(s, axis=-1)
        h = num / jnp.maximum(jnp.abs(den), jnp.exp(-m_t))[..., None]
        m_new = m_t[..., -1]
        w_state = jnp.exp(b[..., -1] + m - m_new)
        w_tok = jnp.exp(b[..., -1:] - b + ib - m_new[..., None])
        C_new = w_state[..., None, None] * C + jnp.einsum('bhs,bhsk,bhsv->bhkv', w_tok, kb, vb)
        n_new = w_state[..., None] * nv + jnp.einsum('bhs,bhsk->bhk', w_tok, kb)
        return (C_new, n_new, m_new), h

    state, h = lax.scan(body, state, tuple(chunks(t) for t in (q, k, v, i_pre, log_f)))
    return jnp.moveaxis(h, 0, 2).reshape(bn, nh, n, dh), state


def mlstm_bidir(pc, pl, gate_b):
    qc, kc, vc, gc = mlstm_inputs(pc[0], pc[1], pc[2], pc[3], gate_b)
    ql, kl, vl, gl = mlstm_inputs(pl[0], pl[1], pl[2], pl[3], gate_b)
    bn = ql.shape[0]
    init = (jnp.zeros((bn, N_HEADS, HEAD_DIM, HEAD_DIM), jnp.float32),
            jnp.zeros((bn, N_HEADS, HEAD_DIM), jnp.float32),
            jnp.zeros((bn, N_HEADS), jnp.float32))
    outs_c, outs_l = [], []
    for d in range(2):
        flip = (lambda t: jnp.flip(t, axis=2)) if d == 1 else (lambda t: t)
        i_c, f_c = gc[2 * d], jax.nn.log_sigmoid(gc[2 * d + 1])
        i_l, f_l = gl[2 * d], jax.nn.log_sigmoid(gl[2 * d + 1])
        h_c, st = mlstm_scan(flip(qc), flip(kc), flip(vc), flip(i_c), flip(f_c), init)
        h_l, _ = mlstm_scan(flip(ql), flip(kl), flip(vl), flip(i_l), flip(f_l), st)
        outs_c.append(flip(h_c))
        outs_l.append(flip(h_l))
    return outs_c[0] + outs_c[1], outs_l[0] + outs_l[1]


def mlstm_out(h, o, g_head, dtype):
    hn = h * lax.rsqrt(jnp.mean(h * h, axis=-1, keepdims=True) + EPS) * g_head.astype(jnp.float32)[None, :, None, :]
    bn, nh, n, dh = h.shape
    hn = hn.transpose(0, 2, 1, 3).reshape(bn, n, nh * dh)
    return (jax.nn.sigmoid(o.astype(jnp.float32)) * hn).astype(dtype)


def mixer(p_self, h_mlstm, conv_fn, conv_w, conv_b, g_head, w_out):
    b_gate, c_gate, x_in, o = p_self[0], p_self[1], p_self[2], p_self[6]
    y_conv = b_gate * conv_fn(c_gate * x_in, conv_w, conv_b)
    y_mlstm = mlstm_out(h_mlstm, o, g_head, b_gate.dtype)
    return jnp.concatenate([y_conv, y_mlstm], axis=-1) @ w_out


def expert_choice_moe(h, w_router, w_gate, w_up, w_down):
    n = h.shape[1]
    cap = CAPACITY_FACTOR * n // N_EXPERTS

    def one_set(hs):
        aff = jax.nn.softmax(jnp.einsum('nd,de->en', hs, w_router).astype(jnp.float32), axis=0)
        g, idx = lax.top_k(aff, cap)
        xe = hs[idx]
        a = jnp.einsum('ecd,edf->ecf', xe, w_gate)
        u = jnp.einsum('ecd,edf->ecf', xe, w_up)
        ye = jnp.einsum('ecf,efd->ecd', jax.nn.silu(a) * u, w_down) * g[..., None].astype(hs.dtype)
        return jax.ops.segment_sum(ye.reshape(-1, hs.shape[-1]), idx.reshape(-1), num_segments=n)

    return lax.map(one_set, h)


def setup_inputs(seed: int = 0) -> dict:
    key = jax.random.key(seed)
    ks = jax.random.split(key, 24)
    def nrm(k, shape, s):
        return jax.random.normal(k, shape, jnp.float32) * s
    d = D_MODEL
    gate_b = jnp.stack([nrm(ks[10], (DEPTH, N_HEADS), 0.1),
                        3.0 + 3.0 * jax.random.uniform(ks[11], (DEPTH, N_HEADS), jnp.float32),
                        nrm(ks[12], (DEPTH, N_HEADS), 0.1),
                        3.0 + 3.0 * jax.random.uniform(ks[13], (DEPTH, N_HEADS), jnp.float32)], axis=1)
    return {
        "x": nrm(ks[0], (BATCH, SEQ, d), 1.0),
        "c": nrm(ks[1], (BATCH, d), 1.0),
        "ctx": nrm(ks[2], (BATCH, CTX_LEN, d), 1.0),
        "c_ctx": nrm(ks[3], (d,), 1.0),
        "w_mod": nrm(ks[4], (DEPTH, d, N_MOD * d), 0.5 * d ** -0.5),
        "b_mod": nrm(ks[5], (DEPTH, N_MOD * d), 0.01),
        "g_norm1": 1.0 + nrm(ks[6], (DEPTH, d), 0.02),
        "g_norm2": 1.0 + nrm(ks[7], (DEPTH, d), 0.02),
        "w_in": nrm(ks[8], (DEPTH, d, D_PROJ), d ** -0.5),
        "w_out": nrm(ks[9], (DEPTH, D_MIX, d), D_MIX ** -0.5),
        "conv_w": nrm(ks[14], (DEPTH, CONV_W, D_CONV), CONV_W ** -0.5),
        "conv_b": nrm(ks[15], (DEPTH, D_CONV), 0.01),
        "gate_b": gate_b,
        "g_head": 1.0 + nrm(ks[16], (DEPTH, N_HEADS, HEAD_DIM), 0.02),
        "w_router": nrm(ks[17], (DEPTH, d, N_EXPERTS), d ** -0.5),
        "w_gate_e": nrm(ks[18], (DEPTH, N_EXPERTS, d, D_EXPERT), d ** -0.5),
        "w_up_e": nrm(ks[19], (DEPTH, N_EXPERTS, d, D_EXPERT), d ** -0.5),
        "w_down_e": nrm(ks[20], (DEPTH, N_EXPERTS, D_EXPERT, d), D_EXPERT ** -0.5),
        "g_final": 1.0 + nrm(ks[21], (d,), 0.02),
    }


def reference(x, c, ctx, c_ctx, w_mod, b_mod, g_norm1, g_norm2, w_in, w_out, conv_w, conv_b,
              gate_b, g_head, w_router, w_gate_e, w_up_e, w_down_e, g_final):
    xl, xc = x, ctx
    for layer in range(DEPTH):
        last = layer == DEPTH - 1
        ml = split_mod(jax.nn.silu(c) @ w_mod[layer] + b_mod[layer])
        mc = split_mod(jax.nn.silu(c_ctx)[None] @ w_mod[layer] + b_mod[layer])

        hl = modulate(rmsnorm(xl, g_norm1[layer]), ml[0], ml[1])
        hc = modulate(rmsnorm(xc, g_norm1[layer]), mc[0], mc[1])
        pl = split_proj(hl @ w_in[layer])
        pc = split_proj(hc @ w_in[layer])
        hm_c, hm_l = mlstm_bidir((pc[3], pc[4], pc[5], pc[7]), (pl[3], pl[4], pl[5], pl[7]), gate_b[layer])
        yl = mixer(pl, hm_l, conv_rows, conv_w[layer], conv_b[layer], g_head[layer], w_out[layer])
        xl = xl + ml[2] * yl
        if not last:
            yc = mixer(pc, hm_c, dwconv3, conv_w[layer], conv_b[layer], g_head[layer], w_out[layer])
            xc = xc + mc[2] * yc

        hl = modulate(rmsnorm(xl, g_norm2[layer]), ml[3], ml[4])
        xl = xl + ml[5] * expert_choice_moe(hl, w_router[layer], w_gate_e[layer], w_up_e[layer], w_down_e[layer])
        if not last:
            hc = modulate(rmsnorm(xc, g_norm2[layer]), mc[3], mc[4])
            xc = xc + mc[5] * expert_choice_moe(hc, w_router[layer], w_gate_e[layer], w_up_e[layer], w_down_e[layer])
    return rmsnorm(xl, g_final)
```

```python
import numpy as np
from contextlib import ExitStack
import concourse.bass as bass
import concourse.mybir as mybir
from concourse.bass_utils import run_bass_kernel_spmd

F32 = mybir.dt.float32
BF16 = mybir.dt.bfloat16
U32 = mybir.dt.uint32
AF = mybir.ActivationFunctionType
ALU = mybir.AluOpType
AX = mybir.AxisListType

D = 1024
KC = 8
NCTX = 256
NH = 4
DH = 128
NE = 16
DEXP = 1408
FCH = 11
DPROJ = 3600
EPS = 1e-6
CW = 1488


class Tl:
    __slots__ = ("w", "r")

    def __init__(self):
        self.w = None
        self.r = {}


class _Rec:
    def __init__(self):
        self.call = None

    def __getattr__(self, name):
        def f(*a, **k):
            self.call = (name, a, k)
            return self
        return f


def _record(fn):
    r = _Rec()
    fn(r)
    name, a, k = r.call
    return lambda e: getattr(e, name)(*a, **k)


class FW:
    NDS = 6

    def __init__(self, nc, es):
        self.nc = nc
        self.engs = ("pe", "act", "dve", "pool", "sp")
        self.esem = {k: es.enter_context(nc.semaphore("s_" + k)) for k in self.engs}
        self.ecnt = {k: 0 for k in self.engs}
        self.dsem = {}
        self.dcnt = {}
        self.drr = {}
        for q in ("sp", "act", "pool"):
            self.drr[q] = 0
            for i in range(self.NDS):
                self.dsem[(q, i)] = es.enter_context(nc.semaphore("d_%s%d" % (q, i)))
                self.dcnt[(q, i)] = 0
        self.seen = {k: {} for k in self.engs}
        self.prog = {k: [] for k in self.engs}
        self.ninst = 0

    def _sem(self, key):
        return self.esem[key[1]] if key[0] == "e" else self.dsem[(key[1], key[2])]

    def _needs(self, eng, reads, writes, extra=()):
        need = {}

        def add(ev):
            if ev is None:
                return
            k, v = ev
            if k == ("e", "pe") and eng == "pe":
                return
            if need.get(k, 0) < v:
                need[k] = v
        for t in reads:
            add(t.w)
        for t in writes:
            add(t.w)
            for k, v in t.r.items():
                add((k, v))
        for ev in extra:
            add(ev)
        out = []
        seen = self.seen[eng]
        for k, v in need.items():
            if seen.get(k, 0) < v:
                seen[k] = v
                out.append((self._sem(k), v))
        return out

    def _commit(self, ev, reads, writes):
        k, v = ev
        for t in writes:
            t.w = ev
            t.r = {}
        for t in reads:
            if t.r.get(k, 0) < v:
                t.r[k] = v

    def op(self, eng, fn, reads=(), writes=()):
        fn = _record(fn)
        waits = self._needs(eng, reads, writes)
        self.ecnt[eng] += 1
        ev = (("e", eng), self.ecnt[eng])
        sem = self.esem[eng]

        def emit(e, waits=waits, fn=fn, sem=sem):
            for s, v in waits:
                e.wait_ge(s, v)
            fn(e).then_inc(sem, 1)
        self.prog[eng].append(emit)
        self._commit(ev, reads, writes)
        self.ninst += 1 + len(waits)
        return ev

    def dma(self, q, fn, reads=(), writes=()):
        fn = _record(fn)
        i = self.drr[q]
        self.drr[q] = (i + 1) % self.NDS
        key = ("d", q, i)
        prev = (key, self.dcnt[(q, i)]) if self.dcnt[(q, i)] else None
        waits = self._needs(q, reads, writes, extra=(prev,) if prev else ())
        self.dcnt[(q, i)] += 16
        ev = (key, self.dcnt[(q, i)])
        sem = self.dsem[(q, i)]

        def emit(e, waits=waits, fn=fn, sem=sem):
            for s, v in waits:
                e.wait_ge(s, v)
            fn(e).then_inc(sem, 16)
        self.prog[q].append(emit)
        self._commit(ev, reads, writes)
        self.ninst += 1 + len(waits)
        return ev

    def barrier(self):
        allev = [(("e", k), self.ecnt[k]) for k in self.engs if self.ecnt[k]]
        allev += [(("d", q, i), c) for (q, i), c in self.dcnt.items() if c]
        for eng in self.engs:
            waits = []
            seen = self.seen[eng]
            for k, v in allev:
                if seen.get(k, 0) < v:
                    seen[k] = v
                    waits.append((self._sem(k), v))

            def emit(e, waits=waits):
                for s, v in waits:
                    e.wait_ge(s, v)
            self.prog[eng].append(emit)

    def flush(self):
        nc = self.nc
        prog = self.prog
        self.prog = {k: [] for k in self.engs}
        with nc.Block() as block:
            @block.tensor
            def _(e):
                for f in prog["pe"]:
                    f(e)

            @block.scalar
            def _(e):
                for f in prog["act"]:
                    f(e)

            @block.vector
            def _(e):
                for f in prog["dve"]:
                    f(e)

            @block.gpsimd
            def _(e):
                for f in prog["pool"]:
                    f(e)

            @block.sync
            def _(e):
                for f in prog["sp"]:
                    f(e)


class B:
    def __init__(self, t):
        self.t = t
        self.tl = Tl()
        self.sub = [Tl() for _ in range(16)]


def make_consts():
    c = np.zeros((128, CW), np.float32)
    c[:, 0:128] = np.eye(128)
    s = np.arange(128)[:, None]
    t = np.arange(128)[None, :]
    c[:, 128:256] = (s <= t)
    c[:, 256:384] = (s >= t)
    MZG, MSG, MLG, MSB, MLB, MC = (c[0:16, 384 + 8 * i:392 + 8 * i] for i in range(6))
    for h in range(4):
        i_f, f_f, i_b, f_b = h, 4 + h, 8 + h, 12 + h
        MZG[i_f, h] = 1
        MZG[i_b, 4 + h] = 1
        MSG[f_f, h] = -1
        MSG[f_b, 4 + h] = 1
        MLG[f_b, 4 + h] = -1
        MSB[f_f, h] = 1
        MSB[f_b, 4 + h] = -1
        MLB[f_b, 4 + h] = 1
        MC[f_b, 4 + h] = 1
    c[0:4, 432] = 1
    c[4:8, 433] = 1
    for r in range(8):
        c[r, 440 + r * 128:440 + (r + 1) * 128] = 1
    c[:, 1464] = 1
    c[0:16, 1472:1488] = 1
    return c


class _Stop(Exception):
    pass


def build(NLAT, DEPTH, dbg=False, stop=None):
    try:
        return _build(NLAT, DEPTH, dbg, stop)
    except _Stop as ex:
        return ex.args[0]


def _build(NLAT, DEPTH, dbg=False, stop=None):
    NT = NCTX + NLAT
    NCH = NT // 128
    CAPL = 2 * NLAT // NE
    CAPC = 2 * NCTX // NE
    nc = bass.Bass("TRN2", target_bir_lowering=False)

    def din(name, shape, dt=F32):
        return nc.dram_tensor(name, shape, dt, kind="ExternalInput").ap()

    def dscr(name, shape, dt=F32):
        return nc.dram_tensor(name, shape, dt, kind="Internal").ap()

    x_in = din("x", [NLAT, D])
    ctx_in = din("ctx", [NCTX, D])
    cT_in = din("cT", [128, 2, 8])
    w_mod = din("w_mod", [DEPTH, D, 6 * D])
    b_mod = din("b_mod", [DEPTH, 6 * D])
    g_norm1 = din("g_norm1", [DEPTH, D])
    g_norm2 = din("g_norm2", [DEPTH, D])
    w_in = din("w_in", [DEPTH, D, DPROJ])
    w_out = din("w_out", [DEPTH, D, D])
    conv_w = din("conv_w", [DEPTH, 128, 3, 4])
    conv_b = din("conv_b", [DEPTH, 128, 4])
    gate_b = din("gate_b", [DEPTH, 16, 1])
    g_head = din("g_head", [DEPTH, 512])
    w_router = din("w_router", [DEPTH, D, NE])
    w_gate_e = din("w_gate_e", [DEPTH, NE, D, DEXP])
    w_up_e = din("w_up_e", [DEPTH, NE, D, DEXP])
    w_down_e = din("w_down_e", [DEPTH, NE, DEXP, D])
    g_final = din("g_final", [D])
    consts_in = din("consts", [128, CW])
    out = nc.dram_tensor("out", [NLAT, D], F32, kind="ExternalOutput").ap()

    X = dscr("Xs", [NT, D])
    MODROW = dscr("MODROW", [2, 6 * D])
    QT = dscr("QT", [NH, 128, NT], BF16)
    KT = dscr("KT", [NH, 128, NT], BF16)
    KTOK = dscr("KTOK", [NT, 512], BF16)
    VTOK = dscr("VTOK", [NT, 512], BF16)
    OTOK = dscr("OTOK", [NT, 512], BF16)
    YT = dscr("YT", [4, 128, NT], BF16)
    HFB = dscr("HFB", [2, NT, 512])
    H2 = dscr("H2", [NT, D], BF16)
    TIS = dscr("TIS", [16, CAPL + CAPC], U32)
    AFD = dscr("AFD", [16, NLAT])
    TVD = dscr("TVD", [32, CAPL])
    TID = dscr("TID", [32, CAPL], U32)
    dbgout = {}
    if dbg:
        dbgout["dX"] = nc.dram_tensor("dX", [NT, D], F32, kind="ExternalOutput").ap()
        dbgout["dHFB"] = nc.dram_tensor("dHFB", [2, NT, 512], F32, kind="ExternalOutput").ap()
        dbgout["dAFF"] = nc.dram_tensor("dAFF", [16, NT], F32, kind="ExternalOutput").ap()
        dbgout["dX1"] = nc.dram_tensor("dX1", [NT, D], F32, kind="ExternalOutput").ap()
        dbgout["dTOKQ"] = nc.dram_tensor("dTOKQ", [128, NCH * 16], F32, kind="ExternalOutput").ap()
        for nm, src in (("QT", QT), ("KT", KT), ("KTOK", KTOK), ("VTOK", VTOK), ("OTOK", OTOK), ("YT", YT), ("H2", H2), ("MODROW", MODROW)):
            dbgout["d" + nm] = (nc.dram_tensor("d" + nm, list(src.shape), src.dtype, kind="ExternalOutput").ap(), src)
        dbgout["dGP"] = nc.dram_tensor("dGP", [16, NT], F32, kind="ExternalOutput").ap()

    blocks = [(0, NCTX, True)] + [(NCTX + i * 512, 512, False) for i in range(NLAT // 512)]
    order_f = list(range(NCH))
    order_b = [1, 0] + list(range(NCH - 1, 1, -1))

    with ExitStack() as es:
        fw = FW(nc, es)
        op, dma = fw.op, fw.dma

        uid = [0]

        def sb(scope, name, shape, dt=F32):
            uid[0] += 1
            return B(scope.enter_context(nc.sbuf_tensor("%s_%d" % (name, uid[0]), shape, dt)))

        def ps(scope, name, shape, dt=F32):
            full = [128, 512] if dt == F32 else [128, 1024]
            uid[0] += 1
            t = scope.enter_context(nc.psum_tensor("%s_%d" % (name, uid[0]), full, dt))
            n = 1
            for v in shape[1:]:
                n *= v
            v = t[0:shape[0], 0:n]
            if len(shape) == 3:
                v = v.rearrange("p (a b) -> p a b", a=shape[1])
            return B(v)

        tX = Tl()
        tD = Tl()

        cst = sb(es, "cst", [128, CW])
        identb = sb(es, "identb", [128, 128], BF16)
        dma("sp", lambda e: e.dma_start(out=cst.t[:], in_=consts_in[:, :]), writes=[cst.tl])
        op("dve", lambda e: e.tensor_copy(out=identb.t[:], in_=cst.t[:, 0:128]), reads=[cst.tl], writes=[identb.tl])
        ident = cst.t[:, 0:128]

        def xrows(l_, r0_):
            if l_ > 0:
                return X[r0_:r0_ + 128, :]
            if r0_ < NCTX:
                return ctx_in[r0_:r0_ + 128, :]
            return x_in[r0_ - NCTX:r0_ - NCTX + 128, :]
        if DEPTH == 1:
            dma("sp", lambda e: e.dma_start(out=X[0:NCTX, :], in_=ctx_in[:, :]), writes=[tX])
        fw.barrier()
        fw.flush()

        for l in range(DEPTH):
            last = (l == DEPTH - 1)
            with ExitStack() as ph:
                scs = sb(ph, "scs", [128, 2, 8])
                wm = [sb(ph, "wm%d" % i, [128, 8, 512]) for i in range(2)]
                rows = [sb(ph, "mrow%d" % s, [1, 6 * D]) for s in range(2)]
                brow = sb(ph, "brow", [1, 6 * D])
                grow = sb(ph, "grow", [1, 2 * D])
                pm = [ps(ph, "pm%d" % s, [1, 512]) for s in range(2)]
                dma("sp", lambda e: e.dma_start(out=scs.t[:], in_=cT_in[:, :, :]), writes=[scs.tl])
                dma("sp", lambda e: e.dma_start(out=brow.t[:], in_=b_mod[l:l + 1, :]), writes=[brow.tl])
                dma("sp", lambda e: e.dma_start(out=grow.t[:, 0:D], in_=g_norm1[l:l + 1, :]), writes=[grow.tl])
                dma("sp", lambda e: e.dma_start(out=grow.t[:, D:2 * D], in_=g_norm2[l:l + 1, :]), writes=[grow.tl])
                op("act", lambda e: e.activation(out=scs.t[:], in_=scs.t[:], func=AF.Silu), reads=[scs.tl], writes=[scs.tl])
                wmv = w_mod[l].rearrange("(k p) n -> p k n", p=128)
                for blk in range(12):
                    w = wm[blk % 2]
                    dma("sp", lambda e, w=w, blk=blk: e.dma_start(out=w.t[:], in_=wmv[:, :, blk * 512:(blk + 1) * 512]), writes=[w.tl])
                    for s in range(2):
                        for kc in range(KC):
                            op("pe", lambda e, s=s, kc=kc, w=w: e.matmul(pm[s].t[:], lhsT=scs.t[:, s, kc:kc + 1], rhs=w.t[:, kc, :],
                                                                        start=(kc == 0), stop=(kc == KC - 1)),
                               reads=[scs.tl, w.tl], writes=[pm[s].tl])
                        op("dve", lambda e, s=s, blk=blk: e.tensor_tensor(out=rows[s].t[:, blk * 512:(blk + 1) * 512], in0=pm[s].t[:],
                                                                         in1=brow.t[:, blk * 512:(blk + 1) * 512], op=ALU.add),
                           reads=[pm[s].tl, brow.tl], writes=[rows[s].tl])
                for s in range(2):
                    for (o, g) in ((D, 0), (4 * D, D)):
                        op("dve", lambda e, s=s, o=o, g=g: e.scalar_tensor_tensor(out=rows[s].t[:, o:o + D], in0=rows[s].t[:, o:o + D], scalar=1.0,
                                                                                 in1=grow.t[:, g:g + D], op0=ALU.add, op1=ALU.mult),
                           reads=[rows[s].tl, grow.tl], writes=[rows[s].tl])
                    dma("sp", lambda e, s=s: e.dma_start(out=MODROW[s:s + 1, :], in_=rows[s].t[:]), reads=[rows[s].tl], writes=[tD])
                fw.barrier()
                fw.flush()

            if stop == "P0":
                return nc
            def load_bc(buf, s, j):
                dma("sp", lambda e: e.dma_start(out=buf.t[:], in_=MODROW[s, j * D:(j + 1) * D].partition_broadcast(128)), writes=[buf.tl])

            with ExitStack() as lay:
                GP = sb(lay, "GP", [16, NT])
                TOKQ = sb(lay, "TOKQ", [128, NCH, 16])
                ABt = sb(lay, "ABt", [128, 8, NCH])
                with ExitStack() as ph:
                    win = sb(ph, "win", [128, KC, DPROJ], BF16)
                    for kc in range(KC):
                        dma("pool", lambda e, kc=kc: e.dma_start(out=win.t[:, kc, :], in_=w_in[l, kc * 128:(kc + 1) * 128, :]), writes=[win.sub[kc]])
                    A1 = [sb(ph, "A1_%d" % s, [128, D]) for s in range(2)]
                    S1 = [sb(ph, "S1_%d" % s, [128, D]) for s in range(2)]
                    for s in range(2):
                        load_bc(S1[s], s, 0)
                        load_bc(A1[s], s, 1)
                    cw = sb(ph, "cw", [128, 3, 4])
                    cb = sb(ph, "cb", [128, 4])
                    gb = sb(ph, "gb", [16, 1])
                    dma("sp", lambda e: e.dma_start(out=cw.t[:], in_=conv_w[l]), writes=[cw.tl])
                    dma("sp", lambda e: e.dma_start(out=cb.t[:], in_=conv_b[l]), writes=[cb.tl])
                    dma("sp", lambda e: e.dma_start(out=gb.t[:], in_=gate_b[l]), writes=[gb.tl])
                    xt = [sb(ph, "xt%d" % i, [128, D]) for i in range(2)]
                    junk = sb(ph, "junk", [128, D], BF16)
                    tmp = sb(ph, "tmp", [128, D])
                    hn = [sb(ph, "hn%d" % i, [128, D]) for i in range(2)]
                    hT = [sb(ph, "hT%d" % i, [128, KC, 512], BF16) for i in range(2)]
                    ss = [sb(ph, "ss%d" % i, [128, 4]) for i in range(2)]
                    xin_sb = sb(ph, "xin_sb", [128, 512])
                    cx = sb(ph, "cx", [128, 512])
                    acc = sb(ph, "acc", [128, 512])
                    stf = [sb(ph, "stf%d" % i, [128, 512], BF16) for i in range(3)]
                    stt = [sb(ph, "stt%d" % i, [128, 512], BF16) for i in range(3)]
                    pT = [ps(ph, "pT%d" % i, [128, 512]) for i in range(2)]
                    pf = [ps(ph, "pf%d" % i, [128, 512]) for i in range(3)]
                    ptm = [ps(ph, "ptm%d" % i, [128, 512]) for i in range(2)]
                    ti = 0
                    pa_rows = [t0_ + tt_ * 128 for (t0_, ntok_, _c) in blocks for tt_ in range(ntok_ // 128)]

                    def pa_load(i):
                        if i < len(pa_rows):
                            xb_ = xt[i % 2]
                            rr = pa_rows[i]
                            dma("sp", lambda e: e.dma_start(out=xb_.t[:], in_=xrows(l, rr)), reads=[tX], writes=[xb_.tl])
                    pa_load(0)
                    nstf = 0
                    nstt = 0
                    npf = 0
                    for bi, (t0, ntok, isctx) in enumerate(blocks):
                        s = 1 if isctx else 0
                        hTb = hT[bi % 2]
                        for tt in range(ntok // 128):
                            r0 = t0 + tt * 128
                            xb = xt[ti % 2]
                            hb = hn[ti % 2]
                            sq = ss[ti % 2]
                            pa_load(ti + 1)
                            op("act", lambda e, xb=xb, sq=sq: e.activation(out=junk.t[:], in_=xb.t[:], func=AF.Square, accum_out=sq.t[:, 0:1]),
                               reads=[xb.tl], writes=[junk.tl, sq.tl])
                            op("act", lambda e, sq=sq: e.activation(out=sq.t[:, 1:2], in_=sq.t[:, 0:1], func=AF.Sqrt, scale=1.0 / D, bias=EPS),
                               reads=[sq.tl], writes=[sq.tl])
                            op("dve", lambda e, sq=sq: e.reciprocal(out=sq.t[:, 2:3], in_=sq.t[:, 1:2]), reads=[sq.tl], writes=[sq.tl])
                            op("dve", lambda e, xb=xb, sq=sq, s=s: e.scalar_tensor_tensor(out=tmp.t[:], in0=xb.t[:], scalar=sq.t[:, 2:3], in1=A1[s].t[:],
                                                                                         op0=ALU.mult, op1=ALU.mult),
                               reads=[xb.tl, sq.tl, A1[s].tl], writes=[tmp.tl])
                            op("dve", lambda e, hb=hb, s=s: e.tensor_tensor(out=hb.t[:], in0=tmp.t[:], in1=S1[s].t[:], op=ALU.add),
                               reads=[tmp.tl, S1[s].tl], writes=[hb.tl])
                            for kc in range(KC):
                                p = pT[kc // 4]
                                op("pe", lambda e, p=p, kc=kc, hb=hb: e.transpose(out=p.t[:, (kc % 4) * 128:(kc % 4 + 1) * 128],
                                                                                 in_=hb.t[:, kc * 128:(kc + 1) * 128], identity=ident),
                                   reads=[hb.tl, cst.tl], writes=[p.tl])
                            for half, eng in ((0, "act"), (1, "dve")):
                                src = pT[half].t[:].rearrange("p (k t) -> p k t", k=4)
                                dst = hTb.t[:, half * 4:(half + 1) * 4, tt * 128:(tt + 1) * 128]
                                if eng == "act":
                                    op("act", lambda e, src=src, dst=dst: e.copy(out=dst, in_=src), reads=[pT[half].tl], writes=[hTb.tl])
                                else:
                                    op("dve", lambda e, src=src, dst=dst: e.tensor_copy(out=dst, in_=src), reads=[pT[half].tl], writes=[hTb.tl])
                            for gi, (col, dst, fn) in enumerate(((2048, KTOK, None), (2560, VTOK, None), (3072, OTOK, AF.Sigmoid))):
                                p = ptm[(ti * 3 + gi) % 2]
                                for kc in range(KC):
                                    op("pe", lambda e, p=p, kc=kc, col=col, tt=tt: e.matmul(p.t[:], lhsT=hTb.t[:, kc, tt * 128:(tt + 1) * 128],
                                                                                           rhs=win.t[:, kc, col:col + 512],
                                                                                           start=(kc == 0), stop=(kc == KC - 1)),
                                       reads=[hTb.tl, win.sub[kc]], writes=[p.tl])
                                st = stt[nstt % 3]
                                nstt += 1
                                if fn is None:
                                    op("act", lambda e, st=st, p=p: e.copy(out=st.t[:], in_=p.t[:]), reads=[p.tl], writes=[st.tl])
                                else:
                                    op("act", lambda e, st=st, p=p, fn=fn: e.activation(out=st.t[:], in_=p.t[:], func=fn), reads=[p.tl], writes=[st.tl])
                                dma("sp", lambda e, st=st, dst=dst, r0=r0: e.dma_start(out=dst[r0:r0 + 128, :], in_=st.t[:]), reads=[st.tl], writes=[tD])
                            ti += 1
                        W = ntok if isctx else 64

                        def fmm(p, col, m=128):
                            for kc in range(KC):
                                op("pe", lambda e, kc=kc: e.matmul(p.t[0:m, 0:ntok], lhsT=win.t[:, kc, col:col + m], rhs=hTb.t[:, kc, 0:ntok],
                                                                  start=(kc == 0), stop=(kc == KC - 1)),
                                   reads=[hTb.tl, win.sub[kc]], writes=[p.tl])
                        for j in range(4):
                            fmm(pf[0], j * 128)
                            fmm(pf[1], 512 + j * 128)
                            fmm(pf[2], 1024 + j * 128)
                            op("act", lambda e: e.copy(out=xin_sb.t[:, 0:ntok], in_=pf[2].t[:, 0:ntok]), reads=[pf[2].tl], writes=[xin_sb.tl])
                            op("dve", lambda e: e.tensor_tensor(out=cx.t[:, 0:ntok], in0=pf[1].t[:, 0:ntok], in1=xin_sb.t[:, 0:ntok], op=ALU.mult),
                               reads=[pf[1].tl, xin_sb.tl], writes=[cx.tl])
                            op("dve", lambda e, j=j: e.tensor_scalar(out=acc.t[:, 0:ntok], in0=cx.t[:, 0:ntok], scalar1=cw.t[:, 1, j:j + 1],
                                                                    scalar2=cb.t[:, j:j + 1], op0=ALU.mult, op1=ALU.add),
                               reads=[cx.tl, cw.tl, cb.tl], writes=[acc.tl])
                            accv = acc.t[:, 0:ntok].rearrange("p (r w) -> p r w", w=W)
                            cxv = cx.t[:, 0:ntok].rearrange("p (r w) -> p r w", w=W)
                            op("dve", lambda e, j=j, accv=accv, cxv=cxv: e.scalar_tensor_tensor(
                                out=accv[:, :, 1:W], in0=cxv[:, :, 0:W - 1], scalar=cw.t[:, 0, j:j + 1], in1=accv[:, :, 1:W],
                                op0=ALU.mult, op1=ALU.add), reads=[cx.tl, cw.tl, acc.tl], writes=[acc.tl])
                            op("dve", lambda e, j=j, accv=accv, cxv=cxv: e.scalar_tensor_tensor(
                                out=accv[:, :, 0:W - 1], in0=cxv[:, :, 1:W], scalar=cw.t[:, 2, j:j + 1], in1=accv[:, :, 0:W - 1],
                                op0=ALU.mult, op1=ALU.add), reads=[cx.tl, cw.tl, acc.tl], writes=[acc.tl])
                            st = stf[nstf % 3]
                            nstf += 1
                            op("dve", lambda e, st=st: e.tensor_tensor(out=st.t[:, 0:ntok], in0=pf[0].t[:, 0:ntok], in1=acc.t[:, 0:ntok], op=ALU.mult),
                               reads=[pf[0].tl, acc.tl], writes=[st.tl])
                            dma("sp", lambda e, st=st, j=j: e.dma_start(out=YT[j, :, t0:t0 + ntok], in_=st.t[:, 0:ntok]), reads=[st.tl], writes=[tD])
                        for which, (col0, dst, scl) in enumerate(((1536, QT, DH ** -0.5), (2048, KT, 1.0))):
                            for h in range(NH):
                                p = pf[npf % 3]
                                npf += 1
                                fmm(p, col0 + h * 128)
                                st = stf[nstf % 3]
                                nstf += 1
                                op("act", lambda e, st=st, p=p, scl=scl: e.activation(out=st.t[:, 0:ntok], in_=p.t[:, 0:ntok], func=AF.Copy, scale=scl),
                                   reads=[p.tl], writes=[st.tl])
                                dma("sp", lambda e, st=st, dst=dst, h=h: e.dma_start(out=dst[h, :, t0:t0 + ntok], in_=st.t[:, 0:ntok]),
                                    reads=[st.tl], writes=[tD])
                        p = pf[npf % 3]
                        npf += 1
                        fmm(p, 3584, m=16)
                        op("act", lambda e, p=p: e.activation(out=GP.t[:, t0:t0 + ntok], in_=p.t[0:16, 0:ntok], func=AF.Identity, bias=gb.t[:, 0:1]),
                           reads=[p.tl, gb.tl], writes=[GP.tl])
                    fw.barrier()
                    fw.flush()

                if stop == "PA":
                    return nc
                if dbg and l == 0:
                    dma("sp", lambda e: e.dma_start(out=dbgout["dGP"][:, :], in_=GP.t[:]), reads=[GP.tl], writes=[tD])
                with ExitStack() as ph:
                    L = sb(ph, "L", [16, NT])
                    Sc = sb(ph, "Sc", [16, NT])
                    Gm = sb(ph, "Gm", [8, NT])
                    Bm = sb(ph, "Bm", [8, NT])
                    ones = sb(ph, "ones", [16, 512])
                    sm = sb(ph, "sm", [16, 16])
                    CM = sb(ph, "CM", [8, NCH])
                    Rd = [sb(ph, "Rd%d" % i, [8, NCH]) for i in range(2)]
                    Dd = [sb(ph, "Dd%d" % i, [8, NCH]) for i in range(2)]
                    R = sb(ph, "R", [8, NCH])
                    Dl = sb(ph, "Dl", [8, NCH])
                    zc = sb(ph, "zc", [8, 1])
                    pg = [ps(ph, "pg%d" % i, [8, 512]) for i in range(2)]
                    pb = [ps(ph, "pb%d" % i, [8, 512]) for i in range(2)]
                    pc = ps(ph, "pc", [8, 2])
                    pa = [ps(ph, "pa%d" % i, [128, 4 * NCH]) for i in range(2)]
                    pq = ps(ph, "pq", [128, 512])
                    Z = GP
                    op("act", lambda e: e.activation(out=L.t[:], in_=Z.t[:], func=AF.Exp, scale=-1.0), reads=[Z.tl], writes=[L.tl])
                    op("act", lambda e: e.activation(out=L.t[:], in_=L.t[:], func=AF.Ln, bias=1.0), reads=[L.tl], writes=[L.tl])
                    op("act", lambda e: e.mul(out=L.t[:], in_=L.t[:], mul=-1.0) if False else e.activation(out=L.t[:], in_=L.t[:], func=AF.Copy, scale=-1.0),
                       reads=[L.tl], writes=[L.tl])
                    op("dve", lambda e: e.memset(ones.t[:], 1.0), writes=[ones.tl])
                    op("dve", lambda e: e.memset(zc.t[:], 0.0), writes=[zc.tl])
                    for i, c0 in enumerate(range(0, NT, 512)):
                        n = min(512, NT - c0)
                        init = 0.0 if i == 0 else Sc.t[:, c0 - 1:c0]
                        op("dve", lambda e, c0=c0, n=n, init=init: e.tensor_tensor_scan(out=Sc.t[:, c0:c0 + n], data0=ones.t[:, 0:n], data1=L.t[:, c0:c0 + n],
                                                                                       initial=init, op0=ALU.mult, op1=ALU.add),
                           reads=[ones.tl, L.tl, Sc.tl], writes=[Sc.tl])
                    op("dve", lambda e: e.tensor_copy(out=sm.t[:, 0:1], in_=Sc.t[:, NCTX - 1:NCTX]), reads=[Sc.tl], writes=[sm.tl])
                    op("dve", lambda e: e.tensor_copy(out=sm.t[:, 1:2], in_=Sc.t[:, NT - 1:NT]), reads=[Sc.tl], writes=[sm.tl])
                    op("pe", lambda e: e.matmul(pc.t[:], lhsT=cst.t[0:16, 424:432], rhs=sm.t[:, 0:2], start=True, stop=True),
                       reads=[sm.tl, cst.tl], writes=[pc.tl])
                    op("dve", lambda e: e.tensor_copy(out=sm.t[0:8, 4:5], in_=pc.t[:, 0:1]), reads=[pc.tl], writes=[sm.tl])
                    op("dve", lambda e: e.tensor_tensor(out=sm.t[0:8, 5:6], in0=pc.t[:, 1:2], in1=sm.t[0:8, 4:5], op=ALU.add), reads=[pc.tl, sm.tl], writes=[sm.tl])
                    for bi, (t0, ntok, isctx) in enumerate(blocks):
                        cs = sm.t[0:8, 4:5] if isctx else sm.t[0:8, 5:6]
                        p1, p2 = pg[bi % 2], pb[bi % 2]
                        for i, (col, src) in enumerate(((384, Z), (392, Sc), (400, L))):
                            op("pe", lambda e, p1=p1, col=col, src=src, i=i: e.matmul(p1.t[:, 0:ntok], lhsT=cst.t[0:16, col:col + 8], rhs=src.t[:, t0:t0 + ntok],
                                                                                     start=(i == 0), stop=(i == 2)),
                               reads=[cst.tl, src.tl], writes=[p1.tl])
                        op("dve", lambda e, p1=p1, cs=cs: e.tensor_scalar(out=Gm.t[:, t0:t0 + ntok], in0=p1.t[:, 0:ntok], scalar1=cs, scalar2=None, op0=ALU.subtract),
                           reads=[p1.tl, sm.tl], writes=[Gm.tl])
                        for i, (col, src) in enumerate(((408, Sc), (416, L))):
                            op("pe", lambda e, p2=p2, col=col, src=src, i=i: e.matmul(p2.t[:, 0:ntok], lhsT=cst.t[0:16, col:col + 8], rhs=src.t[:, t0:t0 + ntok],
                                                                                     start=(i == 0), stop=(i == 1)),
                               reads=[cst.tl, src.tl], writes=[p2.tl])
                        op("dve", lambda e, p2=p2, cs=cs: e.tensor_scalar(out=Bm.t[:, t0:t0 + ntok], in0=p2.t[:, 0:ntok], scalar1=cs, scalar2=None, op0=ALU.add),
                           reads=[p2.tl, sm.tl], writes=[Bm.tl])
                    op("dve", lambda e: e.tensor_reduce(out=CM.t[:], in_=Gm.t[:].rearrange("p (c t) -> p c t", t=128), axis=AX.X, op=ALU.max),
                       reads=[Gm.tl], writes=[CM.tl])
                    for d, order in enumerate((order_f, order_b)):
                        prev = zc.t[:, 0:1]
                        for c in order:
                            op("dve", lambda e, d=d, c=c, prev=prev: e.tensor_tensor(out=Rd[d].t[:, c:c + 1], in0=prev, in1=CM.t[:, c:c + 1], op=ALU.max),
                               reads=[zc.tl, CM.tl, Rd[d].tl], writes=[Rd[d].tl])
                            op("dve", lambda e, d=d, c=c, prev=prev: e.tensor_tensor(out=Dd[d].t[:, c:c + 1], in0=prev, in1=Rd[d].t[:, c:c + 1], op=ALU.subtract),
                               reads=[zc.tl, Rd[d].tl], writes=[Dd[d].tl])
                            prev = Rd[d].t[:, c:c + 1]
                    for dstb, srcs in ((R, Rd), (Dl, Dd)):
                        op("dve", lambda e, dstb=dstb, srcs=srcs: e.tensor_scalar(out=dstb.t[:], in0=srcs[0].t[:], scalar1=cst.t[0:8, 432:433], scalar2=None, op0=ALU.mult),
                           reads=[srcs[0].tl, cst.tl], writes=[dstb.tl])
                        op("dve", lambda e, dstb=dstb, srcs=srcs: e.scalar_tensor_tensor(out=dstb.t[:], in0=srcs[1].t[:], scalar=cst.t[0:8, 433:434], in1=dstb.t[:],
                                                                                        op0=ALU.mult, op1=ALU.add),
                           reads=[srcs[1].tl, cst.tl, dstb.tl], writes=[dstb.tl])
                    op("act", lambda e: e.activation(out=Dl.t[:], in_=Dl.t[:], func=AF.Exp), reads=[Dl.tl], writes=[Dl.tl])
                    op("dve", lambda e: e.tensor_scalar(out=R.t[:], in0=R.t[:], scalar1=-1.0, scalar2=None, op0=ALU.mult), reads=[R.tl], writes=[R.tl])
                    for r in range(8):
                        p = pa[r // 4]
                        op("pe", lambda e, p=p, r=r: e.matmul(p.t[:, (r % 4) * NCH:(r % 4 + 1) * NCH], lhsT=cst.t[0:8, 440 + r * 128:440 + (r + 1) * 128],
                                                             rhs=Dl.t[:, :], start=True, stop=True),
                           reads=[cst.tl, Dl.tl], writes=[p.tl])
                    for i in range(2):
                        op("dve", lambda e, i=i: e.tensor_copy(out=ABt.t[:, i * 4:(i + 1) * 4, :], in_=pa[i].t[:].rearrange("p (r c) -> p r c", r=4)),
                           reads=[pa[i].tl], writes=[ABt.tl])
                    for c in range(NCH):
                        op("act", lambda e, c=c: e.activation(out=GP.t[0:8, c * 128:(c + 1) * 128], in_=Gm.t[:, c * 128:(c + 1) * 128], func=AF.Exp,
                                                             bias=R.t[:, c:c + 1]), reads=[Gm.tl, R.tl], writes=[GP.tl])
                        op("act", lambda e, c=c: e.activation(out=L.t[0:8, c * 128:(c + 1) * 128], in_=Bm.t[:, c * 128:(c + 1) * 128], func=AF.Exp,
                                                             bias=R.t[:, c:c + 1], scale=-1.0), reads=[Bm.tl, R.tl], writes=[L.tl])
                    for c in range(NCH):
                        cc = c % 32
                        op("pe", lambda e, c=c, cc=cc: e.transpose(out=pq.t[:, cc * 16:cc * 16 + 8], in_=GP.t[0:8, c * 128:(c + 1) * 128], identity=cst.t[0:8, 0:8]),
                           reads=[GP.tl, cst.tl], writes=[pq.tl])
                        op("pe", lambda e, c=c, cc=cc: e.transpose(out=pq.t[:, cc * 16 + 8:cc * 16 + 16], in_=L.t[0:8, c * 128:(c + 1) * 128], identity=cst.t[0:8, 0:8]),
                           reads=[L.tl, cst.tl], writes=[pq.tl])
                        if cc == 31 or c == NCH - 1:
                            c0 = c - cc
                            op("dve", lambda e, c0=c0, cc=cc: e.tensor_copy(out=TOKQ.t[:, c0:c0 + cc + 1, :],
                                                                           in_=pq.t[:, 0:(cc + 1) * 16].rearrange("p (c k) -> p c k", k=16)),
                               reads=[pq.tl], writes=[TOKQ.tl])
                    if dbg and l == 0:
                        dma("sp", lambda e: e.dma_start(out=dbgout["dTOKQ"][:, :], in_=TOKQ.t[:].rearrange("p c k -> p (c k)")), reads=[TOKQ.tl], writes=[tD])
                    fw.barrier()
                    fw.flush()

                if stop == "PG":
                    return nc
                with ExitStack() as ph:
                    qTb = [sb(ph, "qTb%d" % i, [128, NH, 128], BF16) for i in range(2)]
                    kTb = [sb(ph, "kTb%d" % i, [128, NH, 128], BF16) for i in range(2)]
                    ktk = [sb(ph, "ktk%d" % i, [128, 512], BF16) for i in range(2)]
                    vau = [sb(ph, "vau%d" % i, [128, NH, 129], BF16) for i in range(2)]
                    Chd = [[sb(ph, "Ch%d_%d" % (d_, h), [128, 129]) for h in range(NH)] for d_ in range(2)]
                    Csb = [sb(ph, "Csb%d" % i, [128, 129], BF16) for i in range(4)]
                    Sm = [sb(ph, "Sm%d" % i, [128, 128], BF16) for i in range(4)]
                    vt = [sb(ph, "vt%d" % i, [128, 129], BF16) for i in range(4)]
                    dn = [sb(ph, "dn%d" % i, [128, 2]) for i in range(4)]
                    hbuf = [sb(ph, "hbuf%d" % i, [128, 512]) for i in range(2)]
                    ps_s = [ps(ph, "ps_s%d" % i, [128, 128]) for i in range(2)]
                    ps_o = [ps(ph, "ps_o%d" % i, [128, 129]) for i in range(4)]
                    ps_c = [ps(ph, "ps_c%d" % i, [128, 129]) for i in range(2)]
                    for v in vau:
                        op("dve", lambda e, v=v: e.memset(v.t[:], 1.0), writes=[v.tl])
                    it = 0
                    n = 0
                    for d in range(2):
                        for h in range(NH):
                            op("dve", lambda e, h=h, d=d: e.memset(Chd[d][h].t[:], 0.0), writes=[Chd[d][h].tl])
                    ps_steps = [(d_, (order_f, order_b)[d_][st_]) for st_ in range(NCH) for d_ in range(2)]

                    def ps_load(i):
                        if i < len(ps_steps):
                            tt0 = ps_steps[i][1] * 128
                            q2, k2, kt2, v2 = qTb[i % 2], kTb[i % 2], ktk[i % 2], vau[i % 2]
                            dma("sp", lambda e: e.dma_start(out=q2.t[:], in_=QT[:, :, tt0:tt0 + 128].rearrange("h p t -> p h t")), writes=[q2.tl])
                            dma("sp", lambda e: e.dma_start(out=k2.t[:], in_=KT[:, :, tt0:tt0 + 128].rearrange("h p t -> p h t")), writes=[k2.tl])
                            dma("sp", lambda e: e.dma_start(out=kt2.t[:], in_=KTOK[tt0:tt0 + 128, :]), writes=[kt2.tl])
                            dma("sp", lambda e: e.dma_start(out=v2.t[:, :, 0:128], in_=VTOK[tt0:tt0 + 128, :].rearrange("t (h k) -> t h k", h=NH)),
                                writes=[v2.tl])
                    ps_load(0)
                    for step in range(NCH):
                        for d in range(2):
                            c = (order_f, order_b)[d][step]
                            Ch = Chd[d]
                            mask = cst.t[:, 128:256] if d == 0 else cst.t[:, 256:384]
                            t0 = c * 128
                            q_, k_, kt_, v_, hb_ = qTb[it % 2], kTb[it % 2], ktk[it % 2], vau[it % 2], hbuf[it % 2]
                            it += 1
                            ps_load(it)
                            skip_out = last and c < 2
                            for h in range(NH):
                                r = d * 4 + h
                                s_, o_, c_ = ps_s[n % 2], ps_o[n % 4], ps_c[n % 2]
                                cs_, sm_, vt_, dn_ = Csb[n % 4], Sm[n % 4], vt[n % 4], dn[n % 4]
                                n += 1
                                alpha = ABt.t[:, r, c:c + 1]
                                op("pool", lambda e, vt_=vt_, v_=v_, h=h, c=c, r=r: e.tensor_scalar(out=vt_.t[:], in0=v_.t[:, h, :], scalar1=TOKQ.t[:, c, r:r + 1],
                                                                                                scalar2=1.0, op0=ALU.mult, op1=ALU.mult),
                                   reads=[v_.tl, TOKQ.tl], writes=[vt_.tl])
                                if not skip_out:
                                    op("pe", lambda e, s_=s_, k_=k_, q_=q_, h=h: e.matmul(s_.t[:], lhsT=k_.t[:, h, :], rhs=q_.t[:, h, :], start=True, stop=True),
                                       reads=[k_.tl, q_.tl], writes=[s_.tl])
                                    op("dve", lambda e, sm_=sm_, s_=s_, mask=mask: e.tensor_tensor(out=sm_.t[:], in0=s_.t[:], in1=mask, op=ALU.mult),
                                       reads=[s_.tl, cst.tl], writes=[sm_.tl])
                                    op("act", lambda e, cs_=cs_, h=h, alpha=alpha: e.activation(out=cs_.t[:], in_=Ch[h].t[:], func=AF.Copy, scale=alpha),
                                       reads=[Ch[h].tl, ABt.tl], writes=[cs_.tl])
                                    op("pe", lambda e, o_=o_, q_=q_, cs_=cs_, h=h: e.matmul(o_.t[:], lhsT=q_.t[:, h, :], rhs=cs_.t[:], start=True, stop=False),
                                       reads=[q_.tl, cs_.tl], writes=[o_.tl])
                                    op("pe", lambda e, o_=o_, sm_=sm_, vt_=vt_: e.matmul(o_.t[:], lhsT=sm_.t[:], rhs=vt_.t[:], start=False, stop=True),
                                       reads=[sm_.tl, vt_.tl], writes=[o_.tl])
                                op("pe", lambda e, c_=c_, kt_=kt_, vt_=vt_, h=h: e.matmul(c_.t[:], lhsT=kt_.t[:, h * 128:(h + 1) * 128], rhs=vt_.t[:], start=True, stop=True),
                                   reads=[kt_.tl, vt_.tl], writes=[c_.tl])
                                op("dve", lambda e, c_=c_, h=h, alpha=alpha: e.scalar_tensor_tensor(out=Ch[h].t[:], in0=Ch[h].t[:], scalar=alpha, in1=c_.t[:],
                                                                                                    op0=ALU.mult, op1=ALU.add),
                                   reads=[Ch[h].tl, ABt.tl, c_.tl], writes=[Ch[h].tl])
                                if not skip_out:
                                    op("act", lambda e, dn_=dn_, o_=o_: e.activation(out=dn_.t[:, 0:1], in_=o_.t[:, 128:129], func=AF.Abs),
                                       reads=[o_.tl], writes=[dn_.tl])
                                    op("dve", lambda e, dn_=dn_, c=c, r=r: e.tensor_scalar(out=dn_.t[:, 0:1], in0=dn_.t[:, 0:1], scalar1=TOKQ.t[:, c, 8 + r:9 + r],
                                                                                          scalar2=None, op0=ALU.max),
                                       reads=[dn_.tl, TOKQ.tl], writes=[dn_.tl])
                                    op("dve", lambda e, dn_=dn_: e.reciprocal(out=dn_.t[:, 1:2], in_=dn_.t[:, 0:1]), reads=[dn_.tl], writes=[dn_.tl])
                                    op("act", lambda e, hb_=hb_, o_=o_, dn_=dn_, h=h: e.activation(out=hb_.t[:, h * 128:(h + 1) * 128], in_=o_.t[:, 0:128], func=AF.Copy,
                                                                                                 scale=dn_.t[:, 1:2]),
                                       reads=[o_.tl, dn_.tl], writes=[hb_.tl])
                            if not skip_out:
                                dma("sp", lambda e, hb_=hb_, d=d, t0=t0: e.dma_start(out=HFB[d, t0:t0 + 128, :], in_=hb_.t[:]), reads=[hb_.tl], writes=[tD])
                    fw.barrier()
                    fw.flush()

            if stop == "PS":
                return nc
            if dbg and l == 0:
                dma("sp", lambda e: e.dma_start(out=dbgout["dHFB"][:, :, :], in_=HFB[:, :, :]), reads=[tD], writes=[tD])

            with ExitStack() as lay:
                G2 = [sb(lay, "G2_%d" % s, [128, D]) for s in range(2)]
                for s in range(2):
                    load_bc(G2[s], s, 5)
                NSLT = (CAPL + 127) // 128 + 1
                IDX = sb(lay, "IDX", [128, NSLT, NE], U32)
                GV = sb(lay, "GV", [128, NSLT, NE])
                affs = lay.enter_context(ExitStack())
                AFF = sb(affs, "AFF", [16, NT])
                with ExitStack() as ph:
                    wo = sb(ph, "wo", [128, KC, D], BF16)
                    for kc in range(KC):
                        dma("pool", lambda e, kc=kc: e.dma_start(out=wo.t[:, kc, :], in_=w_out[l, kc * 128:(kc + 1) * 128, :]), writes=[wo.sub[kc]])
                    wr = sb(ph, "wr", [128, KC, NE])
                    dma("sp", lambda e: e.dma_start(out=wr.t[:], in_=w_router[l].rearrange("(k p) n -> p k n", p=128)), writes=[wr.tl])
                    G1 = [sb(ph, "G1_%d" % s, [128, D]) for s in range(2)]
                    A2 = [sb(ph, "A2_%d" % s, [128, D]) for s in range(2)]
                    S2 = [sb(ph, "S2_%d" % s, [128, D]) for s in range(2)]
                    for s in range(2):
                        load_bc(G1[s], s, 2)
                        load_bc(S2[s], s, 3)
                        load_bc(A2[s], s, 4)
                    ghb = sb(ph, "ghb", [128, 512])
                    dma("sp", lambda e: e.dma_start(out=ghb.t[:], in_=g_head[l].partition_broadcast(128)), writes=[ghb.tl])
                    hf = [sb(ph, "hf%d" % i, [128, 512]) for i in range(2)]
                    hbk = [sb(ph, "hbk%d" % i, [128, 512]) for i in range(2)]
                    so = [sb(ph, "so%d" % i, [128, 512], BF16) for i in range(2)]
                    yc = [sb(ph, "yc%d" % i, [128, 4, 128], BF16) for i in range(2)]
                    xt = [sb(ph, "xo%d" % i, [128, D]) for i in range(2)]
                    hs2 = [sb(ph, "hs%d" % i, [128, 512]) for i in range(2)]
                    junk2 = [sb(ph, "junk2_%d" % i, [128, D], BF16) for i in range(2)]
                    st4 = [sb(ph, "st4_%d" % i, [128, 12]) for i in range(2)]
                    ybf2 = [sb(ph, "ybf%d" % i, [128, 512], BF16) for i in range(2)]
                    yT = [sb(ph, "yT%d" % i, [128, 4, 128], BF16) for i in range(2)]
                    tmp2 = [sb(ph, "tmp2_%d" % i, [128, D]) for i in range(2)]
                    xn = [sb(ph, "xn%d" % i, [128, D]) for i in range(2)]
                    h22 = [sb(ph, "h2_%d" % i, [128, D]) for i in range(2)]
                    h2b = [sb(ph, "h2b%d" % i, [128, D], BF16) for i in range(2)]
                    h2T2 = [sb(ph, "h2T%d" % i, [128, KC, 128]) for i in range(2)]
                    pyt = ps(ph, "pyt", [128, 4, 128], BF16)
                    po = [ps(ph, "po%d" % i, [128, 512]) for i in range(2)]
                    pT = [ps(ph, "pT2_%d" % i, [128, 512]) for i in range(2)]
                    pr = [ps(ph, "pr%d" % i, [16, 128]) for i in range(2)]
                    tiles = [c for c in range(NCH) if not (last and c < 2)]
                    def po_load(i):
                        if i < len(tiles):
                            tt0 = tiles[i] * 128
                            bb = i % 2
                            dma("sp", lambda e: e.dma_start(out=hf[bb].t[:], in_=HFB[0, tt0:tt0 + 128, :]), writes=[hf[bb].tl])
                            dma("sp", lambda e: e.dma_start(out=hbk[bb].t[:], in_=HFB[1, tt0:tt0 + 128, :]), writes=[hbk[bb].tl])
                            dma("sp", lambda e: e.dma_start(out=so[bb].t[:], in_=OTOK[tt0:tt0 + 128, :]), writes=[so[bb].tl])
                            dma("sp", lambda e: e.dma_start(out=yc[bb].t[:], in_=YT[:, :, tt0:tt0 + 128].rearrange("j p t -> p j t")), writes=[yc[bb].tl])
                            dma("sp", lambda e: e.dma_start(out=xt[bb].t[:], in_=xrows(l, tt0)), reads=[tX], writes=[xt[bb].tl])
                    po_load(0)
                    for ti, c in enumerate(tiles):
                        t0 = c * 128
                        s = 1 if c < 2 else 0
                        b = ti % 2
                        po_load(ti + 1)
                        sq = st4[b]
                        hs, junk, ybf, tmp, h2, h2T, yTb = hs2[b], junk2[b], ybf2[b], tmp2[b], h22[b], h2T2[b], yT[b]
                        op("dve", lambda e, b=b: e.tensor_tensor(out=hs.t[:], in0=hf[b].t[:], in1=hbk[b].t[:], op=ALU.add), reads=[hf[b].tl, hbk[b].tl], writes=[hs.tl])
                        for h in range(NH):
                            op("act", lambda e, h=h, sq=sq: e.activation(out=junk.t[:, h * 128:(h + 1) * 128], in_=hs.t[:, h * 128:(h + 1) * 128], func=AF.Square,
                                                                        accum_out=sq.t[:, h:h + 1]), reads=[hs.tl], writes=[junk.tl, sq.tl])
                        op("act", lambda e, sq=sq: e.activation(out=sq.t[:, 4:8], in_=sq.t[:, 0:4], func=AF.Sqrt, scale=1.0 / DH, bias=EPS), reads=[sq.tl], writes=[sq.tl])
                        op("dve", lambda e, sq=sq: e.reciprocal(out=sq.t[:, 8:12], in_=sq.t[:, 4:8]), reads=[sq.tl], writes=[sq.tl])
                        for h in range(NH):
                            op("dve", lambda e, h=h, sq=sq: e.scalar_tensor_tensor(out=hs.t[:, h * 128:(h + 1) * 128], in0=hs.t[:, h * 128:(h + 1) * 128],
                                                                                  scalar=sq.t[:, 8 + h:9 + h], in1=ghb.t[:, h * 128:(h + 1) * 128],
                                                                                  op0=ALU.mult, op1=ALU.mult), reads=[hs.tl, sq.tl, ghb.tl], writes=[hs.tl])
                        op("dve", lambda e, b=b: e.tensor_tensor(out=ybf.t[:], in0=hs.t[:], in1=so[b].t[:], op=ALU.mult), reads=[hs.tl, so[b].tl], writes=[ybf.tl])
                        for h in range(NH):
                            op("pe", lambda e, h=h: e.transpose(out=pyt.t[:, h, :], in_=ybf.t[:, h * 128:(h + 1) * 128], identity=identb.t[:]),
                               reads=[ybf.tl, identb.tl], writes=[pyt.tl])
                        op("act", lambda e: e.copy(out=yTb.t[:], in_=pyt.t[:]), reads=[pyt.tl], writes=[yTb.tl])
                        for half in range(2):
                            p = po[half]
                            for kc in range(KC):
                                lhs = yc[b].t[:, kc, :] if kc < 4 else yTb.t[:, kc - 4, :]
                                op("pe", lambda e, p=p, lhs=lhs, kc=kc, half=half: e.matmul(p.t[:], lhsT=lhs, rhs=wo.t[:, kc, half * 512:(half + 1) * 512],
                                                                                          start=(kc == 0), stop=(kc == KC - 1)),
                                   reads=[yc[b].tl, yT[b].tl, wo.sub[kc]], writes=[p.tl])
                            op("dve", lambda e, p=p, half=half, s=s: e.tensor_tensor(out=tmp.t[:, half * 512:(half + 1) * 512], in0=p.t[:],
                                                                                    in1=G1[s].t[:, half * 512:(half + 1) * 512], op=ALU.mult),
                               reads=[p.tl, G1[s].tl], writes=[tmp.tl])
                        op("dve", lambda e, b=b: e.tensor_tensor(out=xn[b].t[:], in0=tmp.t[:], in1=xt[b].t[:], op=ALU.add), reads=[tmp.tl, xt[b].tl], writes=[xn[b].tl])
                        dma("sp", lambda e, b=b, t0=t0: e.dma_start(out=X[t0:t0 + 128, :], in_=xn[b].t[:]), reads=[xn[b].tl], writes=[tX])
                        op("act", lambda e, b=b, sq=sq: e.activation(out=junk.t[:], in_=xn[b].t[:], func=AF.Square, accum_out=sq.t[:, 0:1]),
                           reads=[xn[b].tl], writes=[junk.tl, sq.tl])
                        op("act", lambda e, sq=sq: e.activation(out=sq.t[:, 1:2], in_=sq.t[:, 0:1], func=AF.Sqrt, scale=1.0 / D, bias=EPS), reads=[sq.tl], writes=[sq.tl])
                        op("dve", lambda e, sq=sq: e.reciprocal(out=sq.t[:, 2:3], in_=sq.t[:, 1:2]), reads=[sq.tl], writes=[sq.tl])
                        op("dve", lambda e, b=b, sq=sq, s=s: e.scalar_tensor_tensor(out=tmp.t[:], in0=xn[b].t[:], scalar=sq.t[:, 2:3], in1=A2[s].t[:],
                                                                                   op0=ALU.mult, op1=ALU.mult), reads=[xn[b].tl, sq.tl, A2[s].tl], writes=[tmp.tl])
                        op("dve", lambda e, s=s: e.tensor_tensor(out=h2.t[:], in0=tmp.t[:], in1=S2[s].t[:], op=ALU.add), reads=[tmp.tl, S2[s].tl], writes=[h2.tl])
                        op("act", lambda e, b=b: e.copy(out=h2b[b].t[:], in_=h2.t[:]), reads=[h2.tl], writes=[h2b[b].tl])
                        dma("sp", lambda e, b=b, t0=t0: e.dma_start(out=H2[t0:t0 + 128, :], in_=h2b[b].t[:]), reads=[h2b[b].tl], writes=[tD])
                        for kc in range(KC):
                            p = pT[kc // 4]
                            op("pe", lambda e, p=p, kc=kc: e.transpose(out=p.t[:, (kc % 4) * 128:(kc % 4 + 1) * 128], in_=h2.t[:, kc * 128:(kc + 1) * 128], identity=ident),
                               reads=[h2.tl, cst.tl], writes=[p.tl])
                        op("act", lambda e: e.copy(out=h2T.t[:, 0:4, :], in_=pT[0].t[:].rearrange("p (k t) -> p k t", k=4)), reads=[pT[0].tl], writes=[h2T.tl])
                        op("dve", lambda e: e.tensor_copy(out=h2T.t[:, 4:8, :], in_=pT[1].t[:].rearrange("p (k t) -> p k t", k=4)), reads=[pT[1].tl], writes=[h2T.tl])
                        p = pr[ti % 2]
                        for kc in range(KC):
                            op("pe", lambda e, p=p, kc=kc: e.matmul(p.t[:], lhsT=wr.t[:, kc, :], rhs=h2T.t[:, kc, :], start=(kc == 0), stop=(kc == KC - 1)),
                               reads=[wr.tl, h2T.tl], writes=[p.tl])
                        op("act", lambda e, p=p, t0=t0: e.activation(out=AFF.t[:, t0:t0 + 128], in_=p.t[:], func=AF.Exp), reads=[p.tl], writes=[AFF.tl])
                    fw.barrier()
                    fw.flush()
                if dbg and l == 0:
                    dma("sp", lambda e: e.dma_start(out=dbgout["dX1"][:, :], in_=X[:, :]), reads=[tX], writes=[tD])

                if stop == "PO":
                    affs.close()
                    return nc
                groups = []
                if not last:
                    groups.append(("c", 0, CAPC))
                for g0 in range(0, CAPL, 512):
                    groups.append(("l", g0, min(512, CAPL - g0)))
                with ExitStack() as ph2:
                    with ExitStack() as ph:
                        TV = sb(ph, "TV", [16, CAPL + CAPC])
                        TI = sb(ph, "TI", [16, CAPL + CAPC], U32)
                        rs = sb(ph, "rs", [16, 512])
                        pn = [ps(ph, "pn%d" % i, [16, 512]) for i in range(2)]
                        pt = ps(ph, "ptk", [128, 32])
                        c0s = ([] if last else [(0, NCTX)]) + [(NCTX + i * 512, 512) for i in range(NLAT // 512)]
                        for i, (c0, n) in enumerate(c0s):
                            p = pn[i % 2]
                            op("pe", lambda e, p=p, c0=c0, n=n: e.matmul(p.t[:, 0:n], lhsT=cst.t[0:16, 1472:1488], rhs=AFF.t[:, c0:c0 + n], start=True, stop=True),
                               reads=[cst.tl, AFF.tl], writes=[p.tl])
                            op("dve", lambda e, p=p, n=n: e.reciprocal(out=rs.t[:, 0:n], in_=p.t[:, 0:n]), reads=[p.tl], writes=[rs.tl])
                            op("dve", lambda e, c0=c0, n=n: e.tensor_tensor(out=AFF.t[:, c0:c0 + n], in0=AFF.t[:, c0:c0 + n], in1=rs.t[:, 0:n], op=ALU.mult),
                               reads=[AFF.tl, rs.tl], writes=[AFF.tl])
                        if dbg and l == 0:
                            dma("sp", lambda e: e.dma_start(out=dbgout["dAFF"][:, :], in_=AFF.t[:]), reads=[AFF.tl], writes=[tD])
                        if stop == "TK1":
                            fw.barrier()
                            fw.flush()
                            raise _Stop(nc)
                        HL = NLAT // 2
                        assert HL & (HL - 1) == 0
                        AF2 = sb(ph, "AF2", [32, HL])
                        TV2 = sb(ph, "TV2", [32, CAPL])
                        TI2 = sb(ph, "TI2", [32, CAPL], U32)
                        TV3 = sb(ph, "TV3", [16, 2, CAPL])
                        TI3 = sb(ph, "TI3", [16, 2, CAPL], U32)
                        chl = sb(ph, "chl", [16, CAPL], U32)
                        msk = sb(ph, "msk", [16, CAPL], U32)
                        tA = Tl()
                        dma("sp", lambda e: e.dma_start(out=AFD[:, :], in_=AFF.t[:, NCTX:NT]), reads=[AFF.tl], writes=[tA])
                        dma("sp", lambda e: e.dma_start(out=AF2.t[:], in_=AFD.rearrange("e (h n) -> (e h) n", h=2)), reads=[tA], writes=[AF2.tl])
                        for it in range(CAPL // 8):
                            o = it * 8
                            op("dve", lambda e: e.max(out=TV2.t[:, o:o + 8], in_=AF2.t[:]), reads=[AF2.tl], writes=[TV2.tl])
                            op("dve", lambda e: e.max_index(out=TI2.t[:, o:o + 8], in_max=TV2.t[:, o:o + 8], in_values=AF2.t[:]), reads=[AF2.tl, TV2.tl], writes=[TI2.tl])
                            op("dve", lambda e: e.match_replace(out=AF2.t[:], in_to_replace=TV2.t[:, o:o + 8], in_values=AF2.t[:], imm_value=-1.0),
                               reads=[TV2.tl, AF2.tl], writes=[AF2.tl])
                        tB = Tl()
                        dma("sp", lambda e: e.dma_start(out=TVD[:, :], in_=TV2.t[:]), reads=[TV2.tl], writes=[tB])
                        dma("sp", lambda e: e.dma_start(out=TV3.t[:], in_=TVD.rearrange("(e h) n -> e h n", h=2)), reads=[tB], writes=[TV3.tl])
                        tC = Tl()
                        dma("sp", lambda e: e.dma_start(out=TID[:, :], in_=TI2.t[:]), reads=[TI2.tl], writes=[tC])
                        dma("sp", lambda e: e.dma_start(out=TI3.t[:], in_=TID.rearrange("(e h) n -> e h n", h=2)), reads=[tC], writes=[TI3.tl])
                        op("dve", lambda e: e.memset(chl.t[:], HL), writes=[chl.tl])
                        op("dve", lambda e: e.tensor_tensor(out=TI3.t[:, 1, :], in0=TI3.t[:, 1, :], in1=chl.t[:], op=ALU.bitwise_or), reads=[TI3.tl, chl.tl], writes=[TI3.tl])
                        op("dve", lambda e: e.tensor_tensor(out=msk.t[:], in0=TV3.t[:, 0, :], in1=TV3.t[:, 1, ::-1], op=ALU.is_ge), reads=[TV3.tl], writes=[msk.tl])
                        op("dve", lambda e: e.tensor_tensor(out=TV.t[:, 0:CAPL], in0=TV3.t[:, 0, :], in1=TV3.t[:, 1, ::-1], op=ALU.max), reads=[TV3.tl], writes=[TV.tl])
                        op("dve", lambda e: e.tensor_copy(out=TI.t[:, 0:CAPL], in_=TI3.t[:, 1, ::-1]), reads=[TI3.tl], writes=[TI.tl])
                        op("dve", lambda e: e.copy_predicated(out=TI.t[:, 0:CAPL], mask=msk.t[:], data=TI3.t[:, 0, :]), reads=[msk.tl, TI3.tl, TI.tl], writes=[TI.tl])
                        sets = ([] if last else [(0, NCTX, CAPL, CAPC)])
                        for (c0, n, o0, cap) in sets:
                            av = AFF.t[:, c0:c0 + n]
                            for it in range(cap // 8):
                                o = o0 + it * 8
                                op("dve", lambda e, av=av, o=o: e.max(out=TV.t[:, o:o + 8], in_=av), reads=[AFF.tl], writes=[TV.tl])
                                op("dve", lambda e, av=av, o=o: e.max_index(out=TI.t[:, o:o + 8], in_max=TV.t[:, o:o + 8], in_values=av), reads=[AFF.tl, TV.tl], writes=[TI.tl])
                                op("dve", lambda e, av=av, o=o: e.match_replace(out=av, in_to_replace=TV.t[:, o:o + 8], in_values=av, imm_value=-1.0),
                                   reads=[TV.tl, AFF.tl], writes=[AFF.tl])
                        if stop == "TK2":
                            fw.barrier()
                            fw.flush()
                            raise _Stop(nc)
                        tT = Tl()
                        dma("sp", lambda e: e.dma_start(out=TIS[:, :], in_=TI.t[:]), reads=[TI.tl], writes=[tT])
                        slot_tiles = [(j, j * 128, min(128, CAPL - j * 128)) for j in range((CAPL + 127) // 128)]
                        if not last:
                            slot_tiles.append((NSLT - 1, CAPL, CAPC))
                        for (j, o, n) in slot_tiles:
                            dma("sp", lambda e, j=j, o=o, n=n: e.dma_start(out=IDX.t[0:n, j, :], in_=TIS[:, o:o + n].rearrange("e p -> p e"),
                                                                          allow_slow_non_contiguous=True),
                                reads=[tT], writes=[IDX.tl])
                            op("pe", lambda e, o=o, n=n: e.transpose(out=pt.t[0:n, 16:32], in_=TV.t[:, o:o + n], identity=cst.t[0:16, 0:16]), reads=[TV.tl, cst.tl], writes=[pt.tl])
                            op("dve", lambda e, j=j, n=n: e.tensor_copy(out=GV.t[0:n, j, :], in_=pt.t[0:n, 16:32]), reads=[pt.tl], writes=[GV.tl])
                        fw.barrier()
                        fw.flush()
                    affs.close()
                    if stop == "TK":
                        return nc
                    with ExitStack() as ph:
                        wg = [sb(ph, "wg%d" % i, [128, KC, DEXP], BF16) for i in range(2)]
                        wu = [sb(ph, "wu%d" % i, [128, KC, DEXP], BF16) for i in range(2)]
                        wd = [sb(ph, "wd%d" % i, [128, FCH, D], BF16) for i in range(2)]
                        xg = [sb(ph, "xg%d" % i, [128, D], BF16) for i in range(8)]
                        xeT2 = [sb(ph, "xeT%d" % i, [128, KC, 512], BF16) for i in range(2)]
                        ngrp = 0
                        sa = [sb(ph, "sa%d" % i, [128, 512]) for i in range(2)]
                        actT = sb(ph, "actT", [128, FCH, 512], BF16)
                        ye = [sb(ph, "ye%d" % i, [128, D]) for i in range(2)]
                        pxt = [ps(ph, "pxt%d" % i, [128, KC, 128], BF16) for i in range(2)]
                        pa_ = [ps(ph, "pa_%d" % i, [128, 512]) for i in range(2)]
                        pu_ = [ps(ph, "pu_%d" % i, [128, 512]) for i in range(1)]
                        py = [ps(ph, "py%d" % i, [128, 512]) for i in range(2)]

                        def load_w(e_):
                            b = e_ % 2
                            for (wt, src, nk) in ((wg[b], w_gate_e, KC), (wu[b], w_up_e, KC), (wd[b], w_down_e, FCH)):
                                srcv = src[l, e_].rearrange("(k p) n -> p k n", p=128)
                                for (k0, k1) in ((0, nk // 2), (nk // 2, nk)):
                                    dma("pool", lambda e: e.dma_start(out=wt.t[:, k0:k1, :], in_=srcv[:, k0:k1, :]), writes=[wt.sub[k] for k in range(k0, k1)])
                        def tiles_of(kind, g0, gn):
                            if kind == "c":
                                return [(NSLT - 1, gn)]
                            return [((g0 + o) // 128, min(128, gn - o)) for o in range(0, gn, 128)]
                        msteps = [(e2, grp) for e2 in range(NE) for grp in groups]

                        def gathers(si):
                            if si >= len(msteps):
                                return
                            e2, (kind2, g02, gn2) = msteps[si]
                            for ji2, (j2, n2) in enumerate(tiles_of(kind2, g02, gn2)):
                                gg = xg[(si % 2) * 4 + ji2]
                                dma("pool", lambda e: e.indirect_dma_start(
                                    out=gg.t[0:n2, :], out_offset=None, in_=H2[:, :], element_offset=(0 if kind2 == "c" else NCTX * D),
                                    in_offset=bass.IndirectOffsetOnAxis(ap=IDX.t[0:n2, j2, e2:e2 + 1], axis=0)), reads=[IDX.tl, tD], writes=[gg.tl])
                        load_w(0)
                        gathers(0)
                        ng = 0
                        ny = 0
                        si = -1
                        for e_ in range(NE):
                            b = e_ % 2
                            for gi_, (kind, g0, gn) in enumerate(groups):
                                si += 1
                                if gi_ == 0 and e_ + 1 < NE:
                                    load_w(e_ + 1)
                                s = 1 if kind == "c" else 0
                                xeTg = xeT2[ngrp % 2]
                                ngrp += 1
                                tl_ = tiles_of(kind, g0, gn)
                                for ji, (j, n) in enumerate(tl_):
                                    g_ = xg[(si % 2) * 4 + ji]
                                    px = pxt[ng % 2]
                                    ng += 1
                                    for kc in range(KC):
                                        op("pe", lambda e, g_=g_, px=px, kc=kc, n=n: e.transpose(out=px.t[:, kc, 0:n], in_=g_.t[0:n, kc * 128:(kc + 1) * 128],
                                                                                                identity=identb.t[0:n, 0:n]),
                                           reads=[g_.tl, identb.tl], writes=[px.tl])
                                    op("act", lambda e, px=px, ji=ji, n=n: e.copy(out=xeTg.t[:, :, ji * 128:ji * 128 + n], in_=px.t[:, :, 0:n]), reads=[px.tl], writes=[xeTg.tl])
                                for fc in range(FCH):
                                    pa1 = pa_[fc % 2]
                                    pu1 = pu_[0]
                                    s1 = sa[fc % 2]
                                    for kc in range(KC):
                                        op("pe", lambda e, pa1=pa1, kc=kc, fc=fc: e.matmul(pa1.t[:, 0:gn], lhsT=wg[b].t[:, kc, fc * 128:(fc + 1) * 128], rhs=xeTg.t[:, kc, 0:gn],
                                                                                          start=(kc == 0), stop=(kc == KC - 1)), reads=[wg[b].sub[kc], xeTg.tl], writes=[pa1.tl])
                                    for kc in range(KC):
                                        op("pe", lambda e, pu1=pu1, kc=kc, fc=fc: e.matmul(pu1.t[:, 0:gn], lhsT=wu[b].t[:, kc, fc * 128:(fc + 1) * 128], rhs=xeTg.t[:, kc, 0:gn],
                                                                                          start=(kc == 0), stop=(kc == KC - 1)), reads=[wu[b].sub[kc], xeTg.tl], writes=[pu1.tl])
                                    op("act", lambda e, pa1=pa1, s1=s1: e.activation(out=s1.t[:, 0:gn], in_=pa1.t[:, 0:gn], func=AF.Silu), reads=[pa1.tl], writes=[s1.tl])
                                    op("dve", lambda e, pu1=pu1, s1=s1, fc=fc: e.tensor_tensor(out=actT.t[:, fc, 0:gn], in0=pu1.t[:, 0:gn], in1=s1.t[:, 0:gn], op=ALU.mult),
                                       reads=[pu1.tl, s1.tl], writes=[actT.tl])
                                gathers(si + 1)
                                for ji, (j, n) in enumerate(tl_):
                                    y_ = ye[ny % 2]
                                    ny += 1
                                    for half in range(2):
                                        p = py[half]
                                        for fc in range(FCH):
                                            op("pe", lambda e, p=p, fc=fc, ji=ji, n=n, half=half: e.matmul(p.t[0:n, :], lhsT=actT.t[:, fc, ji * 128:ji * 128 + n],
                                                                                                          rhs=wd[b].t[:, fc, half * 512:(half + 1) * 512],
                                                                                                          start=(fc == 0), stop=(fc == FCH - 1)),
                                               reads=[actT.tl, wd[b].sub[fc]], writes=[p.tl])
                                        op("dve", lambda e, p=p, y_=y_, j=j, n=n, half=half, s=s, e_=e_: e.scalar_tensor_tensor(
                                            out=y_.t[0:n, half * 512:(half + 1) * 512], in0=p.t[0:n, :], scalar=GV.t[0:n, j, e_:e_ + 1],
                                            in1=G2[s].t[0:n, half * 512:(half + 1) * 512], op0=ALU.mult, op1=ALU.mult),
                                           reads=[p.tl, GV.tl, G2[s].tl], writes=[y_.tl])
                                    dma("pool", lambda e, y_=y_, j=j, n=n, e_=e_: e.indirect_dma_start(
                                        element_offset=(0 if kind == "c" else NCTX * D), out=X[:, :], out_offset=bass.IndirectOffsetOnAxis(ap=IDX.t[0:n, j, e_:e_ + 1], axis=0),
                                        in_=y_.t[0:n, :], in_offset=None, compute_op=ALU.add), reads=[IDX.tl, y_.tl], writes=[tX])
                        fw.barrier()
                        fw.flush()

        with ExitStack() as ph:
            gf = sb(ph, "gf", [128, D])
            dma("sp", lambda e: e.dma_start(out=gf.t[:], in_=g_final.partition_broadcast(128)), writes=[gf.tl])
            xt = [sb(ph, "xf%d" % i, [128, D]) for i in range(2)]
            yo = [sb(ph, "yo%d" % i, [128, D]) for i in range(2)]
            junk = sb(ph, "junk3", [128, D], BF16)
            ss = [sb(ph, "ssf%d" % i, [128, 4]) for i in range(2)]
            tO = Tl()
            def pf_load(i):
                if i < NLAT // 128:
                    rr = NCTX + i * 128
                    dma("sp", lambda e: e.dma_start(out=xt[i % 2].t[:], in_=X[rr:rr + 128, :]), reads=[tX], writes=[xt[i % 2].tl])
            pf_load(0)
            for i in range(NLAT // 128):
                b = i % 2
                r0 = NCTX + i * 128
                sq = ss[b]
                pf_load(i + 1)
                op("act", lambda e, b=b, sq=sq: e.activation(out=junk.t[:], in_=xt[b].t[:], func=AF.Square, accum_out=sq.t[:, 0:1]), reads=[xt[b].tl], writes=[junk.tl, sq.tl])
                op("act", lambda e, sq=sq: e.activation(out=sq.t[:, 1:2], in_=sq.t[:, 0:1], func=AF.Sqrt, scale=1.0 / D, bias=EPS), reads=[sq.tl], writes=[sq.tl])
                op("dve", lambda e, sq=sq: e.reciprocal(out=sq.t[:, 2:3], in_=sq.t[:, 1:2]), reads=[sq.tl], writes=[sq.tl])
                op("dve", lambda e, b=b, sq=sq: e.scalar_tensor_tensor(out=yo[b].t[:], in0=xt[b].t[:], scalar=sq.t[:, 2:3], in1=gf.t[:], op0=ALU.mult, op1=ALU.mult),
                   reads=[xt[b].tl, sq.tl, gf.tl], writes=[yo[b].tl])
                dma("sp", lambda e, b=b, i=i: e.dma_start(out=out[i * 128:(i + 1) * 128, :], in_=yo[b].t[:]), reads=[yo[b].tl], writes=[tO])
            if dbg:
                dma("sp", lambda e: e.dma_start(out=dbgout["dX"][:, :], in_=X[:, :]), reads=[tX], writes=[tD])
                for nm in ("QT", "KT", "KTOK", "VTOK", "OTOK", "YT", "H2", "MODROW"):
                    dst, src = dbgout["d" + nm]
                    if len(src.shape) == 3:
                        dma("sp", lambda e, dst=dst, src=src: e.dma_start(out=dst[:, :, :], in_=src[:, :, :]), reads=[tD], writes=[tD])
                    else:
                        dma("sp", lambda e, dst=dst, src=src: e.dma_start(out=dst[:, :], in_=src[:, :]), reads=[tD], writes=[tD])
            fw.barrier()
            fw.flush()
        print("instructions (incl waits):", fw.ninst)
    return nc


def host_inputs(inputs, b, nlat=None):
    f = lambda a: np.ascontiguousarray(np.asarray(a, dtype=np.float32))
    L = inputs["w_mod"].shape[0]
    c = np.asarray(inputs["c"], np.float32)[b]
    cc = np.asarray(inputs["c_ctx"], np.float32)
    cT = np.stack([c.reshape(8, 128).T, cc.reshape(8, 128).T], axis=1)
    cw = np.asarray(inputs["conv_w"], np.float32).reshape(L, 3, 4, 128).transpose(0, 3, 1, 2)
    cb = np.asarray(inputs["conv_b"], np.float32).reshape(L, 4, 128).transpose(0, 2, 1)
    m = {
        "x": f(inputs["x"][b]), "ctx": f(inputs["ctx"][b]), "cT": f(cT),
        "w_mod": f(inputs["w_mod"]), "b_mod": f(inputs["b_mod"]),
        "g_norm1": f(inputs["g_norm1"]), "g_norm2": f(inputs["g_norm2"]),
        "w_in": f(inputs["w_in"]), "w_out": f(inputs["w_out"]),
        "conv_w": f(cw), "conv_b": f(cb),
        "gate_b": f(np.asarray(inputs["gate_b"], np.float32).reshape(L, 16, 1)),
        "g_head": f(np.asarray(inputs["g_head"], np.float32).reshape(L, 512)),
        "w_router": f(inputs["w_router"]),
        "w_gate_e": f(inputs["w_gate_e"]), "w_up_e": f(inputs["w_up_e"]), "w_down_e": f(inputs["w_down_e"]),
        "g_final": f(inputs["g_final"]), "consts": make_consts(),
    }
    return m


def kernel(**inputs):
    x = np.asarray(inputs["x"])
    nb, nlat, _ = x.shape
    depth = np.asarray(inputs["w_mod"]).shape[0]
    nc = build(nlat, depth)
    shared = host_inputs(inputs, 0)
    in_maps = []
    for b in range(nb):
        m = dict(shared)
        mb = host_inputs({**inputs, "w_mod": inputs["w_mod"]}, b) if False else None
        c = np.asarray(inputs["c"], np.float32)[b]
        cc = np.asarray(inputs["c_ctx"], np.float32)
        m["x"] = np.ascontiguousarray(np.asarray(inputs["x"][b], np.float32))
        m["ctx"] = np.ascontiguousarray(np.asarray(inputs["ctx"][b], np.float32))
        m["cT"] = np.ascontiguousarray(np.stack([c.reshape(8, 128).T, cc.reshape(8, 128).T], axis=1))
        in_maps.append(m)
    res = run_bass_kernel_spmd(nc, in_maps, core_ids=list(range(nb)))
    return np.stack([np.asarray(r["out"], np.float32) for r in res.results], axis=0)
```

```python
import numpy as np
from contextlib import ExitStack
import concourse.bass as bass
import concourse.mybir as mybir
from concourse.bass_utils import run_bass_kernel_spmd

F32 = mybir.dt.float32
BF16 = mybir.dt.bfloat16
U32 = mybir.dt.uint32
AF = mybir.ActivationFunctionType
ALU = mybir.AluOpType
AX = mybir.AxisListType

D = 1024
KC = 8
NCTX = 256
NH = 4
DH = 128
NE = 16
DEXP = 1408
FCH = 11
DPROJ = 3600
EPS = 1e-6
CW = 1488


class Tl:
    __slots__ = ("w", "r")

    def __init__(self):
        self.w = None
        self.r = {}


class _Rec:
    def __init__(self):
        self.call = None

    def __getattr__(self, name):
        def f(*a, **k):
            self.call = (name, a, k)
            return self
        return f


def _record(fn):
    r = _Rec()
    fn(r)
    name, a, k = r.call
    return lambda e: getattr(e, name)(*a, **k)


class FW:
    NDS = 6

    def __init__(self, nc, es):
        self.nc = nc
        self.engs = ("pe", "act", "dve", "pool", "sp")
        self.esem = {k: es.enter_context(nc.semaphore("s_" + k)) for k in self.engs}
        self.ecnt = {k: 0 for k in self.engs}
        self.dsem = {}
        self.dcnt = {}
        self.drr = {}
        for q in ("sp", "act", "pool"):
            self.drr[q] = 0
            for i in range(self.NDS):
                self.dsem[(q, i)] = es.enter_context(nc.semaphore("d_%s%d" % (q, i)))
                self.dcnt[(q, i)] = 0
        self.seen = {k: {} for k in self.engs}
        self.prog = {k: [] for k in self.engs}
        self.ninst = 0

    def _sem(self, key):
        return self.esem[key[1]] if key[0] == "e" else self.dsem[(key[1], key[2])]

    def _needs(self, eng, reads, writes, extra=()):
        need = {}

        def add(ev):
            if ev is None:
                return
            k, v = ev
            if k == ("e", "pe") and eng == "pe":
                return
            if need.get(k, 0) < v:
                need[k] = v
        for t in reads:
            add(t.w)
        for t in writes:
            add(t.w)
            for k, v in t.r.items():
                add((k, v))
        for ev in extra:
            add(ev)
        out = []
        seen = self.seen[eng]
        for k, v in need.items():
            if seen.get(k, 0) < v:
                seen[k] = v
                out.append((self._sem(k), v))
        return out

    def _commit(self, ev, reads, writes):
        k, v = ev
        for t in writes:
            t.w = ev
            t.r = {}
        for t in reads:
            if t.r.get(k, 0) < v:
                t.r[k] = v

    def op(self, eng, fn, reads=(), writes=()):
        fn = _record(fn)
        waits = self._needs(eng, reads, writes)
        self.ecnt[eng] += 1
        ev = (("e", eng), self.ecnt[eng])
        sem = self.esem[eng]

        def emit(e, waits=waits, fn=fn, sem=sem):
            for s, v in waits:
                e.wait_ge(s, v)
            fn(e).then_inc(sem, 1)
        self.prog[eng].append(emit)
        self._commit(ev, reads, writes)
        self.ninst += 1 + len(waits)
        return ev

    def dma(self, q, fn, reads=(), writes=()):
        fn = _record(fn)
        i = self.drr[q]
        self.drr[q] = (i + 1) % self.NDS
        key = ("d", q, i)
        prev = (key, self.dcnt[(q, i)]) if self.dcnt[(q, i)] else None
        waits = self._needs(q, reads, writes, extra=(prev,) if prev else ())
        self.dcnt[(q, i)] += 16
        ev = (key, self.dcnt[(q, i)])
        sem = self.dsem[(q, i)]

        def emit(e, waits=waits, fn=fn, sem=sem):
            for s, v in waits:
                e.wait_ge(s, v)
            fn(e).then_inc(sem, 16)
        self.prog[q].append(emit)
        self._commit(ev, reads, writes)
        self.ninst += 1 + len(waits)
        return ev

    def barrier(self):
        allev = [(("e", k), self.ecnt[k]) for k in self.engs if self.ecnt[k]]
        allev += [(("d", q, i), c) for (q, i), c in self.dcnt.items() if c]
        for eng in self.engs:
            waits = []
            seen = self.seen[eng]
            for k, v in allev:
                if seen.get(k, 0) < v:
                    seen[k] = v
                    waits.append((self._sem(k), v))

            def emit(e, waits=waits):
                for s, v in waits:
                    e.wait_ge(s, v)
            self.prog[eng].append(emit)

    def flush(self):
        nc = self.nc
        prog = self.prog
        self.prog = {k: [] for k in self.engs}
        with nc.Block() as block:
            @block.tensor
            def _(e):
                for f in prog["pe"]:
                    f(e)

            @block.scalar
            def _(e):
                for f in prog["act"]:
                    f(e)

            @block.vector
            def _(e):
                for f in prog["dve"]:
                    f(e)

            @block.gpsimd
            def _(e):
                for f in prog["pool"]:
                    f(e)

            @block.sync
            def _(e):
                for f in prog["sp"]:
                    f(e)


class B:
    def __init__(self, t):
        self.t = t
        self.tl = Tl()
        self.sub = [Tl() for _ in range(16)]


def make_consts():
    c = np.zeros((128, CW), np.float32)
    c[:, 0:128] = np.eye(128)
    s = np.arange(128)[:, None]
    t = np.arange(128)[None, :]
    c[:, 128:256] = (s <= t)
    c[:, 256:384] = (s >= t)
    MZG, MSG, MLG, MSB, MLB, MC = (c[0:16, 384 + 8 * i:392 + 8 * i] for i in range(6))
    for h in range(4):
        i_f, f_f, i_b, f_b = h, 4 + h, 8 + h, 12 + h
        MZG[i_f, h] = 1
        MZG[i_b, 4 + h] = 1
        MSG[f_f, h] = -1
        MSG[f_b, 4 + h] = 1
        MLG[f_b, 4 + h] = -1
        MSB[f_f, h] = 1
        MSB[f_b, 4 + h] = -1
        MLB[f_b, 4 + h] = 1
        MC[f_b, 4 + h] = 1
    c[0:4, 432] = 1
    c[4:8, 433] = 1
    for r in range(8):
        c[r, 440 + r * 128:440 + (r + 1) * 128] = 1
    c[:, 1464] = 1
    c[0:16, 1472:1488] = 1
    return c


class _Stop(Exception):
    pass


def build(NLAT, DEPTH, dbg=False, stop=None):
    try:
        return _build(NLAT, DEPTH, dbg, stop)
    except _Stop as ex:
        return ex.args[0]


def _build(NLAT, DEPTH, dbg=False, stop=None):
    NT = NCTX + NLAT
    NCH = NT // 128
    CAPL = 2 * NLAT // NE
    CAPC = 2 * NCTX // NE
    nc = bass.Bass("TRN2", target_bir_lowering=False)

    def din(name, shape, dt=F32):
        return nc.dram_tensor(name, shape, dt, kind="ExternalInput").ap()

    def dscr(name, shape, dt=F32):
        return nc.dram_tensor(name, shape, dt, kind="Internal").ap()

    x_in = din("x", [NLAT, D])
    ctx_in = din("ctx", [NCTX, D])
    cT_in = din("cT", [128, 2, 8])
    w_mod = din("w_mod", [DEPTH, D, 6 * D])
    b_mod = din("b_mod", [DEPTH, 6 * D])
    g_norm1 = din("g_norm1", [DEPTH, D])
    g_norm2 = din("g_norm2", [DEPTH, D])
    w_in = din("w_in", [DEPTH, D, DPROJ])
    w_out = din("w_out", [DEPTH, D, D])
    conv_w = din("conv_w", [DEPTH, 128, 3, 4])
    conv_b = din("conv_b", [DEPTH, 128, 4])
    gate_b = din("gate_b", [DEPTH, 16, 1])
    g_head = din("g_head", [DEPTH, 512])
    w_router = din("w_router", [DEPTH, D, NE])
    w_gate_e = din("w_gate_e", [DEPTH, NE, D, DEXP])
    w_up_e = din("w_up_e", [DEPTH, NE, D, DEXP])
    w_down_e = din("w_down_e", [DEPTH, NE, DEXP, D])
    g_final = din("g_final", [D])
    consts_in = din("consts", [128, CW])
    out = nc.dram_tensor("out", [NLAT, D], F32, kind="ExternalOutput").ap()

    X = dscr("Xs", [NT, D])
    MODROW = dscr("MODROW", [2, 6 * D])
    QT = dscr("QT", [NH, 128, NT], BF16)
    KT = dscr("KT", [NH, 128, NT], BF16)
    KTOK = dscr("KTOK", [NT, 512], BF16)
    VTOK = dscr("VTOK", [NT, 512], BF16)
    OTOK = dscr("OTOK", [NT, 512], BF16)
    YT = dscr("YT", [4, 128, NT], BF16)
    HFB = dscr("HFB", [2, NT, 512])
    H2 = dscr("H2", [NT, D], BF16)
    TIS = dscr("TIS", [16, CAPL + CAPC], U32)
    AFD = dscr("AFD", [16, NLAT])
    TVD = dscr("TVD", [64, CAPL])
    TID = dscr("TID", [64, CAPL], U32)
    dbgout = {}
    if dbg:
        dbgout["dX"] = nc.dram_tensor("dX", [NT, D], F32, kind="ExternalOutput").ap()
        dbgout["dHFB"] = nc.dram_tensor("dHFB", [2, NT, 512], F32, kind="ExternalOutput").ap()
        dbgout["dAFF"] = nc.dram_tensor("dAFF", [16, NT], F32, kind="ExternalOutput").ap()
        dbgout["dX1"] = nc.dram_tensor("dX1", [NT, D], F32, kind="ExternalOutput").ap()
        dbgout["dTOKQ"] = nc.dram_tensor("dTOKQ", [128, NCH * 16], F32, kind="ExternalOutput").ap()
        for nm, src in (("QT", QT), ("KT", KT), ("KTOK", KTOK), ("VTOK", VTOK), ("OTOK", OTOK), ("YT", YT), ("H2", H2), ("MODROW", MODROW)):
            dbgout["d" + nm] = (nc.dram_tensor("d" + nm, list(src.shape), src.dtype, kind="ExternalOutput").ap(), src)
        dbgout["dGP"] = nc.dram_tensor("dGP", [16, NT], F32, kind="ExternalOutput").ap()

    blocks = [(0, NCTX, True)] + [(NCTX + i * 512, 512, False) for i in range(NLAT // 512)]
    order_f = list(range(NCH))
    order_b = [1, 0] + list(range(NCH - 1, 1, -1))

    with ExitStack() as es:
        fw = FW(nc, es)
        op, dma = fw.op, fw.dma

        uid = [0]

        def sb(scope, name, shape, dt=F32):
            uid[0] += 1
            return B(scope.enter_context(nc.sbuf_tensor("%s_%d" % (name, uid[0]), shape, dt)))

        def ps(scope, name, shape, dt=F32):
            full = [128, 512] if dt == F32 else [128, 1024]
            uid[0] += 1
            t = scope.enter_context(nc.psum_tensor("%s_%d" % (name, uid[0]), full, dt))
            n = 1
            for v in shape[1:]:
                n *= v
            v = t[0:shape[0], 0:n]
            if len(shape) == 3:
                v = v.rearrange("p (a b) -> p a b", a=shape[1])
            return B(v)

        tX = Tl()
        tD = Tl()

        cst = sb(es, "cst", [128, CW])
        identb = sb(es, "identb", [128, 128], BF16)
        dma("sp", lambda e: e.dma_start(out=cst.t[:], in_=consts_in[:, :]), writes=[cst.tl])
        op("dve", lambda e: e.tensor_copy(out=identb.t[:], in_=cst.t[:, 0:128]), reads=[cst.tl], writes=[identb.tl])
        ident = cst.t[:, 0:128]

        def xrows(l_, r0_):
            if l_ > 0:
                return X[r0_:r0_ + 128, :]
            if r0_ < NCTX:
                return ctx_in[r0_:r0_ + 128, :]
            return x_in[r0_ - NCTX:r0_ - NCTX + 128, :]
        if DEPTH == 1:
            dma("sp", lambda e: e.dma_start(out=X[0:NCTX, :], in_=ctx_in[:, :]), writes=[tX])
        fw.barrier()
        fw.flush()

        for l in range(DEPTH):
            last = (l == DEPTH - 1)
            with ExitStack() as ph:
                scs = sb(ph, "scs", [128, 2, 8])
                wm = [sb(ph, "wm%d" % i, [128, 8, 512]) for i in range(2)]
                rows = [sb(ph, "mrow%d" % s, [1, 6 * D]) for s in range(2)]
                brow = sb(ph, "brow", [1, 6 * D])
                grow = sb(ph, "grow", [1, 2 * D])
                pm = [ps(ph, "pm%d" % s, [1, 512]) for s in range(2)]
                dma("sp", lambda e: e.dma_start(out=scs.t[:], in_=cT_in[:, :, :]), writes=[scs.tl])
                dma("sp", lambda e: e.dma_start(out=brow.t[:], in_=b_mod[l:l + 1, :]), writes=[brow.tl])
                dma("sp", lambda e: e.dma_start(out=grow.t[:, 0:D], in_=g_norm1[l:l + 1, :]), writes=[grow.tl])
                dma("sp", lambda e: e.dma_start(out=grow.t[:, D:2 * D], in_=g_norm2[l:l + 1, :]), writes=[grow.tl])
                op("act", lambda e: e.activation(out=scs.t[:], in_=scs.t[:], func=AF.Silu), reads=[scs.tl], writes=[scs.tl])
                wmv = w_mod[l].rearrange("(k p) n -> p k n", p=128)
                for blk in range(12):
                    w = wm[blk % 2]
                    dma("sp", lambda e, w=w, blk=blk: e.dma_start(out=w.t[:], in_=wmv[:, :, blk * 512:(blk + 1) * 512]), writes=[w.tl])
                    for s in range(2):
                        for kc in range(KC):
                            op("pe", lambda e, s=s, kc=kc, w=w: e.matmul(pm[s].t[:], lhsT=scs.t[:, s, kc:kc + 1], rhs=w.t[:, kc, :],
                                                                        start=(kc == 0), stop=(kc == KC - 1)),
                               reads=[scs.tl, w.tl], writes=[pm[s].tl])
                        op("dve", lambda e, s=s, blk=blk: e.tensor_tensor(out=rows[s].t[:, blk * 512:(blk + 1) * 512], in0=pm[s].t[:],
                                                                         in1=brow.t[:, blk * 512:(blk + 1) * 512], op=ALU.add),
                           reads=[pm[s].tl, brow.tl], writes=[rows[s].tl])
                for s in range(2):
                    for (o, g) in ((D, 0), (4 * D, D)):
                        op("dve", lambda e, s=s, o=o, g=g: e.scalar_tensor_tensor(out=rows[s].t[:, o:o + D], in0=rows[s].t[:, o:o + D], scalar=1.0,
                                                                                 in1=grow.t[:, g:g + D], op0=ALU.add, op1=ALU.mult),
                           reads=[rows[s].tl, grow.tl], writes=[rows[s].tl])
                    dma("sp", lambda e, s=s: e.dma_start(out=MODROW[s:s + 1, :], in_=rows[s].t[:]), reads=[rows[s].tl], writes=[tD])
                fw.barrier()
                fw.flush()

            if stop == "P0":
                return nc
            def load_bc(buf, s, j):
                dma("sp", lambda e: e.dma_start(out=buf.t[:], in_=MODROW[s, j * D:(j + 1) * D].partition_broadcast(128)), writes=[buf.tl])

            with ExitStack() as lay:
                GP = sb(lay, "GP", [16, NT])
                TOKQ = sb(lay, "TOKQ", [128, NCH, 16])
                ABt = sb(lay, "ABt", [128, 8, NCH])
                with ExitStack() as ph:
                    win = sb(ph, "win", [128, KC, DPROJ], BF16)
                    for kc in range(KC):
                        dma("pool", lambda e, kc=kc: e.dma_start(out=win.t[:, kc, :], in_=w_in[l, kc * 128:(kc + 1) * 128, :]), writes=[win.sub[kc]])
                    A1 = [sb(ph, "A1_%d" % s, [128, D]) for s in range(2)]
                    S1 = [sb(ph, "S1_%d" % s, [128, D]) for s in range(2)]
                    for s in range(2):
                        load_bc(S1[s], s, 0)
                        load_bc(A1[s], s, 1)
                    cw = sb(ph, "cw", [128, 3, 4])
                    cb = sb(ph, "cb", [128, 4])
                    gb = sb(ph, "gb", [16, 1])
                    dma("sp", lambda e: e.dma_start(out=cw.t[:], in_=conv_w[l]), writes=[cw.tl])
                    dma("sp", lambda e: e.dma_start(out=cb.t[:], in_=conv_b[l]), writes=[cb.tl])
                    dma("sp", lambda e: e.dma_start(out=gb.t[:], in_=gate_b[l]), writes=[gb.tl])
                    xt = [sb(ph, "xt%d" % i, [128, D]) for i in range(2)]
                    junk = sb(ph, "junk", [128, D], BF16)
                    tmp = sb(ph, "tmp", [128, D])
                    hn = [sb(ph, "hn%d" % i, [128, D]) for i in range(2)]
                    hT = [sb(ph, "hT%d" % i, [128, KC, 512], BF16) for i in range(2)]
                    ss = [sb(ph, "ss%d" % i, [128, 4]) for i in range(2)]
                    xin_sb = sb(ph, "xin_sb", [128, 512])
                    cx = sb(ph, "cx", [128, 512])
                    acc = sb(ph, "acc", [128, 512])
                    stf = [sb(ph, "stf%d" % i, [128, 512], BF16) for i in range(3)]
                    stt = [sb(ph, "stt%d" % i, [128, 512], BF16) for i in range(3)]
                    pT = [ps(ph, "pT%d" % i, [128, 512]) for i in range(2)]
                    pf = [ps(ph, "pf%d" % i, [128, 512]) for i in range(3)]
                    ptm = [ps(ph, "ptm%d" % i, [128, 512]) for i in range(2)]
                    ti = 0
                    pa_rows = [t0_ + tt_ * 128 for (t0_, ntok_, _c) in blocks for tt_ in range(ntok_ // 128)]

                    def pa_load(i):
                        if i < len(pa_rows):
                            xb_ = xt[i % 2]
                            rr = pa_rows[i]
                            dma("sp", lambda e: e.dma_start(out=xb_.t[:], in_=xrows(l, rr)), reads=[tX], writes=[xb_.tl])
                    pa_load(0)
                    nstf = 0
                    nstt = 0
                    npf = 0
                    for bi, (t0, ntok, isctx) in enumerate(blocks):
                        s = 1 if isctx else 0
                        hTb = hT[bi % 2]
                        for tt in range(ntok // 128):
                            r0 = t0 + tt * 128
                            xb = xt[ti % 2]
                            hb = hn[ti % 2]
                            sq = ss[ti % 2]
                            pa_load(ti + 1)
                            op("act", lambda e, xb=xb, sq=sq: e.activation(out=junk.t[:], in_=xb.t[:], func=AF.Square, accum_out=sq.t[:, 0:1]),
                               reads=[xb.tl], writes=[junk.tl, sq.tl])
                            op("act", lambda e, sq=sq: e.activation(out=sq.t[:, 1:2], in_=sq.t[:, 0:1], func=AF.Sqrt, scale=1.0 / D, bias=EPS),
                               reads=[sq.tl], writes=[sq.tl])
                            op("dve", lambda e, sq=sq: e.reciprocal(out=sq.t[:, 2:3], in_=sq.t[:, 1:2]), reads=[sq.tl], writes=[sq.tl])
                            op("dve", lambda e, xb=xb, sq=sq, s=s: e.scalar_tensor_tensor(out=tmp.t[:], in0=xb.t[:], scalar=sq.t[:, 2:3], in1=A1[s].t[:],
                                                                                         op0=ALU.mult, op1=ALU.mult),
                               reads=[xb.tl, sq.tl, A1[s].tl], writes=[tmp.tl])
                            op("dve", lambda e, hb=hb, s=s: e.tensor_tensor(out=hb.t[:], in0=tmp.t[:], in1=S1[s].t[:], op=ALU.add),
                               reads=[tmp.tl, S1[s].tl], writes=[hb.tl])
                            for kc in range(KC):
                                p = pT[kc // 4]
                                op("pe", lambda e, p=p, kc=kc, hb=hb: e.transpose(out=p.t[:, (kc % 4) * 128:(kc % 4 + 1) * 128],
                                                                                 in_=hb.t[:, kc * 128:(kc + 1) * 128], identity=ident),
                                   reads=[hb.tl, cst.tl], writes=[p.tl])
                            for half, eng in ((0, "act"), (1, "dve")):
                                src = pT[half].t[:].rearrange("p (k t) -> p k t", k=4)
                                dst = hTb.t[:, half * 4:(half + 1) * 4, tt * 128:(tt + 1) * 128]
                                if eng == "act":
                                    op("act", lambda e, src=src, dst=dst: e.copy(out=dst, in_=src), reads=[pT[half].tl], writes=[hTb.tl])
                                else:
                                    op("dve", lambda e, src=src, dst=dst: e.tensor_copy(out=dst, in_=src), reads=[pT[half].tl], writes=[hTb.tl])
                            for gi, (col, dst, fn) in enumerate(((2048, KTOK, None), (2560, VTOK, None), (3072, OTOK, AF.Sigmoid))):
                                p = ptm[(ti * 3 + gi) % 2]
                                for kc in range(KC):
                                    op("pe", lambda e, p=p, kc=kc, col=col, tt=tt: e.matmul(p.t[:], lhsT=hTb.t[:, kc, tt * 128:(tt + 1) * 128],
                                                                                           rhs=win.t[:, kc, col:col + 512],
                                                                                           start=(kc == 0), stop=(kc == KC - 1)),
                                       reads=[hTb.tl, win.sub[kc]], writes=[p.tl])
                                st = stt[nstt % 3]
                                nstt += 1
                                if fn is None:
                                    op("act", lambda e, st=st, p=p: e.copy(out=st.t[:], in_=p.t[:]), reads=[p.tl], writes=[st.tl])
                                else:
                                    op("act", lambda e, st=st, p=p, fn=fn: e.activation(out=st.t[:], in_=p.t[:], func=fn), reads=[p.tl], writes=[st.tl])
                                dma("sp", lambda e, st=st, dst=dst, r0=r0: e.dma_start(out=dst[r0:r0 + 128, :], in_=st.t[:]), reads=[st.tl], writes=[tD])
                            ti += 1
                        W = ntok if isctx else 64

                        def fmm(p, col, m=128):
                            for kc in range(KC):
                                op("pe", lambda e, kc=kc: e.matmul(p.t[0:m, 0:ntok], lhsT=win.t[:, kc, col:col + m], rhs=hTb.t[:, kc, 0:ntok],
                                                                  start=(kc == 0), stop=(kc == KC - 1)),
                                   reads=[hTb.tl, win.sub[kc]], writes=[p.tl])
                        for j in range(4):
                            fmm(pf[0], j * 128)
                            fmm(pf[1], 512 + j * 128)
                            fmm(pf[2], 1024 + j * 128)
                            op("act", lambda e: e.copy(out=xin_sb.t[:, 0:ntok], in_=pf[2].t[:, 0:ntok]), reads=[pf[2].tl], writes=[xin_sb.tl])
                            op("dve", lambda e: e.tensor_tensor(out=cx.t[:, 0:ntok], in0=pf[1].t[:, 0:ntok], in1=xin_sb.t[:, 0:ntok], op=ALU.mult),
                               reads=[pf[1].tl, xin_sb.tl], writes=[cx.tl])
                            op("dve", lambda e, j=j: e.tensor_scalar(out=acc.t[:, 0:ntok], in0=cx.t[:, 0:ntok], scalar1=cw.t[:, 1, j:j + 1],
                                                                    scalar2=cb.t[:, j:j + 1], op0=ALU.mult, op1=ALU.add),
                               reads=[cx.tl, cw.tl, cb.tl], writes=[acc.tl])
                            accv = acc.t[:, 0:ntok].rearrange("p (r w) -> p r w", w=W)
                            cxv = cx.t[:, 0:ntok].rearrange("p (r w) -> p r w", w=W)
                            op("dve", lambda e, j=j, accv=accv, cxv=cxv: e.scalar_tensor_tensor(
                                out=accv[:, :, 1:W], in0=cxv[:, :, 0:W - 1], scalar=cw.t[:, 0, j:j + 1], in1=accv[:, :, 1:W],
                                op0=ALU.mult, op1=ALU.add), reads=[cx.tl, cw.tl, acc.tl], writes=[acc.tl])
                            op("dve", lambda e, j=j, accv=accv, cxv=cxv: e.scalar_tensor_tensor(
                                out=accv[:, :, 0:W - 1], in0=cxv[:, :, 1:W], scalar=cw.t[:, 2, j:j + 1], in1=accv[:, :, 0:W - 1],
                                op0=ALU.mult, op1=ALU.add), reads=[cx.tl, cw.tl, acc.tl], writes=[acc.tl])
                            st = stf[nstf % 3]
                            nstf += 1
                            op("dve", lambda e, st=st: e.tensor_tensor(out=st.t[:, 0:ntok], in0=pf[0].t[:, 0:ntok], in1=acc.t[:, 0:ntok], op=ALU.mult),
                               reads=[pf[0].tl, acc.tl], writes=[st.tl])
                            dma("sp", lambda e, st=st, j=j: e.dma_start(out=YT[j, :, t0:t0 + ntok], in_=st.t[:, 0:ntok]), reads=[st.tl], writes=[tD])
                        for which, (col0, dst, scl) in enumerate(((1536, QT, DH ** -0.5), (2048, KT, 1.0))):
                            for h in range(NH):
                                p = pf[npf % 3]
                                npf += 1
                                fmm(p, col0 + h * 128)
                                st = stf[nstf % 3]
                                nstf += 1
                                op("act", lambda e, st=st, p=p, scl=scl: e.activation(out=st.t[:, 0:ntok], in_=p.t[:, 0:ntok], func=AF.Copy, scale=scl),
                                   reads=[p.tl], writes=[st.tl])
                                dma("sp", lambda e, st=st, dst=dst, h=h: e.dma_start(out=dst[h, :, t0:t0 + ntok], in_=st.t[:, 0:ntok]),
                                    reads=[st.tl], writes=[tD])
                        p = pf[npf % 3]
                        npf += 1
                        fmm(p, 3584, m=16)
                        op("act", lambda e, p=p: e.activation(out=GP.t[:, t0:t0 + ntok], in_=p.t[0:16, 0:ntok], func=AF.Identity, bias=gb.t[:, 0:1]),
                           reads=[p.tl, gb.tl], writes=[GP.tl])
                    fw.barrier()
                    fw.flush()

                if stop == "PA":
                    return nc
                if dbg and l == 0:
                    dma("sp", lambda e: e.dma_start(out=dbgout["dGP"][:, :], in_=GP.t[:]), reads=[GP.tl], writes=[tD])
                with ExitStack() as ph:
                    L = sb(ph, "L", [16, NT])
                    Sc = sb(ph, "Sc", [16, NT])
                    Gm = sb(ph, "Gm", [8, NT])
                    Bm = sb(ph, "Bm", [8, NT])
                    ones = sb(ph, "ones", [16, 512])
                    sm = sb(ph, "sm", [16, 16])
                    CM = sb(ph, "CM", [8, NCH])
                    Rd = [sb(ph, "Rd%d" % i, [8, NCH]) for i in range(2)]
                    Dd = [sb(ph, "Dd%d" % i, [8, NCH]) for i in range(2)]
                    R = sb(ph, "R", [8, NCH])
                    Dl = sb(ph, "Dl", [8, NCH])
                    zc = sb(ph, "zc", [8, 1])
                    pg = [ps(ph, "pg%d" % i, [8, 512]) for i in range(2)]
                    pb = [ps(ph, "pb%d" % i, [8, 512]) for i in range(2)]
                    pc = ps(ph, "pc", [8, 2])
                    pa = [ps(ph, "pa%d" % i, [128, 4 * NCH]) for i in range(2)]
                    pq = ps(ph, "pq", [128, 512])
                    Z = GP
                    op("act", lambda e: e.activation(out=L.t[:], in_=Z.t[:], func=AF.Exp, scale=-1.0), reads=[Z.tl], writes=[L.tl])
                    op("act", lambda e: e.activation(out=L.t[:], in_=L.t[:], func=AF.Ln, bias=1.0), reads=[L.tl], writes=[L.tl])
                    op("act", lambda e: e.mul(out=L.t[:], in_=L.t[:], mul=-1.0) if False else e.activation(out=L.t[:], in_=L.t[:], func=AF.Copy, scale=-1.0),
                       reads=[L.tl], writes=[L.tl])
                    op("dve", lambda e: e.memset(ones.t[:], 1.0), writes=[ones.tl])
                    op("dve", lambda e: e.memset(zc.t[:], 0.0), writes=[zc.tl])
                    for i, c0 in enumerate(range(0, NT, 512)):
                        n = min(512, NT - c0)
                        init = 0.0 if i == 0 else Sc.t[:, c0 - 1:c0]
                        op("dve", lambda e, c0=c0, n=n, init=init: e.tensor_tensor_scan(out=Sc.t[:, c0:c0 + n], data0=ones.t[:, 0:n], data1=L.t[:, c0:c0 + n],
                                                                                       initial=init, op0=ALU.mult, op1=ALU.add),
                           reads=[ones.tl, L.tl, Sc.tl], writes=[Sc.tl])
                    op("dve", lambda e: e.tensor_copy(out=sm.t[:, 0:1], in_=Sc.t[:, NCTX - 1:NCTX]), reads=[Sc.tl], writes=[sm.tl])
                    op("dve", lambda e: e.tensor_copy(out=sm.t[:, 1:2], in_=Sc.t[:, NT - 1:NT]), reads=[Sc.tl], writes=[sm.tl])
                    op("pe", lambda e: e.matmul(pc.t[:], lhsT=cst.t[0:16, 424:432], rhs=sm.t[:, 0:2], start=True, stop=True),
                       reads=[sm.tl, cst.tl], writes=[pc.tl])
                    op("dve", lambda e: e.tensor_copy(out=sm.t[0:8, 4:5], in_=pc.t[:, 0:1]), reads=[pc.tl], writes=[sm.tl])
                    op("dve", lambda e: e.tensor_tensor(out=sm.t[0:8, 5:6], in0=pc.t[:, 1:2], in1=sm.t[0:8, 4:5], op=ALU.add), reads=[pc.tl, sm.tl], writes=[sm.tl])
                    for bi, (t0, ntok, isctx) in enumerate(blocks):
                        cs = sm.t[0:8, 4:5] if isctx else sm.t[0:8, 5:6]
                        p1, p2 = pg[bi % 2], pb[bi % 2]
                        for i, (col, src) in enumerate(((384, Z), (392, Sc), (400, L))):
                            op("pe", lambda e, p1=p1, col=col, src=src, i=i: e.matmul(p1.t[:, 0:ntok], lhsT=cst.t[0:16, col:col + 8], rhs=src.t[:, t0:t0 + ntok],
                                                                                     start=(i == 0), stop=(i == 2)),
                               reads=[cst.tl, src.tl], writes=[p1.tl])
                        op("dve", lambda e, p1=p1, cs=cs: e.tensor_scalar(out=Gm.t[:, t0:t0 + ntok], in0=p1.t[:, 0:ntok], scalar1=cs, scalar2=None, op0=ALU.subtract),
                           reads=[p1.tl, sm.tl], writes=[Gm.tl])
                        for i, (col, src) in enumerate(((408, Sc), (416, L))):
                            op("pe", lambda e, p2=p2, col=col, src=src, i=i: e.matmul(p2.t[:, 0:ntok], lhsT=cst.t[0:16, col:col + 8], rhs=src.t[:, t0:t0 + ntok],
                                                                                     start=(i == 0), stop=(i == 1)),
                               reads=[cst.tl, src.tl], writes=[p2.tl])
                        op("dve", lambda e, p2=p2, cs=cs: e.tensor_scalar(out=Bm.t[:, t0:t0 + ntok], in0=p2.t[:, 0:ntok], scalar1=cs, scalar2=None, op0=ALU.add),
                           reads=[p2.tl, sm.tl], writes=[Bm.tl])
                    op("dve", lambda e: e.tensor_reduce(out=CM.t[:], in_=Gm.t[:].rearrange("p (c t) -> p c t", t=128), axis=AX.X, op=ALU.max),
                       reads=[Gm.tl], writes=[CM.tl])
                    for d, order in enumerate((order_f, order_b)):
                        prev = zc.t[:, 0:1]
                        for c in order:
                            op("dve", lambda e, d=d, c=c, prev=prev: e.tensor_tensor(out=Rd[d].t[:, c:c + 1], in0=prev, in1=CM.t[:, c:c + 1], op=ALU.max),
                               reads=[zc.tl, CM.tl, Rd[d].tl], writes=[Rd[d].tl])
                            op("dve", lambda e, d=d, c=c, prev=prev: e.tensor_tensor(out=Dd[d].t[:, c:c + 1], in0=prev, in1=Rd[d].t[:, c:c + 1], op=ALU.subtract),
                               reads=[zc.tl, Rd[d].tl], writes=[Dd[d].tl])
                            prev = Rd[d].t[:, c:c + 1]
                    for dstb, srcs in ((R, Rd), (Dl, Dd)):
                        op("dve", lambda e, dstb=dstb, srcs=srcs: e.tensor_scalar(out=dstb.t[:], in0=srcs[0].t[:], scalar1=cst.t[0:8, 432:433], scalar2=None, op0=ALU.mult),
                           reads=[srcs[0].tl, cst.tl], writes=[dstb.tl])
                        op("dve", lambda e, dstb=dstb, srcs=srcs: e.scalar_tensor_tensor(out=dstb.t[:], in0=srcs[1].t[:], scalar=cst.t[0:8, 433:434], in1=dstb.t[:],
                                                                                        op0=ALU.mult, op1=ALU.add),
                           reads=[srcs[1].tl, cst.tl, dstb.tl], writes=[dstb.tl])
                    op("act", lambda e: e.activation(out=Dl.t[:], in_=Dl.t[:], func=AF.Exp), reads=[Dl.tl], writes=[Dl.tl])
                    op("dve", lambda e: e.tensor_scalar(out=R.t[:], in0=R.t[:], scalar1=-1.0, scalar2=None, op0=ALU.mult), reads=[R.tl], writes=[R.tl])
                    for r in range(8):
                        p = pa[r // 4]
                        op("pe", lambda e, p=p, r=r: e.matmul(p.t[:, (r % 4) * NCH:(r % 4 + 1) * NCH], lhsT=cst.t[0:8, 440 + r * 128:440 + (r + 1) * 128],
                                                             rhs=Dl.t[:, :], start=True, stop=True),
                           reads=[cst.tl, Dl.tl], writes=[p.tl])
                    for i in range(2):
                        op("dve", lambda e, i=i: e.tensor_copy(out=ABt.t[:, i * 4:(i + 1) * 4, :], in_=pa[i].t[:].rearrange("p (r c) -> p r c", r=4)),
                           reads=[pa[i].tl], writes=[ABt.tl])
                    for c in range(NCH):
                        op("act", lambda e, c=c: e.activation(out=GP.t[0:8, c * 128:(c + 1) * 128], in_=Gm.t[:, c * 128:(c + 1) * 128], func=AF.Exp,
                                                             bias=R.t[:, c:c + 1]), reads=[Gm.tl, R.tl], writes=[GP.tl])
                        op("act", lambda e, c=c: e.activation(out=L.t[0:8, c * 128:(c + 1) * 128], in_=Bm.t[:, c * 128:(c + 1) * 128], func=AF.Exp,
                                                             bias=R.t[:, c:c + 1], scale=-1.0), reads=[Bm.tl, R.tl], writes=[L.tl])
                    for c in range(NCH):
                        cc = c % 32
                        op("pe", lambda e, c=c, cc=cc: e.transpose(out=pq.t[:, cc * 16:cc * 16 + 8], in_=GP.t[0:8, c * 128:(c + 1) * 128], identity=cst.t[0:8, 0:8]),
                           reads=[GP.tl, cst.tl], writes=[pq.tl])
                        op("pe", lambda e, c=c, cc=cc: e.transpose(out=pq.t[:, cc * 16 + 8:cc * 16 + 16], in_=L.t[0:8, c * 128:(c + 1) * 128], identity=cst.t[0:8, 0:8]),
                           reads=[L.tl, cst.tl], writes=[pq.tl])
                        if cc == 31 or c == NCH - 1:
                            c0 = c - cc
                            op("dve", lambda e, c0=c0, cc=cc: e.tensor_copy(out=TOKQ.t[:, c0:c0 + cc + 1, :],
                                                                           in_=pq.t[:, 0:(cc + 1) * 16].rearrange("p (c k) -> p c k", k=16)),
                               reads=[pq.tl], writes=[TOKQ.tl])
                    if dbg and l == 0:
                        dma("sp", lambda e: e.dma_start(out=dbgout["dTOKQ"][:, :], in_=TOKQ.t[:].rearrange("p c k -> p (c k)")), reads=[TOKQ.tl], writes=[tD])
                    fw.barrier()
                    fw.flush()

                if stop == "PG":
                    return nc
                with ExitStack() as ph:
                    qTb = [sb(ph, "qTb%d" % i, [128, NH, 128], BF16) for i in range(2)]
                    kTb = [sb(ph, "kTb%d" % i, [128, NH, 128], BF16) for i in range(2)]
                    ktk = [sb(ph, "ktk%d" % i, [128, 512], BF16) for i in range(2)]
                    vau = [sb(ph, "vau%d" % i, [128, NH, 129], BF16) for i in range(2)]
                    Chd = [[sb(ph, "Ch%d_%d" % (d_, h), [128, 129]) for h in range(NH)] for d_ in range(2)]
                    Csb = [sb(ph, "Csb%d" % i, [128, 129], BF16) for i in range(4)]
                    Sm = [sb(ph, "Sm%d" % i, [128, 128], BF16) for i in range(4)]
                    vt = [sb(ph, "vt%d" % i, [128, 129], BF16) for i in range(4)]
                    dn = [sb(ph, "dn%d" % i, [128, 2]) for i in range(4)]
                    hbuf = [sb(ph, "hbuf%d" % i, [128, 512]) for i in range(2)]
                    ps_s = [ps(ph, "ps_s%d" % i, [128, 128]) for i in range(2)]
                    ps_o = [ps(ph, "ps_o%d" % i, [128, 129]) for i in range(4)]
                    ps_c = [ps(ph, "ps_c%d" % i, [128, 129]) for i in range(2)]
                    for v in vau:
                        op("dve", lambda e, v=v: e.memset(v.t[:], 1.0), writes=[v.tl])
                    it = 0
                    n = 0
                    for d in range(2):
                        for h in range(NH):
                            op("dve", lambda e, h=h, d=d: e.memset(Chd[d][h].t[:], 0.0), writes=[Chd[d][h].tl])
                    ps_steps = [(d_, (order_f, order_b)[d_][st_]) for st_ in range(NCH) for d_ in range(2)]

                    def ps_load(i):
                        if i < len(ps_steps):
                            tt0 = ps_steps[i][1] * 128
                            q2, k2, kt2, v2 = qTb[i % 2], kTb[i % 2], ktk[i % 2], vau[i % 2]
                            dma("sp", lambda e: e.dma_start(out=q2.t[:], in_=QT[:, :, tt0:tt0 + 128].rearrange("h p t -> p h t")), writes=[q2.tl])
                            dma("sp", lambda e: e.dma_start(out=k2.t[:], in_=KT[:, :, tt0:tt0 + 128].rearrange("h p t -> p h t")), writes=[k2.tl])
                            dma("sp", lambda e: e.dma_start(out=kt2.t[:], in_=KTOK[tt0:tt0 + 128, :]), writes=[kt2.tl])
                            dma("sp", lambda e: e.dma_start(out=v2.t[:, :, 0:128], in_=VTOK[tt0:tt0 + 128, :].rearrange("t (h k) -> t h k", h=NH)),
                                writes=[v2.tl])
                    ps_load(0)
                    for step in range(NCH):
                        for d in range(2):
                            c = (order_f, order_b)[d][step]
                            Ch = Chd[d]
                            mask = cst.t[:, 128:256] if d == 0 else cst.t[:, 256:384]
                            t0 = c * 128
                            q_, k_, kt_, v_, hb_ = qTb[it % 2], kTb[it % 2], ktk[it % 2], vau[it % 2], hbuf[it % 2]
                            it += 1
                            ps_load(it)
                            skip_out = last and c < 2
                            for h in range(NH):
                                r = d * 4 + h
                                s_, o_, c_ = ps_s[n % 2], ps_o[n % 4], ps_c[n % 2]
                                cs_, sm_, vt_, dn_ = Csb[n % 4], Sm[n % 4], vt[n % 4], dn[n % 4]
                                n += 1
                                alpha = ABt.t[:, r, c:c + 1]
                                op("pool", lambda e, vt_=vt_, v_=v_, h=h, c=c, r=r: e.tensor_scalar(out=vt_.t[:], in0=v_.t[:, h, :], scalar1=TOKQ.t[:, c, r:r + 1],
                                                                                                scalar2=1.0, op0=ALU.mult, op1=ALU.mult),
                                   reads=[v_.tl, TOKQ.tl], writes=[vt_.tl])
                                if not skip_out:
                                    op("pe", lambda e, s_=s_, k_=k_, q_=q_, h=h: e.matmul(s_.t[:], lhsT=k_.t[:, h, :], rhs=q_.t[:, h, :], start=True, stop=True),
                                       reads=[k_.tl, q_.tl], writes=[s_.tl])
                                    op("dve", lambda e, sm_=sm_, s_=s_, mask=mask: e.tensor_tensor(out=sm_.t[:], in0=s_.t[:], in1=mask, op=ALU.mult),
                                       reads=[s_.tl, cst.tl], writes=[sm_.tl])
                                    op("act", lambda e, cs_=cs_, h=h, alpha=alpha: e.activation(out=cs_.t[:], in_=Ch[h].t[:], func=AF.Copy, scale=alpha),
                                       reads=[Ch[h].tl, ABt.tl], writes=[cs_.tl])
                                    op("pe", lambda e, o_=o_, q_=q_, cs_=cs_, h=h: e.matmul(o_.t[:], lhsT=q_.t[:, h, :], rhs=cs_.t[:], start=True, stop=False),
                                       reads=[q_.tl, cs_.tl], writes=[o_.tl])
                                    op("pe", lambda e, o_=o_, sm_=sm_, vt_=vt_: e.matmul(o_.t[:], lhsT=sm_.t[:], rhs=vt_.t[:], start=False, stop=True),
                                       reads=[sm_.tl, vt_.tl], writes=[o_.tl])
                                op("pe", lambda e, c_=c_, kt_=kt_, vt_=vt_, h=h: e.matmul(c_.t[:], lhsT=kt_.t[:, h * 128:(h + 1) * 128], rhs=vt_.t[:], start=True, stop=True),
                                   reads=[kt_.tl, vt_.tl], writes=[c_.tl])
                                op("dve", lambda e, c_=c_, h=h, alpha=alpha: e.scalar_tensor_tensor(out=Ch[h].t[:], in0=Ch[h].t[:], scalar=alpha, in1=c_.t[:],
                                                                                                    op0=ALU.mult, op1=ALU.add),
                                   reads=[Ch[h].tl, ABt.tl, c_.tl], writes=[Ch[h].tl])
                                if not skip_out:
                                    op("act", lambda e, dn_=dn_, o_=o_: e.activation(out=dn_.t[:, 0:1], in_=o_.t[:, 128:129], func=AF.Abs),
                                       reads=[o_.tl], writes=[dn_.tl])
                                    op("dve", lambda e, dn_=dn_, c=c, r=r: e.tensor_scalar(out=dn_.t[:, 0:1], in0=dn_.t[:, 0:1], scalar1=TOKQ.t[:, c, 8 + r:9 + r],
                                                                                          scalar2=None, op0=ALU.max),
                                       reads=[dn_.tl, TOKQ.tl], writes=[dn_.tl])
                                    op("dve", lambda e, dn_=dn_: e.reciprocal(out=dn_.t[:, 1:2], in_=dn_.t[:, 0:1]), reads=[dn_.tl], writes=[dn_.tl])
                                    op("act", lambda e, hb_=hb_, o_=o_, dn_=dn_, h=h: e.activation(out=hb_.t[:, h * 128:(h + 1) * 128], in_=o_.t[:, 0:128], func=AF.Copy,
                                                                                                 scale=dn_.t[:, 1:2]),
                                       reads=[o_.tl, dn_.tl], writes=[hb_.tl])
                            if not skip_out:
                                dma("sp", lambda e, hb_=hb_, d=d, t0=t0: e.dma_start(out=HFB[d, t0:t0 + 128, :], in_=hb_.t[:]), reads=[hb_.tl], writes=[tD])
                    fw.barrier()
                    fw.flush()

            if stop == "PS":
                return nc
            if dbg and l == 0:
                dma("sp", lambda e: e.dma_start(out=dbgout["dHFB"][:, :, :], in_=HFB[:, :, :]), reads=[tD], writes=[tD])

            with ExitStack() as lay:
                G2 = [sb(lay, "G2_%d" % s, [128, D]) for s in range(2)]
                for s in range(2):
                    load_bc(G2[s], s, 5)
                NSLT = (CAPL + 127) // 128 + 1
                IDX = sb(lay, "IDX", [128, NSLT, NE], U32)
                GV = sb(lay, "GV", [128, NSLT, NE])
                affs = lay.enter_context(ExitStack())
                AFF = sb(affs, "AFF", [16, NT])
                with ExitStack() as ph:
                    wo = sb(ph, "wo", [128, KC, D], BF16)
                    for kc in range(KC):
                        dma("pool", lambda e, kc=kc: e.dma_start(out=wo.t[:, kc, :], in_=w_out[l, kc * 128:(kc + 1) * 128, :]), writes=[wo.sub[kc]])
                    wr = sb(ph, "wr", [128, KC, NE])
                    dma("sp", lambda e: e.dma_start(out=wr.t[:], in_=w_router[l].rearrange("(k p) n -> p k n", p=128)), writes=[wr.tl])
                    G1 = [sb(ph, "G1_%d" % s, [128, D]) for s in range(2)]
                    A2 = [sb(ph, "A2_%d" % s, [128, D]) for s in range(2)]
                    S2 = [sb(ph, "S2_%d" % s, [128, D]) for s in range(2)]
                    for s in range(2):
                        load_bc(G1[s], s, 2)
                        load_bc(S2[s], s, 3)
                        load_bc(A2[s], s, 4)
                    ghb = sb(ph, "ghb", [128, 512])
                    dma("sp", lambda e: e.dma_start(out=ghb.t[:], in_=g_head[l].partition_broadcast(128)), writes=[ghb.tl])
                    hf = [sb(ph, "hf%d" % i, [128, 512]) for i in range(2)]
                    hbk = [sb(ph, "hbk%d" % i, [128, 512]) for i in range(2)]
                    so = [sb(ph, "so%d" % i, [128, 512], BF16) for i in range(2)]
                    yc = [sb(ph, "yc%d" % i, [128, 4, 128], BF16) for i in range(2)]
                    xt = [sb(ph, "xo%d" % i, [128, D]) for i in range(2)]
                    hs2 = [sb(ph, "hs%d" % i, [128, 512]) for i in range(2)]
                    junk2 = [sb(ph, "junk2_%d" % i, [128, D], BF16) for i in range(2)]
                    st4 = [sb(ph, "st4_%d" % i, [128, 12]) for i in range(2)]
                    ybf2 = [sb(ph, "ybf%d" % i, [128, 512], BF16) for i in range(2)]
                    yT = [sb(ph, "yT%d" % i, [128, 4, 128], BF16) for i in range(2)]
                    tmp2 = [sb(ph, "tmp2_%d" % i, [128, D]) for i in range(2)]
                    xn = [sb(ph, "xn%d" % i, [128, D]) for i in range(2)]
                    h22 = [sb(ph, "h2_%d" % i, [128, D]) for i in range(2)]
                    h2b = [sb(ph, "h2b%d" % i, [128, D], BF16) for i in range(2)]
                    h2T2 = [sb(ph, "h2T%d" % i, [128, KC, 128]) for i in range(2)]
                    pyt = ps(ph, "pyt", [128, 4, 128], BF16)
                    po = [ps(ph, "po%d" % i, [128, 512]) for i in range(2)]
                    pT = [ps(ph, "pT2_%d" % i, [128, 512]) for i in range(2)]
                    pr = [ps(ph, "pr%d" % i, [16, 128]) for i in range(2)]
                    tiles = [c for c in range(NCH) if not (last and c < 2)]
                    def po_load(i):
                        if i < len(tiles):
                            tt0 = tiles[i] * 128
                            bb = i % 2
                            dma("sp", lambda e: e.dma_start(out=hf[bb].t[:], in_=HFB[0, tt0:tt0 + 128, :]), writes=[hf[bb].tl])
                            dma("sp", lambda e: e.dma_start(out=hbk[bb].t[:], in_=HFB[1, tt0:tt0 + 128, :]), writes=[hbk[bb].tl])
                            dma("sp", lambda e: e.dma_start(out=so[bb].t[:], in_=OTOK[tt0:tt0 + 128, :]), writes=[so[bb].tl])
                            dma("sp", lambda e: e.dma_start(out=yc[bb].t[:], in_=YT[:, :, tt0:tt0 + 128].rearrange("j p t -> p j t")), writes=[yc[bb].tl])
                            dma("sp", lambda e: e.dma_start(out=xt[bb].t[:], in_=xrows(l, tt0)), reads=[tX], writes=[xt[bb].tl])
                    po_load(0)
                    for ti, c in enumerate(tiles):
                        t0 = c * 128
                        s = 1 if c < 2 else 0
                        b = ti % 2
                        po_load(ti + 1)
                        sq = st4[b]
                        hs, junk, ybf, tmp, h2, h2T, yTb = hs2[b], junk2[b], ybf2[b], tmp2[b], h22[b], h2T2[b], yT[b]
                        op("dve", lambda e, b=b: e.tensor_tensor(out=hs.t[:], in0=hf[b].t[:], in1=hbk[b].t[:], op=ALU.add), reads=[hf[b].tl, hbk[b].tl], writes=[hs.tl])
                        for h in range(NH):
                            op("act", lambda e, h=h, sq=sq: e.activation(out=junk.t[:, h * 128:(h + 1) * 128], in_=hs.t[:, h * 128:(h + 1) * 128], func=AF.Square,
                                                                        accum_out=sq.t[:, h:h + 1]), reads=[hs.tl], writes=[junk.tl, sq.tl])
                        op("act", lambda e, sq=sq: e.activation(out=sq.t[:, 4:8], in_=sq.t[:, 0:4], func=AF.Sqrt, scale=1.0 / DH, bias=EPS), reads=[sq.tl], writes=[sq.tl])
                        op("dve", lambda e, sq=sq: e.reciprocal(out=sq.t[:, 8:12], in_=sq.t[:, 4:8]), reads=[sq.tl], writes=[sq.tl])
                        for h in range(NH):
                            op("dve", lambda e, h=h, sq=sq: e.scalar_tensor_tensor(out=hs.t[:, h * 128:(h + 1) * 128], in0=hs.t[:, h * 128:(h + 1) * 128],
                                                                                  scalar=sq.t[:, 8 + h:9 + h], in1=ghb.t[:, h * 128:(h + 1) * 128],
                                                                                  op0=ALU.mult, op1=ALU.mult), reads=[hs.tl, sq.tl, ghb.tl], writes=[hs.tl])
                        op("dve", lambda e, b=b: e.tensor_tensor(out=ybf.t[:], in0=hs.t[:], in1=so[b].t[:], op=ALU.mult), reads=[hs.tl, so[b].tl], writes=[ybf.tl])
                        for h in range(NH):
                            op("pe", lambda e, h=h: e.transpose(out=pyt.t[:, h, :], in_=ybf.t[:, h * 128:(h + 1) * 128], identity=identb.t[:]),
                               reads=[ybf.tl, identb.tl], writes=[pyt.tl])
                        op("act", lambda e: e.copy(out=yTb.t[:], in_=pyt.t[:]), reads=[pyt.tl], writes=[yTb.tl])
                        for half in range(2):
                            p = po[half]
                            for kc in range(KC):
                                lhs = yc[b].t[:, kc, :] if kc < 4 else yTb.t[:, kc - 4, :]
                                op("pe", lambda e, p=p, lhs=lhs, kc=kc, half=half: e.matmul(p.t[:], lhsT=lhs, rhs=wo.t[:, kc, half * 512:(half + 1) * 512],
                                                                                          start=(kc == 0), stop=(kc == KC - 1)),
                                   reads=[yc[b].tl, yT[b].tl, wo.sub[kc]], writes=[p.tl])
                            op("dve", lambda e, p=p, half=half, s=s: e.tensor_tensor(out=tmp.t[:, half * 512:(half + 1) * 512], in0=p.t[:],
                                                                                    in1=G1[s].t[:, half * 512:(half + 1) * 512], op=ALU.mult),
                               reads=[p.tl, G1[s].tl], writes=[tmp.tl])
                        op("dve", lambda e, b=b: e.tensor_tensor(out=xn[b].t[:], in0=tmp.t[:], in1=xt[b].t[:], op=ALU.add), reads=[tmp.tl, xt[b].tl], writes=[xn[b].tl])
                        dma("sp", lambda e, b=b, t0=t0: e.dma_start(out=X[t0:t0 + 128, :], in_=xn[b].t[:]), reads=[xn[b].tl], writes=[tX])
                        op("act", lambda e, b=b, sq=sq: e.activation(out=junk.t[:], in_=xn[b].t[:], func=AF.Square, accum_out=sq.t[:, 0:1]),
                           reads=[xn[b].tl], writes=[junk.tl, sq.tl])
                        op("act", lambda e, sq=sq: e.activation(out=sq.t[:, 1:2], in_=sq.t[:, 0:1], func=AF.Sqrt, scale=1.0 / D, bias=EPS), reads=[sq.tl], writes=[sq.tl])
                        op("dve", lambda e, sq=sq: e.reciprocal(out=sq.t[:, 2:3], in_=sq.t[:, 1:2]), reads=[sq.tl], writes=[sq.tl])
                        op("dve", lambda e, b=b, sq=sq, s=s: e.scalar_tensor_tensor(out=tmp.t[:], in0=xn[b].t[:], scalar=sq.t[:, 2:3], in1=A2[s].t[:],
                                                                                   op0=ALU.mult, op1=ALU.mult), reads=[xn[b].tl, sq.tl, A2[s].tl], writes=[tmp.tl])
                        op("dve", lambda e, s=s: e.tensor_tensor(out=h2.t[:], in0=tmp.t[:], in1=S2[s].t[:], op=ALU.add), reads=[tmp.tl, S2[s].tl], writes=[h2.tl])
                        op("act", lambda e, b=b: e.copy(out=h2b[b].t[:], in_=h2.t[:]), reads=[h2.tl], writes=[h2b[b].tl])
                        dma("sp", lambda e, b=b, t0=t0: e.dma_start(out=H2[t0:t0 + 128, :], in_=h2b[b].t[:]), reads=[h2b[b].tl], writes=[tD])
                        for kc in range(KC):
                            p = pT[kc // 4]
                            op("pe", lambda e, p=p, kc=kc: e.transpose(out=p.t[:, (kc % 4) * 128:(kc % 4 + 1) * 128], in_=h2.t[:, kc * 128:(kc + 1) * 128], identity=ident),
                               reads=[h2.tl, cst.tl], writes=[p.tl])
                        op("act", lambda e: e.copy(out=h2T.t[:, 0:4, :], in_=pT[0].t[:].rearrange("p (k t) -> p k t", k=4)), reads=[pT[0].tl], writes=[h2T.tl])
                        op("dve", lambda e: e.tensor_copy(out=h2T.t[:, 4:8, :], in_=pT[1].t[:].rearrange("p (k t) -> p k t", k=4)), reads=[pT[1].tl], writes=[h2T.tl])
                        p = pr[ti % 2]
                        for kc in range(KC):
                            op("pe", lambda e, p=p, kc=kc: e.matmul(p.t[:], lhsT=wr.t[:, kc, :], rhs=h2T.t[:, kc, :], start=(kc == 0), stop=(kc == KC - 1)),
                               reads=[wr.tl, h2T.tl], writes=[p.tl])
                        op("act", lambda e, p=p, t0=t0: e.activation(out=AFF.t[:, t0:t0 + 128], in_=p.t[:], func=AF.Exp), reads=[p.tl], writes=[AFF.tl])
                    fw.barrier()
                    fw.flush()
                if dbg and l == 0:
                    dma("sp", lambda e: e.dma_start(out=dbgout["dX1"][:, :], in_=X[:, :]), reads=[tX], writes=[tD])

                if stop == "PO":
                    affs.close()
                    return nc
                groups = []
                if not last:
                    groups.append(("c", 0, CAPC))
                for g0 in range(0, CAPL, 512):
                    groups.append(("l", g0, min(512, CAPL - g0)))
                with ExitStack() as ph2:
                    with ExitStack() as ph:
                        TV = sb(ph, "TV", [16, CAPL + CAPC])
                        TI = sb(ph, "TI", [16, CAPL + CAPC], U32)
                        rs = sb(ph, "rs", [16, 512])
                        pn = [ps(ph, "pn%d" % i, [16, 512]) for i in range(2)]
                        pt = ps(ph, "ptk", [128, 32])
                        c0s = ([] if last else [(0, NCTX)]) + [(NCTX + i * 512, 512) for i in range(NLAT // 512)]
                        for i, (c0, n) in enumerate(c0s):
                            p = pn[i % 2]
                            op("pe", lambda e, p=p, c0=c0, n=n: e.matmul(p.t[:, 0:n], lhsT=cst.t[0:16, 1472:1488], rhs=AFF.t[:, c0:c0 + n], start=True, stop=True),
                               reads=[cst.tl, AFF.tl], writes=[p.tl])
                            op("dve", lambda e, p=p, n=n: e.reciprocal(out=rs.t[:, 0:n], in_=p.t[:, 0:n]), reads=[p.tl], writes=[rs.tl])
                            op("dve", lambda e, c0=c0, n=n: e.tensor_tensor(out=AFF.t[:, c0:c0 + n], in0=AFF.t[:, c0:c0 + n], in1=rs.t[:, 0:n], op=ALU.mult),
                               reads=[AFF.tl, rs.tl], writes=[AFF.tl])
                        if dbg and l == 0:
                            dma("sp", lambda e: e.dma_start(out=dbgout["dAFF"][:, :], in_=AFF.t[:]), reads=[AFF.tl], writes=[tD])
                        if stop == "TK1":
                            fw.barrier()
                            fw.flush()
                            raise _Stop(nc)
                        NQ = 4
                        QL = NLAT // NQ
                        K = CAPL
                        assert QL & (QL - 1) == 0 and K & (K - 1) == 0
                        AF2 = sb(ph, "AF2", [16 * NQ, QL])
                        TV2 = sb(ph, "TV2", [16 * NQ, K])
                        TI2 = sb(ph, "TI2", [16 * NQ, K], U32)
                        TV3 = sb(ph, "TV3", [16, NQ, K])
                        TI3 = sb(ph, "TI3", [16, NQ, K], U32)
                        qof = sb(ph, "qof", [16, NQ, K], U32)
                        VV = [sb(ph, "VV%d" % i, [16, 2 * K]) for i in range(2)]
                        II = [sb(ph, "II%d" % i, [16, 2 * K], U32) for i in range(2)]
                        msk = sb(ph, "msk", [16, 2 * K], U32)
                        tA = Tl()
                        dma("sp", lambda e: e.dma_start(out=AFD[:, :], in_=AFF.t[:, NCTX:NT]), reads=[AFF.tl], writes=[tA])
                        dma("sp", lambda e: e.dma_start(out=AF2.t[:], in_=AFD.rearrange("e (h n) -> (e h) n", h=NQ)), reads=[tA], writes=[AF2.tl])
                        for it in range(K // 8):
                            o = it * 8
                            op("dve", lambda e: e.max(out=TV2.t[:, o:o + 8], in_=AF2.t[:]), reads=[AF2.tl], writes=[TV2.tl])
                            op("dve", lambda e: e.max_index(out=TI2.t[:, o:o + 8], in_max=TV2.t[:, o:o + 8], in_values=AF2.t[:]), reads=[AF2.tl, TV2.tl], writes=[TI2.tl])
                            op("dve", lambda e: e.match_replace(out=AF2.t[:], in_to_replace=TV2.t[:, o:o + 8], in_values=AF2.t[:], imm_value=-1.0),
                               reads=[TV2.tl, AF2.tl], writes=[AF2.tl])
                        tB = Tl()
                        dma("sp", lambda e: e.dma_start(out=TVD[:, :], in_=TV2.t[:]), reads=[TV2.tl], writes=[tB])
                        dma("sp", lambda e: e.dma_start(out=TV3.t[:], in_=TVD.rearrange("(e h) n -> e h n", h=NQ)), reads=[tB], writes=[TV3.tl])
                        tC = Tl()
                        dma("sp", lambda e: e.dma_start(out=TID[:, :], in_=TI2.t[:]), reads=[TI2.tl], writes=[tC])
                        dma("sp", lambda e: e.dma_start(out=TI3.t[:], in_=TID.rearrange("(e h) n -> e h n", h=NQ)), reads=[tC], writes=[TI3.tl])
                        for q in range(NQ):
                            op("dve", lambda e: e.memset(qof.t[:, q, :], q * QL), writes=[qof.tl])
                        op("dve", lambda e: e.tensor_tensor(out=TI3.t[:], in0=TI3.t[:], in1=qof.t[:], op=ALU.bitwise_or), reads=[TI3.tl, qof.tl], writes=[TI3.tl])
                        v0 = VV[0].t[:].rearrange("p (j k) -> p j k", j=2)
                        i0 = II[0].t[:].rearrange("p (j k) -> p j k", j=2)
                        m0 = msk.t[:].rearrange("p (j k) -> p j k", j=2)
                        A_, B_ = TV3.t[:, 0:NQ:2, :], TV3.t[:, 1:NQ:2, ::-1]
                        IA_, IB_ = TI3.t[:, 0:NQ:2, :], TI3.t[:, 1:NQ:2, ::-1]
                        op("dve", lambda e: e.tensor_tensor(out=m0, in0=A_, in1=B_, op=ALU.is_ge), reads=[TV3.tl], writes=[msk.tl])
                        op("dve", lambda e: e.tensor_tensor(out=v0, in0=A_, in1=B_, op=ALU.max), reads=[TV3.tl], writes=[VV[0].tl])
                        op("dve", lambda e: e.tensor_copy(out=i0, in_=IB_), reads=[TI3.tl], writes=[II[0].tl])
                        op("dve", lambda e: e.copy_predicated(out=i0, mask=m0, data=IA_), reads=[msk.tl, TI3.tl, II[0].tl], writes=[II[0].tl])
                        cur = 0
                        dd = K // 2
                        while dd >= 1:
                            nxt = 1 - cur

                            def vw(buf):
                                return buf.t[:].rearrange("p (b t d) -> p b t d", t=2, d=dd)
                            sv, si_, dv, di, mv = vw(VV[cur]), vw(II[cur]), vw(VV[nxt]), vw(II[nxt]), vw(msk)
                            lo, hi, ilo, ihi = sv[:, :, 0, :], sv[:, :, 1, :], si_[:, :, 0, :], si_[:, :, 1, :]
                            mm = mv[:, :, 0, :]
                            op("dve", lambda e: e.tensor_tensor(out=mm, in0=lo, in1=hi, op=ALU.is_ge), reads=[VV[cur].tl], writes=[msk.tl])
                            op("dve", lambda e: e.tensor_tensor(out=dv[:, :, 0, :], in0=lo, in1=hi, op=ALU.max), reads=[VV[cur].tl], writes=[VV[nxt].tl])
                            op("dve", lambda e: e.tensor_tensor(out=dv[:, :, 1, :], in0=lo, in1=hi, op=ALU.min), reads=[VV[cur].tl], writes=[VV[nxt].tl])
                            op("dve", lambda e: e.tensor_copy(out=di[:, :, 0, :], in_=ihi), reads=[II[cur].tl], writes=[II[nxt].tl])
                            op("dve", lambda e: e.copy_predicated(out=di[:, :, 0, :], mask=mm, data=ilo), reads=[msk.tl, II[cur].tl, II[nxt].tl], writes=[II[nxt].tl])
                            op("dve", lambda e: e.tensor_copy(out=di[:, :, 1, :], in_=ilo), reads=[II[cur].tl], writes=[II[nxt].tl])
                            op("dve", lambda e: e.copy_predicated(out=di[:, :, 1, :], mask=mm, data=ihi), reads=[msk.tl, II[cur].tl, II[nxt].tl], writes=[II[nxt].tl])
                            cur = nxt
                            dd //= 2
                        sA, sB = VV[cur].t[:, 0:K], VV[cur].t[:, K:2 * K][:, ::-1]
                        iA, iB = II[cur].t[:, 0:K], II[cur].t[:, K:2 * K][:, ::-1]
                        op("dve", lambda e: e.tensor_tensor(out=msk.t[:, 0:K], in0=sA, in1=sB, op=ALU.is_ge), reads=[VV[cur].tl], writes=[msk.tl])
                        op("dve", lambda e: e.tensor_tensor(out=TV.t[:, 0:K], in0=sA, in1=sB, op=ALU.max), reads=[VV[cur].tl], writes=[TV.tl])
                        op("dve", lambda e: e.tensor_copy(out=TI.t[:, 0:K], in_=iB), reads=[II[cur].tl], writes=[TI.tl])
                        op("dve", lambda e: e.copy_predicated(out=TI.t[:, 0:K], mask=msk.t[:, 0:K], data=iA), reads=[msk.tl, II[cur].tl, TI.tl], writes=[TI.tl])
                        sets = ([] if last else [(0, NCTX, CAPL, CAPC)])
                        for (c0, n, o0, cap) in sets:
                            av = AFF.t[:, c0:c0 + n]
                            for it in range(cap // 8):
                                o = o0 + it * 8
                                op("dve", lambda e, av=av, o=o: e.max(out=TV.t[:, o:o + 8], in_=av), reads=[AFF.tl], writes=[TV.tl])
                                op("dve", lambda e, av=av, o=o: e.max_index(out=TI.t[:, o:o + 8], in_max=TV.t[:, o:o + 8], in_values=av), reads=[AFF.tl, TV.tl], writes=[TI.tl])
                                op("dve", lambda e, av=av, o=o: e.match_replace(out=av, in_to_replace=TV.t[:, o:o + 8], in_values=av, imm_value=-1.0),
                                   reads=[TV.tl, AFF.tl], writes=[AFF.tl])
                        if stop == "TK2":
                            fw.barrier()
                            fw.flush()
                            raise _Stop(nc)
                        tT = Tl()
                        dma("sp", lambda e: e.dma_start(out=TIS[:, :], in_=TI.t[:]), reads=[TI.tl], writes=[tT])
                        slot_tiles = [(j, j * 128, min(128, CAPL - j * 128)) for j in range((CAPL + 127) // 128)]
                        if not last:
                            slot_tiles.append((NSLT - 1, CAPL, CAPC))
                        for (j, o, n) in slot_tiles:
                            dma("sp", lambda e, j=j, o=o, n=n: e.dma_start(out=IDX.t[0:n, j, :], in_=TIS[:, o:o + n].rearrange("e p -> p e"),
                                                                          allow_slow_non_contiguous=True),
                                reads=[tT], writes=[IDX.tl])
                            op("pe", lambda e, o=o, n=n: e.transpose(out=pt.t[0:n, 16:32], in_=TV.t[:, o:o + n], identity=cst.t[0:16, 0:16]), reads=[TV.tl, cst.tl], writes=[pt.tl])
                            op("dve", lambda e, j=j, n=n: e.tensor_copy(out=GV.t[0:n, j, :], in_=pt.t[0:n, 16:32]), reads=[pt.tl], writes=[GV.tl])
                        fw.barrier()
                        fw.flush()
                    affs.close()
                    if stop == "TK":
                        return nc
                    with ExitStack() as ph:
                        wg = [sb(ph, "wg%d" % i, [128, KC, DEXP], BF16) for i in range(2)]
                        wu = [sb(ph, "wu%d" % i, [128, KC, DEXP], BF16) for i in range(2)]
                        wd = [sb(ph, "wd%d" % i, [128, FCH, D], BF16) for i in range(2)]
                        xg = [sb(ph, "xg%d" % i, [128, D], BF16) for i in range(8)]
                        xeT2 = [sb(ph, "xeT%d" % i, [128, KC, 512], BF16) for i in range(2)]
                        ngrp = 0
                        sa = [sb(ph, "sa%d" % i, [128, 512]) for i in range(2)]
                        actT = sb(ph, "actT", [128, FCH, 512], BF16)
                        ye = [sb(ph, "ye%d" % i, [128, D]) for i in range(2)]
                        pxt = [ps(ph, "pxt%d" % i, [128, KC, 128], BF16) for i in range(2)]
                        pa_ = [ps(ph, "pa_%d" % i, [128, 512]) for i in range(2)]
                        pu_ = [ps(ph, "pu_%d" % i, [128, 512]) for i in range(1)]
                        py = [ps(ph, "py%d" % i, [128, 512]) for i in range(2)]

                        def load_w(e_):
                            b = e_ % 2
                            for (wt, src, nk) in ((wg[b], w_gate_e, KC), (wu[b], w_up_e, KC), (wd[b], w_down_e, FCH)):
                                srcv = src[l, e_].rearrange("(k p) n -> p k n", p=128)
                                for (k0, k1) in ((0, nk // 2), (nk // 2, nk)):
                                    dma("pool", lambda e: e.dma_start(out=wt.t[:, k0:k1, :], in_=srcv[:, k0:k1, :]), writes=[wt.sub[k] for k in range(k0, k1)])
                        def tiles_of(kind, g0, gn):
                            if kind == "c":
                                return [(NSLT - 1, gn)]
                            return [((g0 + o) // 128, min(128, gn - o)) for o in range(0, gn, 128)]
                        msteps = [(e2, grp) for e2 in range(NE) for grp in groups]

                        def gathers(si):
                            if si >= len(msteps):
                                return
                            e2, (kind2, g02, gn2) = msteps[si]
                            for ji2, (j2, n2) in enumerate(tiles_of(kind2, g02, gn2)):
                                gg = xg[(si % 2) * 4 + ji2]
                                dma("pool", lambda e: e.indirect_dma_start(
                                    out=gg.t[0:n2, :], out_offset=None, in_=H2[:, :], element_offset=(0 if kind2 == "c" else NCTX * D),
                                    in_offset=bass.IndirectOffsetOnAxis(ap=IDX.t[0:n2, j2, e2:e2 + 1], axis=0)), reads=[IDX.tl, tD], writes=[gg.tl])
                        load_w(0)
                        gathers(0)
                        ng = 0
                        ny = 0
                        si = -1
                        for e_ in range(NE):
                            b = e_ % 2
                            for gi_, (kind, g0, gn) in enumerate(groups):
                                si += 1
                                if gi_ == 0 and e_ + 1 < NE:
                                    load_w(e_ + 1)
                                s = 1 if kind == "c" else 0
                                xeTg = xeT2[ngrp % 2]
                                ngrp += 1
                                tl_ = tiles_of(kind, g0, gn)
                                for ji, (j, n) in enumerate(tl_):
                                    g_ = xg[(si % 2) * 4 + ji]
                                    px = pxt[ng % 2]
                                    ng += 1
                                    for kc in range(KC):
                                        op("pe", lambda e, g_=g_, px=px, kc=kc, n=n: e.transpose(out=px.t[:, kc, 0:n], in_=g_.t[0:n, kc * 128:(kc + 1) * 128],
                                                                                                identity=identb.t[0:n, 0:n]),
                                           reads=[g_.tl, identb.tl], writes=[px.tl])
                                    op("act", lambda e, px=px, ji=ji, n=n: e.copy(out=xeTg.t[:, :, ji * 128:ji * 128 + n], in_=px.t[:, :, 0:n]), reads=[px.tl], writes=[xeTg.tl])
                                for fc in range(FCH):
                                    pa1 = pa_[fc % 2]
                                    pu1 = pu_[0]
                                    s1 = sa[fc % 2]
                                    for kc in range(KC):
                                        op("pe", lambda e, pa1=pa1, kc=kc, fc=fc: e.matmul(pa1.t[:, 0:gn], lhsT=wg[b].t[:, kc, fc * 128:(fc + 1) * 128], rhs=xeTg.t[:, kc, 0:gn],
                                                                                          start=(kc == 0), stop=(kc == KC - 1)), reads=[wg[b].sub[kc], xeTg.tl], writes=[pa1.tl])
                                    for kc in range(KC):
                                        op("pe", lambda e, pu1=pu1, kc=kc, fc=fc: e.matmul(pu1.t[:, 0:gn], lhsT=wu[b].t[:, kc, fc * 128:(fc + 1) * 128], rhs=xeTg.t[:, kc, 0:gn],
                                                                                          start=(kc == 0), stop=(kc == KC - 1)), reads=[wu[b].sub[kc], xeTg.tl], writes=[pu1.tl])
                                    op("act", lambda e, pa1=pa1, s1=s1: e.activation(out=s1.t[:, 0:gn], in_=pa1.t[:, 0:gn], func=AF.Silu), reads=[pa1.tl], writes=[s1.tl])
                                    op("dve", lambda e, pu1=pu1, s1=s1, fc=fc: e.tensor_tensor(out=actT.t[:, fc, 0:gn], in0=pu1.t[:, 0:gn], in1=s1.t[:, 0:gn], op=ALU.mult),
                                       reads=[pu1.tl, s1.tl], writes=[actT.tl])
                                gathers(si + 1)
                                for ji, (j, n) in enumerate(tl_):
                                    y_ = ye[ny % 2]
                                    ny += 1
                                    for half in range(2):
                                        p = py[half]
                                        for fc in range(FCH):
                                            op("pe", lambda e, p=p, fc=fc, ji=ji, n=n, half=half: e.matmul(p.t[0:n, :], lhsT=actT.t[:, fc, ji * 128:ji * 128 + n],
                                                                                                          rhs=wd[b].t[:, fc, half * 512:(half + 1) * 512],
                                                                                                          start=(fc == 0), stop=(fc == FCH - 1)),
                                               reads=[actT.tl, wd[b].sub[fc]], writes=[p.tl])
                                        op("dve", lambda e, p=p, y_=y_, j=j, n=n, half=half, s=s, e_=e_: e.scalar_tensor_tensor(
                                            out=y_.t[0:n, half * 512:(half + 1) * 512], in0=p.t[0:n, :], scalar=GV.t[0:n, j, e_:e_ + 1],
                                            in1=G2[s].t[0:n, half * 512:(half + 1) * 512], op0=ALU.mult, op1=ALU.mult),
                                           reads=[p.tl, GV.tl, G2[s].tl], writes=[y_.tl])
                                    dma("pool", lambda e, y_=y_, j=j, n=n, e_=e_: e.indirect_dma_start(
                                        element_offset=(0 if kind == "c" else NCTX * D), out=X[:, :], out_offset=bass.IndirectOffsetOnAxis(ap=IDX.t[0:n, j, e_:e_ + 1], axis=0),
                                        in_=y_.t[0:n, :], in_offset=None, compute_op=ALU.add), reads=[IDX.tl, y_.tl], writes=[tX])
                        fw.barrier()
                        fw.flush()

        with ExitStack() as ph:
            gf = sb(ph, "gf", [128, D])
            dma("sp", lambda e: e.dma_start(out=gf.t[:], in_=g_final.partition_broadcast(128)), writes=[gf.tl])
            xt = [sb(ph, "xf%d" % i, [128, D]) for i in range(2)]
            yo = [sb(ph, "yo%d" % i, [128, D]) for i in range(2)]
            junk = sb(ph, "junk3", [128, D], BF16)
            ss = [sb(ph, "ssf%d" % i, [128, 4]) for i in range(2)]
            tO = Tl()
            def pf_load(i):
                if i < NLAT // 128:
                    rr = NCTX + i * 128
                    dma("sp", lambda e: e.dma_start(out=xt[i % 2].t[:], in_=X[rr:rr + 128, :]), reads=[tX], writes=[xt[i % 2].tl])
            pf_load(0)
            for i in range(NLAT // 128):
                b = i % 2
                r0 = NCTX + i * 128
                sq = ss[b]
                pf_load(i + 1)
                op("act", lambda e, b=b, sq=sq: e.activation(out=junk.t[:], in_=xt[b].t[:], func=AF.Square, accum_out=sq.t[:, 0:1]), reads=[xt[b].tl], writes=[junk.tl, sq.tl])
                op("act", lambda e, sq=sq: e.activation(out=sq.t[:, 1:2], in_=sq.t[:, 0:1], func=AF.Sqrt, scale=1.0 / D, bias=EPS), reads=[sq.tl], writes=[sq.tl])
                op("dve", lambda e, sq=sq: e.reciprocal(out=sq.t[:, 2:3], in_=sq.t[:, 1:2]), reads=[sq.tl], writes=[sq.tl])
                op("dve", lambda e, b=b, sq=sq: e.scalar_tensor_tensor(out=yo[b].t[:], in0=xt[b].t[:], scalar=sq.t[:, 2:3], in1=gf.t[:], op0=ALU.mult, op1=ALU.mult),
                   reads=[xt[b].tl, sq.tl, gf.tl], writes=[yo[b].tl])
                dma("sp", lambda e, b=b, i=i: e.dma_start(out=out[i * 128:(i + 1) * 128, :], in_=yo[b].t[:]), reads=[yo[b].tl], writes=[tO])
            if dbg:
                dma("sp", lambda e: e.dma_start(out=dbgout["dX"][:, :], in_=X[:, :]), reads=[tX], writes=[tD])
                for nm in ("QT", "KT", "KTOK", "VTOK", "OTOK", "YT", "H2", "MODROW"):
                    dst, src = dbgout["d" + nm]
                    if len(src.shape) == 3:
                        dma("sp", lambda e, dst=dst, src=src: e.dma_start(out=dst[:, :, :], in_=src[:, :, :]), reads=[tD], writes=[tD])
                    else:
                        dma("sp", lambda e, dst=dst, src=src: e.dma_start(out=dst[:, :], in_=src[:, :]), reads=[tD], writes=[tD])
            fw.barrier()
            fw.flush()
        print("instructions (incl waits):", fw.ninst)
    return nc


def host_inputs(inputs, b, nlat=None):
    f = lambda a: np.ascontiguousarray(np.asarray(a, dtype=np.float32))
    L = inputs["w_mod"].shape[0]
    c = np.asarray(inputs["c"], np.float32)[b]
    cc = np.asarray(inputs["c_ctx"], np.float32)
    cT = np.stack([c.reshape(8, 128).T, cc.reshape(8, 128).T], axis=1)
    cw = np.asarray(inputs["conv_w"], np.float32).reshape(L, 3, 4, 128).transpose(0, 3, 1, 2)
    cb = np.asarray(inputs["conv_b"], np.float32).reshape(L, 4, 128).transpose(0, 2, 1)
    m = {
        "x": f(inputs["x"][b]), "ctx": f(inputs["ctx"][b]), "cT": f(cT),
        "w_mod": f(inputs["w_mod"]), "b_mod": f(inputs["b_mod"]),
        "g_norm1": f(inputs["g_norm1"]), "g_norm2": f(inputs["g_norm2"]),
        "w_in": f(inputs["w_in"]), "w_out": f(inputs["w_out"]),
        "conv_w": f(cw), "conv_b": f(cb),
        "gate_b": f(np.asarray(inputs["gate_b"], np.float32).reshape(L, 16, 1)),
        "g_head": f(np.asarray(inputs["g_head"], np.float32).reshape(L, 512)),
        "w_router": f(inputs["w_router"]),
        "w_gate_e": f(inputs["w_gate_e"]), "w_up_e": f(inputs["w_up_e"]), "w_down_e": f(inputs["w_down_e"]),
        "g_final": f(inputs["g_final"]), "consts": make_consts(),
    }
    return m


def kernel(**inputs):
    x = np.asarray(inputs["x"])
    nb, nlat, _ = x.shape
    depth = np.asarray(inputs["w_mod"]).shape[0]
    nc = build(nlat, depth)
    shared = host_inputs(inputs, 0)
    in_maps = []
    for b in range(nb):
        m = dict(shared)
        mb = host_inputs({**inputs, "w_mod": inputs["w_mod"]}, b) if False else None
        c = np.asarray(inputs["c"], np.float32)[b]
        cc = np.asarray(inputs["c_ctx"], np.float32)
        m["x"] = np.ascontiguousarray(np.asarray(inputs["x"][b], np.float32))
        m["ctx"] = np.ascontiguousarray(np.asarray(inputs["ctx"][b], np.float32))
        m["cT"] = np.ascontiguousarray(np.stack([c.reshape(8, 128).T, cc.reshape(8, 128).T], axis=1))
        in_maps.append(m)
    res = run_bass_kernel_spmd(nc, in_maps, core_ids=list(range(nb)))
    return np.stack([np.asarray(r["out"], np.float32) for r in res.results], axis=0)
```

```python
import numpy as np
from contextlib import ExitStack
import concourse.bass as bass
import concourse.mybir as mybir
from concourse.bass_utils import run_bass_kernel_spmd

F32 = mybir.dt.float32
BF16 = mybir.dt.bfloat16
U32 = mybir.dt.uint32
AF = mybir.ActivationFunctionType
ALU = mybir.AluOpType
AX = mybir.AxisListType

D = 1024
KC = 8
NCTX = 256
NH = 4
DH = 128
NE = 16
DEXP = 1408
FCH = 11
DPROJ = 3600
EPS = 1e-6
CW = 1488


class Tl:
    __slots__ = ("w", "r")

    def __init__(self):
        self.w = None
        self.r = {}


class _Rec:
    def __init__(self):
        self.call = None

    def __getattr__(self, name):
        def f(*a, **k):
            self.call = (name, a, k)
            return self
        return f


def _record(fn):
    r = _Rec()
    fn(r)
    name, a, k = r.call
    return lambda e: getattr(e, name)(*a, **k)


class FW:
    NDS = 6

    def __init__(self, nc, es):
        self.nc = nc
        self.engs = ("pe", "act", "dve", "pool", "sp")
        self.esem = {k: es.enter_context(nc.semaphore("s_" + k)) for k in self.engs}
        self.ecnt = {k: 0 for k in self.engs}
        self.dsem = {}
        self.dcnt = {}
        self.drr = {}
        for q in ("sp", "act", "pool"):
            self.drr[q] = 0
            for i in range(self.NDS):
                self.dsem[(q, i)] = es.enter_context(nc.semaphore("d_%s%d" % (q, i)))
                self.dcnt[(q, i)] = 0
        self.seen = {k: {} for k in self.engs}
        self.prog = {k: [] for k in self.engs}
        self.ninst = 0

    def _sem(self, key):
        return self.esem[key[1]] if key[0] == "e" else self.dsem[(key[1], key[2])]

    def _needs(self, eng, reads, writes, extra=()):
        need = {}

        def add(ev):
            if ev is None:
                return
            k, v = ev
            if k == ("e", "pe") and eng == "pe":
                return
            if need.get(k, 0) < v:
                need[k] = v
        for t in reads:
            add(t.w)
        for t in writes:
            add(t.w)
            for k, v in t.r.items():
                add((k, v))
        for ev in extra:
            add(ev)
        out = []
        seen = self.seen[eng]
        for k, v in need.items():
            if seen.get(k, 0) < v:
                seen[k] = v
                out.append((self._sem(k), v))
        return out

    def _commit(self, ev, reads, writes):
        k, v = ev
        for t in writes:
            t.w = ev
            t.r = {}
        for t in reads:
            if t.r.get(k, 0) < v:
                t.r[k] = v

    def op(self, eng, fn, reads=(), writes=()):
        fn = _record(fn)
        waits = self._needs(eng, reads, writes)
        self.ecnt[eng] += 1
        ev = (("e", eng), self.ecnt[eng])
        sem = self.esem[eng]

        def emit(e, waits=waits, fn=fn, sem=sem):
            for s, v in waits:
                e.wait_ge(s, v)
            fn(e).then_inc(sem, 1)
        self.prog[eng].append(emit)
        self._commit(ev, reads, writes)
        self.ninst += 1 + len(waits)
        return ev

    def dma(self, q, fn, reads=(), writes=()):
        fn = _record(fn)
        i = self.drr[q]
        self.drr[q] = (i + 1) % self.NDS
        key = ("d", q, i)
        prev = (key, self.dcnt[(q, i)]) if self.dcnt[(q, i)] else None
        waits = self._needs(q, reads, writes, extra=(prev,) if prev else ())
        self.dcnt[(q, i)] += 16
        ev = (key, self.dcnt[(q, i)])
        sem = self.dsem[(q, i)]

        def emit(e, waits=waits, fn=fn, sem=sem):
            for s, v in waits:
                e.wait_ge(s, v)
            fn(e).then_inc(sem, 16)
        self.prog[q].append(emit)
        self._commit(ev, reads, writes)
        self.ninst += 1 + len(waits)
        return ev

    def barrier(self):
        allev = [(("e", k), self.ecnt[k]) for k in self.engs if self.ecnt[k]]
        allev += [(("d", q, i), c) for (q, i), c in self.dcnt.items() if c]
        for eng in self.engs:
            waits = []
            seen = self.seen[eng]
            for k, v in allev:
                if seen.get(k, 0) < v:
                    seen[k] = v
                    waits.append((self._sem(k), v))

            def emit(e, waits=waits):
                for s, v in waits:
                    e.wait_ge(s, v)
            self.prog[eng].append(emit)

    def flush(self):
        nc = self.nc
        prog = self.prog
        self.prog = {k: [] for k in self.engs}
        with nc.Block() as block:
            @block.tensor
            def _(e):
                for f in prog["pe"]:
                    f(e)

            @block.scalar
            def _(e):
                for f in prog["act"]:
                    f(e)

            @block.vector
            def _(e):
                for f in prog["dve"]:
                    f(e)

            @block.gpsimd
            def _(e):
                for f in prog["pool"]:
                    f(e)

            @block.sync
            def _(e):
                for f in prog["sp"]:
                    f(e)


class B:
    def __init__(self, t):
        self.t = t
        self.tl = Tl()
        self.sub = [Tl() for _ in range(16)]


def make_consts():
    c = np.zeros((128, CW), np.float32)
    c[:, 0:128] = np.eye(128)
    s = np.arange(128)[:, None]
    t = np.arange(128)[None, :]
    c[:, 128:256] = (s <= t)
    c[:, 256:384] = (s >= t)
    MZG, MSG, MLG, MSB, MLB, MC = (c[0:16, 384 + 8 * i:392 + 8 * i] for i in range(6))
    for h in range(4):
        i_f, f_f, i_b, f_b = h, 4 + h, 8 + h, 12 + h
        MZG[i_f, h] = 1
        MZG[i_b, 4 + h] = 1
        MSG[f_f, h] = -1
        MSG[f_b, 4 + h] = 1
        MLG[f_b, 4 + h] = -1
        MSB[f_f, h] = 1
        MSB[f_b, 4 + h] = -1
        MLB[f_b, 4 + h] = 1
        MC[f_b, 4 + h] = 1
    c[0:4, 432] = 1
    c[4:8, 433] = 1
    for r in range(8):
        c[r, 440 + r * 128:440 + (r + 1) * 128] = 1
    c[:, 1464] = 1
    c[0:16, 1472:1488] = 1
    return c


class _Stop(Exception):
    pass


def build(NLAT, DEPTH, dbg=False, stop=None):
    try:
        return _build(NLAT, DEPTH, dbg, stop)
    except _Stop as ex:
        return ex.args[0]


def _build(NLAT, DEPTH, dbg=False, stop=None):
    NT = NCTX + NLAT
    NCH = NT // 128
    CAPL = 2 * NLAT // NE
    CAPC = 2 * NCTX // NE
    nc = bass.Bass("TRN2", target_bir_lowering=False)

    def din(name, shape, dt=F32):
        return nc.dram_tensor(name, shape, dt, kind="ExternalInput").ap()

    def dscr(name, shape, dt=F32):
        return nc.dram_tensor(name, shape, dt, kind="Internal").ap()

    x_in = din("x", [NLAT, D])
    ctx_in = din("ctx", [NCTX, D])
    cT_in = din("cT", [128, 2, 8])
    w_mod = din("w_mod", [DEPTH, D, 6 * D])
    b_mod = din("b_mod", [DEPTH, 6 * D])
    g_norm1 = din("g_norm1", [DEPTH, D])
    g_norm2 = din("g_norm2", [DEPTH, D])
    w_in = din("w_in", [DEPTH, D, DPROJ])
    w_out = din("w_out", [DEPTH, D, D])
    conv_w = din("conv_w", [DEPTH, 128, 3, 4])
    conv_b = din("conv_b", [DEPTH, 128, 4])
    gate_b = din("gate_b", [DEPTH, 16, 1])
    g_head = din("g_head", [DEPTH, 512])
    w_router = din("w_router", [DEPTH, D, NE])
    w_gate_e = din("w_gate_e", [DEPTH, NE, D, DEXP])
    w_up_e = din("w_up_e", [DEPTH, NE, D, DEXP])
    w_down_e = din("w_down_e", [DEPTH, NE, DEXP, D])
    g_final = din("g_final", [D])
    consts_in = din("consts", [128, CW])
    out = nc.dram_tensor("out", [NLAT, D], F32, kind="ExternalOutput").ap()

    X = dscr("Xs", [NT, D])
    MODROW = dscr("MODROW", [2, 6 * D])
    QT = dscr("QT", [NH, 128, NT], BF16)
    KT = dscr("KT", [NH, 128, NT], BF16)
    KTOK = dscr("KTOK", [NT, 512], BF16)
    VTOK = dscr("VTOK", [NT, 512], BF16)
    OTOK = dscr("OTOK", [NT, 512], BF16)
    YT = dscr("YT", [4, 128, NT], BF16)
    HFB = dscr("HFB", [2, NT, 512])
    H2 = dscr("H2", [NT, D], BF16)
    TIS = dscr("TIS", [16, CAPL + CAPC], U32)
    AFD = dscr("AFD", [16, NLAT])
    TVD = dscr("TVD", [128, CAPL])
    TID = dscr("TID", [128, CAPL], U32)
    dbgout = {}
    if dbg:
        dbgout["dX"] = nc.dram_tensor("dX", [NT, D], F32, kind="ExternalOutput").ap()
        dbgout["dHFB"] = nc.dram_tensor("dHFB", [2, NT, 512], F32, kind="ExternalOutput").ap()
        dbgout["dAFF"] = nc.dram_tensor("dAFF", [16, NT], F32, kind="ExternalOutput").ap()
        dbgout["dX1"] = nc.dram_tensor("dX1", [NT, D], F32, kind="ExternalOutput").ap()
        dbgout["dTOKQ"] = nc.dram_tensor("dTOKQ", [128, NCH * 16], F32, kind="ExternalOutput").ap()
        for nm, src in (("QT", QT), ("KT", KT), ("KTOK", KTOK), ("VTOK", VTOK), ("OTOK", OTOK), ("YT", YT), ("H2", H2), ("MODROW", MODROW)):
            dbgout["d" + nm] = (nc.dram_tensor("d" + nm, list(src.shape), src.dtype, kind="ExternalOutput").ap(), src)
        dbgout["dGP"] = nc.dram_tensor("dGP", [16, NT], F32, kind="ExternalOutput").ap()

    blocks = [(0, NCTX, True)] + [(NCTX + i * 512, 512, False) for i in range(NLAT // 512)]
    order_f = list(range(NCH))
    order_b = [1, 0] + list(range(NCH - 1, 1, -1))

    with ExitStack() as es:
        fw = FW(nc, es)
        op, dma = fw.op, fw.dma

        uid = [0]

        def sb(scope, name, shape, dt=F32):
            uid[0] += 1
            return B(scope.enter_context(nc.sbuf_tensor("%s_%d" % (name, uid[0]), shape, dt)))

        def ps(scope, name, shape, dt=F32):
            full = [128, 512] if dt == F32 else [128, 1024]
            uid[0] += 1
            t = scope.enter_context(nc.psum_tensor("%s_%d" % (name, uid[0]), full, dt))
            n = 1
            for v in shape[1:]:
                n *= v
            v = t[0:shape[0], 0:n]
            if len(shape) == 3:
                v = v.rearrange("p (a b) -> p a b", a=shape[1])
            return B(v)

        tX = Tl()
        tD = Tl()

        cst = sb(es, "cst", [128, CW])
        identb = sb(es, "identb", [128, 128], BF16)
        dma("sp", lambda e: e.dma_start(out=cst.t[:], in_=consts_in[:, :]), writes=[cst.tl])
        op("dve", lambda e: e.tensor_copy(out=identb.t[:], in_=cst.t[:, 0:128]), reads=[cst.tl], writes=[identb.tl])
        ident = cst.t[:, 0:128]

        def xrows(l_, r0_):
            if l_ > 0:
                return X[r0_:r0_ + 128, :]
            if r0_ < NCTX:
                return ctx_in[r0_:r0_ + 128, :]
            return x_in[r0_ - NCTX:r0_ - NCTX + 128, :]
        if DEPTH == 1:
            dma("sp", lambda e: e.dma_start(out=X[0:NCTX, :], in_=ctx_in[:, :]), writes=[tX])
        fw.barrier()
        fw.flush()

        for l in range(DEPTH):
            last = (l == DEPTH - 1)
            with ExitStack() as ph:
                scs = sb(ph, "scs", [128, 2, 8])
                wm = [sb(ph, "wm%d" % i, [128, 8, 512]) for i in range(2)]
                rows = [sb(ph, "mrow%d" % s, [1, 6 * D]) for s in range(2)]
                brow = sb(ph, "brow", [1, 6 * D])
                grow = sb(ph, "grow", [1, 2 * D])
                pm = [ps(ph, "pm%d" % s, [1, 512]) for s in range(2)]
                dma("sp", lambda e: e.dma_start(out=scs.t[:], in_=cT_in[:, :, :]), writes=[scs.tl])
                dma("sp", lambda e: e.dma_start(out=brow.t[:], in_=b_mod[l:l + 1, :]), writes=[brow.tl])
                dma("sp", lambda e: e.dma_start(out=grow.t[:, 0:D], in_=g_norm1[l:l + 1, :]), writes=[grow.tl])
                dma("sp", lambda e: e.dma_start(out=grow.t[:, D:2 * D], in_=g_norm2[l:l + 1, :]), writes=[grow.tl])
                op("act", lambda e: e.activation(out=scs.t[:], in_=scs.t[:], func=AF.Silu), reads=[scs.tl], writes=[scs.tl])
                wmv = w_mod[l].rearrange("(k p) n -> p k n", p=128)
                for blk in range(12):
                    w = wm[blk % 2]
                    dma("sp", lambda e, w=w, blk=blk: e.dma_start(out=w.t[:], in_=wmv[:, :, blk * 512:(blk + 1) * 512]), writes=[w.tl])
                    for s in range(2):
                        for kc in range(KC):
                            op("pe", lambda e, s=s, kc=kc, w=w: e.matmul(pm[s].t[:], lhsT=scs.t[:, s, kc:kc + 1], rhs=w.t[:, kc, :],
                                                                        start=(kc == 0), stop=(kc == KC - 1)),
                               reads=[scs.tl, w.tl], writes=[pm[s].tl])
                        op("dve", lambda e, s=s, blk=blk: e.tensor_tensor(out=rows[s].t[:, blk * 512:(blk + 1) * 512], in0=pm[s].t[:],
                                                                         in1=brow.t[:, blk * 512:(blk + 1) * 512], op=ALU.add),
                           reads=[pm[s].tl, brow.tl], writes=[rows[s].tl])
                for s in range(2):
                    for (o, g) in ((D, 0), (4 * D, D)):
                        op("dve", lambda e, s=s, o=o, g=g: e.scalar_tensor_tensor(out=rows[s].t[:, o:o + D], in0=rows[s].t[:, o:o + D], scalar=1.0,
                                                                                 in1=grow.t[:, g:g + D], op0=ALU.add, op1=ALU.mult),
                           reads=[rows[s].tl, grow.tl], writes=[rows[s].tl])
                    dma("sp", lambda e, s=s: e.dma_start(out=MODROW[s:s + 1, :], in_=rows[s].t[:]), reads=[rows[s].tl], writes=[tD])
                fw.barrier()
                fw.flush()

            if stop == "P0":
                return nc
            def load_bc(buf, s, j):
                dma("sp", lambda e: e.dma_start(out=buf.t[:], in_=MODROW[s, j * D:(j + 1) * D].partition_broadcast(128)), writes=[buf.tl])

            with ExitStack() as lay:
                GP = sb(lay, "GP", [16, NT])
                TOKQ = sb(lay, "TOKQ", [128, NCH, 16])
                ABt = sb(lay, "ABt", [128, 8, NCH])
                with ExitStack() as ph:
                    win = sb(ph, "win", [128, KC, DPROJ], BF16)
                    for kc in range(KC):
                        dma("pool", lambda e, kc=kc: e.dma_start(out=win.t[:, kc, :], in_=w_in[l, kc * 128:(kc + 1) * 128, :]), writes=[win.sub[kc]])
                    A1 = [sb(ph, "A1_%d" % s, [128, D]) for s in range(2)]
                    S1 = [sb(ph, "S1_%d" % s, [128, D]) for s in range(2)]
                    for s in range(2):
                        load_bc(S1[s], s, 0)
                        load_bc(A1[s], s, 1)
                    cw = sb(ph, "cw", [128, 3, 4])
                    cb = sb(ph, "cb", [128, 4])
                    gb = sb(ph, "gb", [16, 1])
                    dma("sp", lambda e: e.dma_start(out=cw.t[:], in_=conv_w[l]), writes=[cw.tl])
                    dma("sp", lambda e: e.dma_start(out=cb.t[:], in_=conv_b[l]), writes=[cb.tl])
                    dma("sp", lambda e: e.dma_start(out=gb.t[:], in_=gate_b[l]), writes=[gb.tl])
                    xt = [sb(ph, "xt%d" % i, [128, D]) for i in range(2)]
                    junk = sb(ph, "junk", [128, D], BF16)
                    tmp = sb(ph, "tmp", [128, D])
                    hn = [sb(ph, "hn%d" % i, [128, D]) for i in range(2)]
                    hT = [sb(ph, "hT%d" % i, [128, KC, 512], BF16) for i in range(2)]
                    ss = [sb(ph, "ss%d" % i, [128, 4]) for i in range(2)]
                    xin_sb = sb(ph, "xin_sb", [128, 512])
                    cx = sb(ph, "cx", [128, 512])
                    acc = sb(ph, "acc", [128, 512])
                    stf = [sb(ph, "stf%d" % i, [128, 512], BF16) for i in range(3)]
                    stt = [sb(ph, "stt%d" % i, [128, 512], BF16) for i in range(3)]
                    pT = [ps(ph, "pT%d" % i, [128, 512]) for i in range(2)]
                    pf = [ps(ph, "pf%d" % i, [128, 512]) for i in range(3)]
                    ptm = [ps(ph, "ptm%d" % i, [128, 512]) for i in range(2)]
                    ti = 0
                    pa_rows = [t0_ + tt_ * 128 for (t0_, ntok_, _c) in blocks for tt_ in range(ntok_ // 128)]

                    def pa_load(i):
                        if i < len(pa_rows):
                            xb_ = xt[i % 2]
                            rr = pa_rows[i]
                            dma("sp", lambda e: e.dma_start(out=xb_.t[:], in_=xrows(l, rr)), reads=[tX], writes=[xb_.tl])
                    pa_load(0)
                    nstf = 0
                    nstt = 0
                    npf = 0
                    for bi, (t0, ntok, isctx) in enumerate(blocks):
                        s = 1 if isctx else 0
                        hTb = hT[bi % 2]
                        for tt in range(ntok // 128):
                            r0 = t0 + tt * 128
                            xb = xt[ti % 2]
                            hb = hn[ti % 2]
                            sq = ss[ti % 2]
                            pa_load(ti + 1)
                            op("act", lambda e, xb=xb, sq=sq: e.activation(out=junk.t[:], in_=xb.t[:], func=AF.Square, accum_out=sq.t[:, 0:1]),
                               reads=[xb.tl], writes=[junk.tl, sq.tl])
                            op("act", lambda e, sq=sq: e.activation(out=sq.t[:, 1:2], in_=sq.t[:, 0:1], func=AF.Sqrt, scale=1.0 / D, bias=EPS),
                               reads=[sq.tl], writes=[sq.tl])
                            op("dve", lambda e, sq=sq: e.reciprocal(out=sq.t[:, 2:3], in_=sq.t[:, 1:2]), reads=[sq.tl], writes=[sq.tl])
                            op("dve", lambda e, xb=xb, sq=sq, s=s: e.scalar_tensor_tensor(out=tmp.t[:], in0=xb.t[:], scalar=sq.t[:, 2:3], in1=A1[s].t[:],
                                                                                         op0=ALU.mult, op1=ALU.mult),
                               reads=[xb.tl, sq.tl, A1[s].tl], writes=[tmp.tl])
                            op("dve", lambda e, hb=hb, s=s: e.tensor_tensor(out=hb.t[:], in0=tmp.t[:], in1=S1[s].t[:], op=ALU.add),
                               reads=[tmp.tl, S1[s].tl], writes=[hb.tl])
                            for kc in range(KC):
                                p = pT[kc // 4]
                                op("pe", lambda e, p=p, kc=kc, hb=hb: e.transpose(out=p.t[:, (kc % 4) * 128:(kc % 4 + 1) * 128],
                                                                                 in_=hb.t[:, kc * 128:(kc + 1) * 128], identity=ident),
                                   reads=[hb.tl, cst.tl], writes=[p.tl])
                            for half, eng in ((0, "act"), (1, "dve")):
                                src = pT[half].t[:].rearrange("p (k t) -> p k t", k=4)
                                dst = hTb.t[:, half * 4:(half + 1) * 4, tt * 128:(tt + 1) * 128]
                                if eng == "act":
                                    op("act", lambda e, src=src, dst=dst: e.copy(out=dst, in_=src), reads=[pT[half].tl], writes=[hTb.tl])
                                else:
                                    op("dve", lambda e, src=src, dst=dst: e.tensor_copy(out=dst, in_=src), reads=[pT[half].tl], writes=[hTb.tl])
                            for gi, (col, dst, fn) in enumerate(((2048, KTOK, None), (2560, VTOK, None), (3072, OTOK, AF.Sigmoid))):
                                p = ptm[(ti * 3 + gi) % 2]
                                for kc in range(KC):
                                    op("pe", lambda e, p=p, kc=kc, col=col, tt=tt: e.matmul(p.t[:], lhsT=hTb.t[:, kc, tt * 128:(tt + 1) * 128],
                                                                                           rhs=win.t[:, kc, col:col + 512],
                                                                                           start=(kc == 0), stop=(kc == KC - 1)),
                                       reads=[hTb.tl, win.sub[kc]], writes=[p.tl])
                                st = stt[nstt % 3]
                                nstt += 1
                                if fn is None:
                                    op("act", lambda e, st=st, p=p: e.copy(out=st.t[:], in_=p.t[:]), reads=[p.tl], writes=[st.tl])
                                else:
                                    op("act", lambda e, st=st, p=p, fn=fn: e.activation(out=st.t[:], in_=p.t[:], func=fn), reads=[p.tl], writes=[st.tl])
                                dma("sp", lambda e, st=st, dst=dst, r0=r0: e.dma_start(out=dst[r0:r0 + 128, :], in_=st.t[:]), reads=[st.tl], writes=[tD])
                            ti += 1
                        W = ntok if isctx else 64

                        def fmm(p, col, m=128):
                            for kc in range(KC):
                                op("pe", lambda e, kc=kc: e.matmul(p.t[0:m, 0:ntok], lhsT=win.t[:, kc, col:col + m], rhs=hTb.t[:, kc, 0:ntok],
                                                                  start=(kc == 0), stop=(kc == KC - 1)),
                                   reads=[hTb.tl, win.sub[kc]], writes=[p.tl])
                        for j in range(4):
                            fmm(pf[0], j * 128)
                            fmm(pf[1], 512 + j * 128)
                            fmm(pf[2], 1024 + j * 128)
                            op("act", lambda e: e.copy(out=xin_sb.t[:, 0:ntok], in_=pf[2].t[:, 0:ntok]), reads=[pf[2].tl], writes=[xin_sb.tl])
                            op("dve", lambda e: e.tensor_tensor(out=cx.t[:, 0:ntok], in0=pf[1].t[:, 0:ntok], in1=xin_sb.t[:, 0:ntok], op=ALU.mult),
                               reads=[pf[1].tl, xin_sb.tl], writes=[cx.tl])
                            op("dve", lambda e, j=j: e.tensor_scalar(out=acc.t[:, 0:ntok], in0=cx.t[:, 0:ntok], scalar1=cw.t[:, 1, j:j + 1],
                                                                    scalar2=cb.t[:, j:j + 1], op0=ALU.mult, op1=ALU.add),
                               reads=[cx.tl, cw.tl, cb.tl], writes=[acc.tl])
                            accv = acc.t[:, 0:ntok].rearrange("p (r w) -> p r w", w=W)
                            cxv = cx.t[:, 0:ntok].rearrange("p (r w) -> p r w", w=W)
                            op("dve", lambda e, j=j, accv=accv, cxv=cxv: e.scalar_tensor_tensor(
                                out=accv[:, :, 1:W], in0=cxv[:, :, 0:W - 1], scalar=cw.t[:, 0, j:j + 1], in1=accv[:, :, 1:W],
                                op0=ALU.mult, op1=ALU.add), reads=[cx.tl, cw.tl, acc.tl], writes=[acc.tl])
                            op("dve", lambda e, j=j, accv=accv, cxv=cxv: e.scalar_tensor_tensor(
                                out=accv[:, :, 0:W - 1], in0=cxv[:, :, 1:W], scalar=cw.t[:, 2, j:j + 1], in1=accv[:, :, 0:W - 1],
                                op0=ALU.mult, op1=ALU.add), reads=[cx.tl, cw.tl, acc.tl], writes=[acc.tl])
                            st = stf[nstf % 3]
                            nstf += 1
                            op("dve", lambda e, st=st: e.tensor_tensor(out=st.t[:, 0:ntok], in0=pf[0].t[:, 0:ntok], in1=acc.t[:, 0:ntok], op=ALU.mult),
                               reads=[pf[0].tl, acc.tl], writes=[st.tl])
                            dma("sp", lambda e, st=st, j=j: e.dma_start(out=YT[j, :, t0:t0 + ntok], in_=st.t[:, 0:ntok]), reads=[st.tl], writes=[tD])
                        for which, (col0, dst, scl) in enumerate(((1536, QT, DH ** -0.5), (2048, KT, 1.0))):
                            for h in range(NH):
                                p = pf[npf % 3]
                                npf += 1
                                fmm(p, col0 + h * 128)
                                st = stf[nstf % 3]
                                nstf += 1
                                op("act", lambda e, st=st, p=p, scl=scl: e.activation(out=st.t[:, 0:ntok], in_=p.t[:, 0:ntok], func=AF.Copy, scale=scl),
                                   reads=[p.tl], writes=[st.tl])
                                dma("sp", lambda e, st=st, dst=dst, h=h: e.dma_start(out=dst[h, :, t0:t0 + ntok], in_=st.t[:, 0:ntok]),
                                    reads=[st.tl], writes=[tD])
                        p = pf[npf % 3]
                        npf += 1
                        fmm(p, 3584, m=16)
                        op("act", lambda e, p=p: e.activation(out=GP.t[:, t0:t0 + ntok], in_=p.t[0:16, 0:ntok], func=AF.Identity, bias=gb.t[:, 0:1]),
                           reads=[p.tl, gb.tl], writes=[GP.tl])
                    fw.barrier()
                    fw.flush()

                if stop == "PA":
                    return nc
                if dbg and l == 0:
                    dma("sp", lambda e: e.dma_start(out=dbgout["dGP"][:, :], in_=GP.t[:]), reads=[GP.tl], writes=[tD])
                with ExitStack() as ph:
                    L = sb(ph, "L", [16, NT])
                    Sc = sb(ph, "Sc", [16, NT])
                    Gm = sb(ph, "Gm", [8, NT])
                    Bm = sb(ph, "Bm", [8, NT])
                    ones = sb(ph, "ones", [16, 512])
                    sm = sb(ph, "sm", [16, 16])
                    CM = sb(ph, "CM", [8, NCH])
                    Rd = [sb(ph, "Rd%d" % i, [8, NCH]) for i in range(2)]
                    Dd = [sb(ph, "Dd%d" % i, [8, NCH]) for i in range(2)]
                    R = sb(ph, "R", [8, NCH])
                    Dl = sb(ph, "Dl", [8, NCH])
                    zc = sb(ph, "zc", [8, 1])
                    pg = [ps(ph, "pg%d" % i, [8, 512]) for i in range(2)]
                    pb = [ps(ph, "pb%d" % i, [8, 512]) for i in range(2)]
                    pc = ps(ph, "pc", [8, 2])
                    pa = [ps(ph, "pa%d" % i, [128, 4 * NCH]) for i in range(2)]
                    pq = ps(ph, "pq", [128, 512])
                    Z = GP
                    op("act", lambda e: e.activation(out=L.t[:], in_=Z.t[:], func=AF.Exp, scale=-1.0), reads=[Z.tl], writes=[L.tl])
                    op("act", lambda e: e.activation(out=L.t[:], in_=L.t[:], func=AF.Ln, bias=1.0), reads=[L.tl], writes=[L.tl])
                    op("act", lambda e: e.mul(out=L.t[:], in_=L.t[:], mul=-1.0) if False else e.activation(out=L.t[:], in_=L.t[:], func=AF.Copy, scale=-1.0),
                       reads=[L.tl], writes=[L.tl])
                    op("dve", lambda e: e.memset(ones.t[:], 1.0), writes=[ones.tl])
                    op("dve", lambda e: e.memset(zc.t[:], 0.0), writes=[zc.tl])
                    for i, c0 in enumerate(range(0, NT, 512)):
                        n = min(512, NT - c0)
                        init = 0.0 if i == 0 else Sc.t[:, c0 - 1:c0]
                        op("dve", lambda e, c0=c0, n=n, init=init: e.tensor_tensor_scan(out=Sc.t[:, c0:c0 + n], data0=ones.t[:, 0:n], data1=L.t[:, c0:c0 + n],
                                                                                       initial=init, op0=ALU.mult, op1=ALU.add),
                           reads=[ones.tl, L.tl, Sc.tl], writes=[Sc.tl])
                    op("dve", lambda e: e.tensor_copy(out=sm.t[:, 0:1], in_=Sc.t[:, NCTX - 1:NCTX]), reads=[Sc.tl], writes=[sm.tl])
                    op("dve", lambda e: e.tensor_copy(out=sm.t[:, 1:2], in_=Sc.t[:, NT - 1:NT]), reads=[Sc.tl], writes=[sm.tl])
                    op("pe", lambda e: e.matmul(pc.t[:], lhsT=cst.t[0:16, 424:432], rhs=sm.t[:, 0:2], start=True, stop=True),
                       reads=[sm.tl, cst.tl], writes=[pc.tl])
                    op("dve", lambda e: e.tensor_copy(out=sm.t[0:8, 4:5], in_=pc.t[:, 0:1]), reads=[pc.tl], writes=[sm.tl])
                    op("dve", lambda e: e.tensor_tensor(out=sm.t[0:8, 5:6], in0=pc.t[:, 1:2], in1=sm.t[0:8, 4:5], op=ALU.add), reads=[pc.tl, sm.tl], writes=[sm.tl])
                    for bi, (t0, ntok, isctx) in enumerate(blocks):
                        cs = sm.t[0:8, 4:5] if isctx else sm.t[0:8, 5:6]
                        p1, p2 = pg[bi % 2], pb[bi % 2]
                        for i, (col, src) in enumerate(((384, Z), (392, Sc), (400, L))):
                            op("pe", lambda e, p1=p1, col=col, src=src, i=i: e.matmul(p1.t[:, 0:ntok], lhsT=cst.t[0:16, col:col + 8], rhs=src.t[:, t0:t0 + ntok],
                                                                                     start=(i == 0), stop=(i == 2)),
                               reads=[cst.tl, src.tl], writes=[p1.tl])
                        op("dve", lambda e, p1=p1, cs=cs: e.tensor_scalar(out=Gm.t[:, t0:t0 + ntok], in0=p1.t[:, 0:ntok], scalar1=cs, scalar2=None, op0=ALU.subtract),
                           reads=[p1.tl, sm.tl], writes=[Gm.tl])
                        for i, (col, src) in enumerate(((408, Sc), (416, L))):
                            op("pe", lambda e, p2=p2, col=col, src=src, i=i: e.matmul(p2.t[:, 0:ntok], lhsT=cst.t[0:16, col:col + 8], rhs=src.t[:, t0:t0 + ntok],
                                                                                     start=(i == 0), stop=(i == 1)),
                               reads=[cst.tl, src.tl], writes=[p2.tl])
                        op("dve", lambda e, p2=p2, cs=cs: e.tensor_scalar(out=Bm.t[:, t0:t0 + ntok], in0=p2.t[:, 0:ntok], scalar1=cs, scalar2=None, op0=ALU.add),
                           reads=[p2.tl, sm.tl], writes=[Bm.tl])
                    op("dve", lambda e: e.tensor_reduce(out=CM.t[:], in_=Gm.t[:].rearrange("p (c t) -> p c t", t=128), axis=AX.X, op=ALU.max),
                       reads=[Gm.tl], writes=[CM.tl])
                    for d, order in enumerate((order_f, order_b)):
                        prev = zc.t[:, 0:1]
                        for c in order:
                            op("dve", lambda e, d=d, c=c, prev=prev: e.tensor_tensor(out=Rd[d].t[:, c:c + 1], in0=prev, in1=CM.t[:, c:c + 1], op=ALU.max),
                               reads=[zc.tl, CM.tl, Rd[d].tl], writes=[Rd[d].tl])
                            op("dve", lambda e, d=d, c=c, prev=prev: e.tensor_tensor(out=Dd[d].t[:, c:c + 1], in0=prev, in1=Rd[d].t[:, c:c + 1], op=ALU.subtract),
                               reads=[zc.tl, Rd[d].tl], writes=[Dd[d].tl])
                            prev = Rd[d].t[:, c:c + 1]
                    for dstb, srcs in ((R, Rd), (Dl, Dd)):
                        op("dve", lambda e, dstb=dstb, srcs=srcs: e.tensor_scalar(out=dstb.t[:], in0=srcs[0].t[:], scalar1=cst.t[0:8, 432:433], scalar2=None, op0=ALU.mult),
                           reads=[srcs[0].tl, cst.tl], writes=[dstb.tl])
                        op("dve", lambda e, dstb=dstb, srcs=srcs: e.scalar_tensor_tensor(out=dstb.t[:], in0=srcs[1].t[:], scalar=cst.t[0:8, 433:434], in1=dstb.t[:],
                                                                                        op0=ALU.mult, op1=ALU.add),
                           reads=[srcs[1].tl, cst.tl, dstb.tl], writes=[dstb.tl])
                    op("act", lambda e: e.activation(out=Dl.t[:], in_=Dl.t[:], func=AF.Exp), reads=[Dl.tl], writes=[Dl.tl])
                    op("dve", lambda e: e.tensor_scalar(out=R.t[:], in0=R.t[:], scalar1=-1.0, scalar2=None, op0=ALU.mult), reads=[R.tl], writes=[R.tl])
                    for r in range(8):
                        p = pa[r // 4]
                        op("pe", lambda e, p=p, r=r: e.matmul(p.t[:, (r % 4) * NCH:(r % 4 + 1) * NCH], lhsT=cst.t[0:8, 440 + r * 128:440 + (r + 1) * 128],
                                                             rhs=Dl.t[:, :], start=True, stop=True),
                           reads=[cst.tl, Dl.tl], writes=[p.tl])
                    for i in range(2):
                        op("dve", lambda e, i=i: e.tensor_copy(out=ABt.t[:, i * 4:(i + 1) * 4, :], in_=pa[i].t[:].rearrange("p (r c) -> p r c", r=4)),
                           reads=[pa[i].tl], writes=[ABt.tl])
                    for c in range(NCH):
                        op("act", lambda e, c=c: e.activation(out=GP.t[0:8, c * 128:(c + 1) * 128], in_=Gm.t[:, c * 128:(c + 1) * 128], func=AF.Exp,
                                                             bias=R.t[:, c:c + 1]), reads=[Gm.tl, R.tl], writes=[GP.tl])
                        op("act", lambda e, c=c: e.activation(out=L.t[0:8, c * 128:(c + 1) * 128], in_=Bm.t[:, c * 128:(c + 1) * 128], func=AF.Exp,
                                                             bias=R.t[:, c:c + 1], scale=-1.0), reads=[Bm.tl, R.tl], writes=[L.tl])
                    for c in range(NCH):
                        cc = c % 32
                        op("pe", lambda e, c=c, cc=cc: e.transpose(out=pq.t[:, cc * 16:cc * 16 + 8], in_=GP.t[0:8, c * 128:(c + 1) * 128], identity=cst.t[0:8, 0:8]),
                           reads=[GP.tl, cst.tl], writes=[pq.tl])
                        op("pe", lambda e, c=c, cc=cc: e.transpose(out=pq.t[:, cc * 16 + 8:cc * 16 + 16], in_=L.t[0:8, c * 128:(c + 1) * 128], identity=cst.t[0:8, 0:8]),
                           reads=[L.tl, cst.tl], writes=[pq.tl])
                        if cc == 31 or c == NCH - 1:
                            c0 = c - cc
                            op("dve", lambda e, c0=c0, cc=cc: e.tensor_copy(out=TOKQ.t[:, c0:c0 + cc + 1, :],
                                                                           in_=pq.t[:, 0:(cc + 1) * 16].rearrange("p (c k) -> p c k", k=16)),
                               reads=[pq.tl], writes=[TOKQ.tl])
                    if dbg and l == 0:
                        dma("sp", lambda e: e.dma_start(out=dbgout["dTOKQ"][:, :], in_=TOKQ.t[:].rearrange("p c k -> p (c k)")), reads=[TOKQ.tl], writes=[tD])
                    fw.barrier()
                    fw.flush()

                if stop == "PG":
                    return nc
                with ExitStack() as ph:
                    qTb = [sb(ph, "qTb%d" % i, [128, NH, 128], BF16) for i in range(2)]
                    kTb = [sb(ph, "kTb%d" % i, [128, NH, 128], BF16) for i in range(2)]
                    ktk = [sb(ph, "ktk%d" % i, [128, 512], BF16) for i in range(2)]
                    vau = [sb(ph, "vau%d" % i, [128, NH, 129], BF16) for i in range(2)]
                    Chd = [[sb(ph, "Ch%d_%d" % (d_, h), [128, 129]) for h in range(NH)] for d_ in range(2)]
                    Csb = [sb(ph, "Csb%d" % i, [128, 129], BF16) for i in range(4)]
                    Sm = [sb(ph, "Sm%d" % i, [128, 128], BF16) for i in range(4)]
                    vt = [sb(ph, "vt%d" % i, [128, 129], BF16) for i in range(4)]
                    dn = [sb(ph, "dn%d" % i, [128, 2]) for i in range(4)]
                    hbuf = [sb(ph, "hbuf%d" % i, [128, 512]) for i in range(2)]
                    ps_s = [ps(ph, "ps_s%d" % i, [128, 128]) for i in range(2)]
                    ps_o = [ps(ph, "ps_o%d" % i, [128, 129]) for i in range(4)]
                    ps_c = [ps(ph, "ps_c%d" % i, [128, 129]) for i in range(2)]
                    for v in vau:
                        op("dve", lambda e, v=v: e.memset(v.t[:], 1.0), writes=[v.tl])
                    it = 0
                    n = 0
                    for d in range(2):
                        for h in range(NH):
                            op("dve", lambda e, h=h, d=d: e.memset(Chd[d][h].t[:], 0.0), writes=[Chd[d][h].tl])
                    ps_steps = [(d_, (order_f, order_b)[d_][st_]) for st_ in range(NCH) for d_ in range(2)]

                    def ps_load(i):
                        if i < len(ps_steps):
                            tt0 = ps_steps[i][1] * 128
                            q2, k2, kt2, v2 = qTb[i % 2], kTb[i % 2], ktk[i % 2], vau[i % 2]
                            dma("sp", lambda e: e.dma_start(out=q2.t[:], in_=QT[:, :, tt0:tt0 + 128].rearrange("h p t -> p h t")), writes=[q2.tl])
                            dma("sp", lambda e: e.dma_start(out=k2.t[:], in_=KT[:, :, tt0:tt0 + 128].rearrange("h p t -> p h t")), writes=[k2.tl])
                            dma("sp", lambda e: e.dma_start(out=kt2.t[:], in_=KTOK[tt0:tt0 + 128, :]), writes=[kt2.tl])
                            dma("sp", lambda e: e.dma_start(out=v2.t[:, :, 0:128], in_=VTOK[tt0:tt0 + 128, :].rearrange("t (h k) -> t h k", h=NH)),
                                writes=[v2.tl])
                    ps_load(0)
                    for step in range(NCH):
                        for d in range(2):
                            c = (order_f, order_b)[d][step]
                            Ch = Chd[d]
                            mask = cst.t[:, 128:256] if d == 0 else cst.t[:, 256:384]
                            t0 = c * 128
                            q_, k_, kt_, v_, hb_ = qTb[it % 2], kTb[it % 2], ktk[it % 2], vau[it % 2], hbuf[it % 2]
                            it += 1
                            ps_load(it)
                            skip_out = last and c < 2
                            for h in range(NH):
                                r = d * 4 + h
                                s_, o_, c_ = ps_s[n % 2], ps_o[n % 4], ps_c[n % 2]
                                cs_, sm_, vt_, dn_ = Csb[n % 4], Sm[n % 4], vt[n % 4], dn[n % 4]
                                n += 1
                                alpha = ABt.t[:, r, c:c + 1]
                                op("pool", lambda e, vt_=vt_, v_=v_, h=h, c=c, r=r: e.tensor_scalar(out=vt_.t[:], in0=v_.t[:, h, :], scalar1=TOKQ.t[:, c, r:r + 1],
                                                                                                scalar2=1.0, op0=ALU.mult, op1=ALU.mult),
                                   reads=[v_.tl, TOKQ.tl], writes=[vt_.tl])
                                if not skip_out:
                                    op("pe", lambda e, s_=s_, k_=k_, q_=q_, h=h: e.matmul(s_.t[:], lhsT=k_.t[:, h, :], rhs=q_.t[:, h, :], start=True, stop=True),
                                       reads=[k_.tl, q_.tl], writes=[s_.tl])
                                    op("dve", lambda e, sm_=sm_, s_=s_, mask=mask: e.tensor_tensor(out=sm_.t[:], in0=s_.t[:], in1=mask, op=ALU.mult),
                                       reads=[s_.tl, cst.tl], writes=[sm_.tl])
                                    op("act", lambda e, cs_=cs_, h=h, alpha=alpha: e.activation(out=cs_.t[:], in_=Ch[h].t[:], func=AF.Copy, scale=alpha),
                                       reads=[Ch[h].tl, ABt.tl], writes=[cs_.tl])
                                    op("pe", lambda e, o_=o_, q_=q_, cs_=cs_, h=h: e.matmul(o_.t[:], lhsT=q_.t[:, h, :], rhs=cs_.t[:], start=True, stop=False),
                                       reads=[q_.tl, cs_.tl], writes=[o_.tl])
                                    op("pe", lambda e, o_=o_, sm_=sm_, vt_=vt_: e.matmul(o_.t[:], lhsT=sm_.t[:], rhs=vt_.t[:], start=False, stop=True),
                                       reads=[sm_.tl, vt_.tl], writes=[o_.tl])
                                op("pe", lambda e, c_=c_, kt_=kt_, vt_=vt_, h=h: e.matmul(c_.t[:], lhsT=kt_.t[:, h * 128:(h + 1) * 128], rhs=vt_.t[:], start=True, stop=True),
                                   reads=[kt_.tl, vt_.tl], writes=[c_.tl])
                                op("dve", lambda e, c_=c_, h=h, alpha=alpha: e.scalar_tensor_tensor(out=Ch[h].t[:], in0=Ch[h].t[:], scalar=alpha, in1=c_.t[:],
                                                                                                    op0=ALU.mult, op1=ALU.add),
                                   reads=[Ch[h].tl, ABt.tl, c_.tl], writes=[Ch[h].tl])
                                if not skip_out:
                                    op("act", lambda e, dn_=dn_, o_=o_: e.activation(out=dn_.t[:, 0:1], in_=o_.t[:, 128:129], func=AF.Abs),
                                       reads=[o_.tl], writes=[dn_.tl])
                                    op("dve", lambda e, dn_=dn_, c=c, r=r: e.tensor_scalar(out=dn_.t[:, 0:1], in0=dn_.t[:, 0:1], scalar1=TOKQ.t[:, c, 8 + r:9 + r],
                                                                                          scalar2=None, op0=ALU.max),
                                       reads=[dn_.tl, TOKQ.tl], writes=[dn_.tl])
                                    op("dve", lambda e, dn_=dn_: e.reciprocal(out=dn_.t[:, 1:2], in_=dn_.t[:, 0:1]), reads=[dn_.tl], writes=[dn_.tl])
                                    op("act", lambda e, hb_=hb_, o_=o_, dn_=dn_, h=h: e.activation(out=hb_.t[:, h * 128:(h + 1) * 128], in_=o_.t[:, 0:128], func=AF.Copy,
                                                                                                 scale=dn_.t[:, 1:2]),
                                       reads=[o_.tl, dn_.tl], writes=[hb_.tl])
                            if not skip_out:
                                dma("sp", lambda e, hb_=hb_, d=d, t0=t0: e.dma_start(out=HFB[d, t0:t0 + 128, :], in_=hb_.t[:]), reads=[hb_.tl], writes=[tD])
                    fw.barrier()
                    fw.flush()

            if stop == "PS":
                return nc
            if dbg and l == 0:
                dma("sp", lambda e: e.dma_start(out=dbgout["dHFB"][:, :, :], in_=HFB[:, :, :]), reads=[tD], writes=[tD])

            with ExitStack() as lay:
                G2 = [sb(lay, "G2_%d" % s, [128, D]) for s in range(2)]
                for s in range(2):
                    load_bc(G2[s], s, 5)
                NSLT = (CAPL + 127) // 128 + 1
                IDX = sb(lay, "IDX", [128, NSLT, NE], U32)
                GV = sb(lay, "GV", [128, NSLT, NE])
                affs = lay.enter_context(ExitStack())
                AFF = sb(affs, "AFF", [16, NT])
                with ExitStack() as ph:
                    wo = sb(ph, "wo", [128, KC, D], BF16)
                    for kc in range(KC):
                        dma("pool", lambda e, kc=kc: e.dma_start(out=wo.t[:, kc, :], in_=w_out[l, kc * 128:(kc + 1) * 128, :]), writes=[wo.sub[kc]])
                    wr = sb(ph, "wr", [128, KC, NE])
                    dma("sp", lambda e: e.dma_start(out=wr.t[:], in_=w_router[l].rearrange("(k p) n -> p k n", p=128)), writes=[wr.tl])
                    G1 = [sb(ph, "G1_%d" % s, [128, D]) for s in range(2)]
                    A2 = [sb(ph, "A2_%d" % s, [128, D]) for s in range(2)]
                    S2 = [sb(ph, "S2_%d" % s, [128, D]) for s in range(2)]
                    for s in range(2):
                        load_bc(G1[s], s, 2)
                        load_bc(S2[s], s, 3)
                        load_bc(A2[s], s, 4)
                    ghb = sb(ph, "ghb", [128, 512])
                    dma("sp", lambda e: e.dma_start(out=ghb.t[:], in_=g_head[l].partition_broadcast(128)), writes=[ghb.tl])
                    hf = [sb(ph, "hf%d" % i, [128, 512]) for i in range(2)]
                    hbk = [sb(ph, "hbk%d" % i, [128, 512]) for i in range(2)]
                    so = [sb(ph, "so%d" % i, [128, 512], BF16) for i in range(2)]
                    yc = [sb(ph, "yc%d" % i, [128, 4, 128], BF16) for i in range(2)]
                    xt = [sb(ph, "xo%d" % i, [128, D]) for i in range(2)]
                    hs2 = [sb(ph, "hs%d" % i, [128, 512]) for i in range(2)]
                    junk2 = [sb(ph, "junk2_%d" % i, [128, D], BF16) for i in range(2)]
                    st4 = [sb(ph, "st4_%d" % i, [128, 12]) for i in range(2)]
                    ybf2 = [sb(ph, "ybf%d" % i, [128, 512], BF16) for i in range(2)]
                    yT = [sb(ph, "yT%d" % i, [128, 4, 128], BF16) for i in range(2)]
                    tmp2 = [sb(ph, "tmp2_%d" % i, [128, D]) for i in range(2)]
                    xn = [sb(ph, "xn%d" % i, [128, D]) for i in range(2)]
                    h22 = [sb(ph, "h2_%d" % i, [128, D]) for i in range(2)]
                    h2b = [sb(ph, "h2b%d" % i, [128, D], BF16) for i in range(2)]
                    h2T2 = [sb(ph, "h2T%d" % i, [128, KC, 128]) for i in range(2)]
                    pyt = ps(ph, "pyt", [128, 4, 128], BF16)
                    po = [ps(ph, "po%d" % i, [128, 512]) for i in range(2)]
                    pT = [ps(ph, "pT2_%d" % i, [128, 512]) for i in range(2)]
                    pr = [ps(ph, "pr%d" % i, [16, 128]) for i in range(2)]
                    tiles = [c for c in range(NCH) if not (last and c < 2)]
                    def po_load(i):
                        if i < len(tiles):
                            tt0 = tiles[i] * 128
                            bb = i % 2
                            dma("sp", lambda e: e.dma_start(out=hf[bb].t[:], in_=HFB[0, tt0:tt0 + 128, :]), writes=[hf[bb].tl])
                            dma("sp", lambda e: e.dma_start(out=hbk[bb].t[:], in_=HFB[1, tt0:tt0 + 128, :]), writes=[hbk[bb].tl])
                            dma("sp", lambda e: e.dma_start(out=so[bb].t[:], in_=OTOK[tt0:tt0 + 128, :]), writes=[so[bb].tl])
                            dma("sp", lambda e: e.dma_start(out=yc[bb].t[:], in_=YT[:, :, tt0:tt0 + 128].rearrange("j p t -> p j t")), writes=[yc[bb].tl])
                            dma("sp", lambda e: e.dma_start(out=xt[bb].t[:], in_=xrows(l, tt0)), reads=[tX], writes=[xt[bb].tl])
                    po_load(0)
                    for ti, c in enumerate(tiles):
                        t0 = c * 128
                        s = 1 if c < 2 else 0
                        b = ti % 2
                        po_load(ti + 1)
                        sq = st4[b]
                        hs, junk, ybf, tmp, h2, h2T, yTb = hs2[b], junk2[b], ybf2[b], tmp2[b], h22[b], h2T2[b], yT[b]
                        op("dve", lambda e, b=b: e.tensor_tensor(out=hs.t[:], in0=hf[b].t[:], in1=hbk[b].t[:], op=ALU.add), reads=[hf[b].tl, hbk[b].tl], writes=[hs.tl])
                        for h in range(NH):
                            op("act", lambda e, h=h, sq=sq: e.activation(out=junk.t[:, h * 128:(h + 1) * 128], in_=hs.t[:, h * 128:(h + 1) * 128], func=AF.Square,
                                                                        accum_out=sq.t[:, h:h + 1]), reads=[hs.tl], writes=[junk.tl, sq.tl])
                        op("act", lambda e, sq=sq: e.activation(out=sq.t[:, 4:8], in_=sq.t[:, 0:4], func=AF.Sqrt, scale=1.0 / DH, bias=EPS), reads=[sq.tl], writes=[sq.tl])
                        op("dve", lambda e, sq=sq: e.reciprocal(out=sq.t[:, 8:12], in_=sq.t[:, 4:8]), reads=[sq.tl], writes=[sq.tl])
                        for h in range(NH):
                            op("dve", lambda e, h=h, sq=sq: e.scalar_tensor_tensor(out=hs.t[:, h * 128:(h + 1) * 128], in0=hs.t[:, h * 128:(h + 1) * 128],
                                                                                  scalar=sq.t[:, 8 + h:9 + h], in1=ghb.t[:, h * 128:(h + 1) * 128],
                                                                                  op0=ALU.mult, op1=ALU.mult), reads=[hs.tl, sq.tl, ghb.tl], writes=[hs.tl])
                        op("dve", lambda e, b=b: e.tensor_tensor(out=ybf.t[:], in0=hs.t[:], in1=so[b].t[:], op=ALU.mult), reads=[hs.tl, so[b].tl], writes=[ybf.tl])
                        for h in range(NH):
                            op("pe", lambda e, h=h: e.transpose(out=pyt.t[:, h, :], in_=ybf.t[:, h * 128:(h + 1) * 128], identity=identb.t[:]),
                               reads=[ybf.tl, identb.tl], writes=[pyt.tl])
                        op("act", lambda e: e.copy(out=yTb.t[:], in_=pyt.t[:]), reads=[pyt.tl], writes=[yTb.tl])
                        for half in range(2):
                            p = po[half]
                            for kc in range(KC):
                                lhs = yc[b].t[:, kc, :] if kc < 4 else yTb.t[:, kc - 4, :]
                                op("pe", lambda e, p=p, lhs=lhs, kc=kc, half=half: e.matmul(p.t[:], lhsT=lhs, rhs=wo.t[:, kc, half * 512:(half + 1) * 512],
                                                                                          start=(kc == 0), stop=(kc == KC - 1)),
                                   reads=[yc[b].tl, yT[b].tl, wo.sub[kc]], writes=[p.tl])
                            op("dve", lambda e, p=p, half=half, s=s: e.tensor_tensor(out=tmp.t[:, half * 512:(half + 1) * 512], in0=p.t[:],
                                                                                    in1=G1[s].t[:, half * 512:(half + 1) * 512], op=ALU.mult),
                               reads=[p.tl, G1[s].tl], writes=[tmp.tl])
                        op("dve", lambda e, b=b: e.tensor_tensor(out=xn[b].t[:], in0=tmp.t[:], in1=xt[b].t[:], op=ALU.add), reads=[tmp.tl, xt[b].tl], writes=[xn[b].tl])
                        dma("sp", lambda e, b=b, t0=t0: e.dma_start(out=X[t0:t0 + 128, :], in_=xn[b].t[:]), reads=[xn[b].tl], writes=[tX])
                        op("act", lambda e, b=b, sq=sq: e.activation(out=junk.t[:], in_=xn[b].t[:], func=AF.Square, accum_out=sq.t[:, 0:1]),
                           reads=[xn[b].tl], writes=[junk.tl, sq.tl])
                        op("act", lambda e, sq=sq: e.activation(out=sq.t[:, 1:2], in_=sq.t[:, 0:1], func=AF.Sqrt, scale=1.0 / D, bias=EPS), reads=[sq.tl], writes=[sq.tl])
                        op("dve", lambda e, sq=sq: e.reciprocal(out=sq.t[:, 2:3], in_=sq.t[:, 1:2]), reads=[sq.tl], writes=[sq.tl])
                        op("dve", lambda e, b=b, sq=sq, s=s: e.scalar_tensor_tensor(out=tmp.t[:], in0=xn[b].t[:], scalar=sq.t[:, 2:3], in1=A2[s].t[:],
                                                                                   op0=ALU.mult, op1=ALU.mult), reads=[xn[b].tl, sq.tl, A2[s].tl], writes=[tmp.tl])
                        op("dve", lambda e, s=s: e.tensor_tensor(out=h2.t[:], in0=tmp.t[:], in1=S2[s].t[:], op=ALU.add), reads=[tmp.tl, S2[s].tl], writes=[h2.tl])
                        op("act", lambda e, b=b: e.copy(out=h2b[b].t[:], in_=h2.t[:]), reads=[h2.tl], writes=[h2b[b].tl])
                        dma("sp", lambda e, b=b, t0=t0: e.dma_start(out=H2[t0:t0 + 128, :], in_=h2b[b].t[:]), reads=[h2b[b].tl], writes=[tD])
                        for kc in range(KC):
                            p = pT[kc // 4]
                            op("pe", lambda e, p=p, kc=kc: e.transpose(out=p.t[:, (kc % 4) * 128:(kc % 4 + 1) * 128], in_=h2.t[:, kc * 128:(kc + 1) * 128], identity=ident),
                               reads=[h2.tl, cst.tl], writes=[p.tl])
                        op("act", lambda e: e.copy(out=h2T.t[:, 0:4, :], in_=pT[0].t[:].rearrange("p (k t) -> p k t", k=4)), reads=[pT[0].tl], writes=[h2T.tl])
                        op("dve", lambda e: e.tensor_copy(out=h2T.t[:, 4:8, :], in_=pT[1].t[:].rearrange("p (k t) -> p k t", k=4)), reads=[pT[1].tl], writes=[h2T.tl])
                        p = pr[ti % 2]
                        for kc in range(KC):
                            op("pe", lambda e, p=p, kc=kc: e.matmul(p.t[:], lhsT=wr.t[:, kc, :], rhs=h2T.t[:, kc, :], start=(kc == 0), stop=(kc == KC - 1)),
                               reads=[wr.tl, h2T.tl], writes=[p.tl])
                        op("act", lambda e, p=p, t0=t0: e.activation(out=AFF.t[:, t0:t0 + 128], in_=p.t[:], func=AF.Exp), reads=[p.tl], writes=[AFF.tl])
                    fw.barrier()
                    fw.flush()
                if dbg and l == 0:
                    dma("sp", lambda e: e.dma_start(out=dbgout["dX1"][:, :], in_=X[:, :]), reads=[tX], writes=[tD])

                if stop == "PO":
                    affs.close()
                    return nc
                groups = []
                if not last:
                    groups.append(("c", 0, CAPC))
                for g0 in range(0, CAPL, 512):
                    groups.append(("l", g0, min(512, CAPL - g0)))
                with ExitStack() as ph2:
                    with ExitStack() as ph:
                        TV = sb(ph, "TV", [16, CAPL + CAPC])
                        TI = sb(ph, "TI", [16, CAPL + CAPC], U32)
                        rs = sb(ph, "rs", [16, 512])
                        pn = [ps(ph, "pn%d" % i, [16, 512]) for i in range(2)]
                        pt = ps(ph, "ptk", [128, 32])
                        c0s = ([] if last else [(0, NCTX)]) + [(NCTX + i * 512, 512) for i in range(NLAT // 512)]
                        for i, (c0, n) in enumerate(c0s):
                            p = pn[i % 2]
                            op("pe", lambda e, p=p, c0=c0, n=n: e.matmul(p.t[:, 0:n], lhsT=cst.t[0:16, 1472:1488], rhs=AFF.t[:, c0:c0 + n], start=True, stop=True),
                               reads=[cst.tl, AFF.tl], writes=[p.tl])
                            op("dve", lambda e, p=p, n=n: e.reciprocal(out=rs.t[:, 0:n], in_=p.t[:, 0:n]), reads=[p.tl], writes=[rs.tl])
                            op("dve", lambda e, c0=c0, n=n: e.tensor_tensor(out=AFF.t[:, c0:c0 + n], in0=AFF.t[:, c0:c0 + n], in1=rs.t[:, 0:n], op=ALU.mult),
                               reads=[AFF.tl, rs.tl], writes=[AFF.tl])
                        if dbg and l == 0:
                            dma("sp", lambda e: e.dma_start(out=dbgout["dAFF"][:, :], in_=AFF.t[:]), reads=[AFF.tl], writes=[tD])
                        if stop == "TK1":
                            fw.barrier()
                            fw.flush()
                            raise _Stop(nc)
                        NQ = 8
                        QL = NLAT // NQ
                        K = CAPL
                        assert QL & (QL - 1) == 0 and K & (K - 1) == 0
                        AF2 = sb(ph, "AF2", [16 * NQ, QL])
                        TV2 = sb(ph, "TV2", [16 * NQ, K])
                        TI2 = sb(ph, "TI2", [16 * NQ, K], U32)
                        bufV = [sb(ph, "VVa", [16, (NQ // 2) * K]), sb(ph, "TV3", [16, NQ * K])]
                        bufI = [sb(ph, "IIa", [16, (NQ // 2) * K], U32), sb(ph, "TI3", [16, NQ * K], U32)]
                        qof = sb(ph, "qof", [16, K], U32)
                        msk = sb(ph, "msk", [16, (NQ // 2) * K], U32)
                        tA = Tl()
                        dma("sp", lambda e: e.dma_start(out=AFD[:, :], in_=AFF.t[:, NCTX:NT]), reads=[AFF.tl], writes=[tA])
                        dma("sp", lambda e: e.dma_start(out=AF2.t[:], in_=AFD.rearrange("e (h n) -> (e h) n", h=NQ)), reads=[tA], writes=[AF2.tl])
                        for it in range(K // 8):
                            o = it * 8
                            op("dve", lambda e: e.max(out=TV2.t[:, o:o + 8], in_=AF2.t[:]), reads=[AF2.tl], writes=[TV2.tl])
                            op("dve", lambda e: e.max_index(out=TI2.t[:, o:o + 8], in_max=TV2.t[:, o:o + 8], in_values=AF2.t[:]), reads=[AF2.tl, TV2.tl], writes=[TI2.tl])
                            op("dve", lambda e: e.match_replace(out=AF2.t[:], in_to_replace=TV2.t[:, o:o + 8], in_values=AF2.t[:], imm_value=-1.0),
                               reads=[TV2.tl, AF2.tl], writes=[AF2.tl])
                        tB = Tl()
                        dma("sp", lambda e: e.dma_start(out=TVD[:, :], in_=TV2.t[:]), reads=[TV2.tl], writes=[tB])
                        dma("sp", lambda e: e.dma_start(out=bufV[1].t[:], in_=TVD.rearrange("(e h) n -> e (h n)", h=NQ)), reads=[tB], writes=[bufV[1].tl])
                        tC = Tl()
                        dma("sp", lambda e: e.dma_start(out=TID[:, :], in_=TI2.t[:]), reads=[TI2.tl], writes=[tC])
                        dma("sp", lambda e: e.dma_start(out=bufI[1].t[:], in_=TID.rearrange("(e h) n -> e (h n)", h=NQ)), reads=[tC], writes=[bufI[1].tl])
                        for q in range(1, NQ):
                            op("dve", lambda e: e.memset(qof.t[:], q * QL), writes=[qof.tl])
                            op("dve", lambda e: e.tensor_tensor(out=bufI[1].t[:, q * K:(q + 1) * K], in0=bufI[1].t[:, q * K:(q + 1) * K], in1=qof.t[:], op=ALU.bitwise_or),
                               reads=[bufI[1].tl, qof.tl], writes=[bufI[1].tl])

                        def v3(buf, n_):
                            return buf.t[:, 0:n_ * K].rearrange("p (j k) -> p j k", j=n_)
                        cur = 1
                        nl = NQ
                        while nl > 2:
                            dst = 1 - cur
                            m_ = nl // 2
                            A_, B_ = v3(bufV[cur], nl)[:, 0:nl:2, :], v3(bufV[cur], nl)[:, 1:nl:2, ::-1]
                            IA_, IB_ = v3(bufI[cur], nl)[:, 0:nl:2, :], v3(bufI[cur], nl)[:, 1:nl:2, ::-1]
                            m0, v0, i0 = v3(msk, m_), v3(bufV[dst], m_), v3(bufI[dst], m_)
                            op("dve", lambda e: e.tensor_tensor(out=m0, in0=A_, in1=B_, op=ALU.is_ge), reads=[bufV[cur].tl], writes=[msk.tl])
                            op("dve", lambda e: e.tensor_tensor(out=v0, in0=A_, in1=B_, op=ALU.max), reads=[bufV[cur].tl], writes=[bufV[dst].tl])
                            op("dve", lambda e: e.tensor_copy(out=i0, in_=IB_), reads=[bufI[cur].tl], writes=[bufI[dst].tl])
                            op("dve", lambda e: e.copy_predicated(out=i0, mask=m0, data=IA_), reads=[msk.tl, bufI[cur].tl, bufI[dst].tl], writes=[bufI[dst].tl])
                            cur = dst
                            nl = m_
                            dd = K // 2
                            while dd >= 1:
                                nxt = 1 - cur

                                def vw(buf):
                                    return buf.t[:, 0:nl * K].rearrange("p (b t d) -> p b t d", t=2, d=dd)
                                sv, si_, dv, di, mv = vw(bufV[cur]), vw(bufI[cur]), vw(bufV[nxt]), vw(bufI[nxt]), vw(msk)
                                lo, hi, ilo, ihi = sv[:, :, 0, :], sv[:, :, 1, :], si_[:, :, 0, :], si_[:, :, 1, :]
                                mm = mv[:, :, 0, :]
                                op("dve", lambda e: e.tensor_tensor(out=mm, in0=lo, in1=hi, op=ALU.is_ge), reads=[bufV[cur].tl], writes=[msk.tl])
                                op("dve", lambda e: e.tensor_tensor(out=dv[:, :, 0, :], in0=lo, in1=hi, op=ALU.max), reads=[bufV[cur].tl], writes=[bufV[nxt].tl])
                                op("dve", lambda e: e.tensor_tensor(out=dv[:, :, 1, :], in0=lo, in1=hi, op=ALU.min), reads=[bufV[cur].tl], writes=[bufV[nxt].tl])
                                op("dve", lambda e: e.tensor_copy(out=di[:, :, 0, :], in_=ihi), reads=[bufI[cur].tl], writes=[bufI[nxt].tl])
                                op("dve", lambda e: e.copy_predicated(out=di[:, :, 0, :], mask=mm, data=ilo), reads=[msk.tl, bufI[cur].tl, bufI[nxt].tl], writes=[bufI[nxt].tl])
                                op("dve", lambda e: e.tensor_copy(out=di[:, :, 1, :], in_=ilo), reads=[bufI[cur].tl], writes=[bufI[nxt].tl])
                                op("dve", lambda e: e.copy_predicated(out=di[:, :, 1, :], mask=mm, data=ihi), reads=[msk.tl, bufI[cur].tl, bufI[nxt].tl], writes=[bufI[nxt].tl])
                                cur = nxt
                                dd //= 2
                        sA, sB = bufV[cur].t[:, 0:K], bufV[cur].t[:, K:2 * K][:, ::-1]
                        iA, iB = bufI[cur].t[:, 0:K], bufI[cur].t[:, K:2 * K][:, ::-1]
                        op("dve", lambda e: e.tensor_tensor(out=msk.t[:, 0:K], in0=sA, in1=sB, op=ALU.is_ge), reads=[bufV[cur].tl], writes=[msk.tl])
                        op("dve", lambda e: e.tensor_tensor(out=TV.t[:, 0:K], in0=sA, in1=sB, op=ALU.max), reads=[bufV[cur].tl], writes=[TV.tl])
                        op("dve", lambda e: e.tensor_copy(out=TI.t[:, 0:K], in_=iB), reads=[bufI[cur].tl], writes=[TI.tl])
                        op("dve", lambda e: e.copy_predicated(out=TI.t[:, 0:K], mask=msk.t[:, 0:K], data=iA), reads=[msk.tl, bufI[cur].tl, TI.tl], writes=[TI.tl])
                        sets = ([] if last else [(0, NCTX, CAPL, CAPC)])
                        for (c0, n, o0, cap) in sets:
                            av = AFF.t[:, c0:c0 + n]
                            for it in range(cap // 8):
                                o = o0 + it * 8
                                op("dve", lambda e, av=av, o=o: e.max(out=TV.t[:, o:o + 8], in_=av), reads=[AFF.tl], writes=[TV.tl])
                                op("dve", lambda e, av=av, o=o: e.max_index(out=TI.t[:, o:o + 8], in_max=TV.t[:, o:o + 8], in_values=av), reads=[AFF.tl, TV.tl], writes=[TI.tl])
                                op("dve", lambda e, av=av, o=o: e.match_replace(out=av, in_to_replace=TV.t[:, o:o + 8], in_values=av, imm_value=-1.0),
                                   reads=[TV.tl, AFF.tl], writes=[AFF.tl])
                        if stop == "TK2":
                            fw.barrier()
                            fw.flush()
                            raise _Stop(nc)
                        tT = Tl()
                        dma("sp", lambda e: e.dma_start(out=TIS[:, :], in_=TI.t[:]), reads=[TI.tl], writes=[tT])
                        slot_tiles = [(j, j * 128, min(128, CAPL - j * 128)) for j in range((CAPL + 127) // 128)]
                        if not last:
                            slot_tiles.append((NSLT - 1, CAPL, CAPC))
                        for (j, o, n) in slot_tiles:
                            dma("sp", lambda e, j=j, o=o, n=n: e.dma_start(out=IDX.t[0:n, j, :], in_=TIS[:, o:o + n].rearrange("e p -> p e"),
                                                                          allow_slow_non_contiguous=True),
                                reads=[tT], writes=[IDX.tl])
                            op("pe", lambda e, o=o, n=n: e.transpose(out=pt.t[0:n, 16:32], in_=TV.t[:, o:o + n], identity=cst.t[0:16, 0:16]), reads=[TV.tl, cst.tl], writes=[pt.tl])
                            op("dve", lambda e, j=j, n=n: e.tensor_copy(out=GV.t[0:n, j, :], in_=pt.t[0:n, 16:32]), reads=[pt.tl], writes=[GV.tl])
                        fw.barrier()
                        fw.flush()
                    affs.close()
                    if stop == "TK":
                        return nc
                    with ExitStack() as ph:
                        wg = [sb(ph, "wg%d" % i, [128, KC, DEXP], BF16) for i in range(2)]
                        wu = [sb(ph, "wu%d" % i, [128, KC, DEXP], BF16) for i in range(2)]
                        wd = [sb(ph, "wd%d" % i, [128, FCH, D], BF16) for i in range(2)]
                        xg = [sb(ph, "xg%d" % i, [128, D], BF16) for i in range(8)]
                        xeT2 = [sb(ph, "xeT%d" % i, [128, KC, 512], BF16) for i in range(2)]
                        ngrp = 0
                        sa = [sb(ph, "sa%d" % i, [128, 512]) for i in range(2)]
                        actT = sb(ph, "actT", [128, FCH, 512], BF16)
                        ye = [sb(ph, "ye%d" % i, [128, D]) for i in range(2)]
                        pxt = [ps(ph, "pxt%d" % i, [128, KC, 128], BF16) for i in range(2)]
                        pa_ = [ps(ph, "pa_%d" % i, [128, 512]) for i in range(2)]
                        pu_ = [ps(ph, "pu_%d" % i, [128, 512]) for i in range(1)]
                        py = [ps(ph, "py%d" % i, [128, 512]) for i in range(2)]

                        def load_w(e_):
                            b = e_ % 2
                            for (wt, src, nk) in ((wg[b], w_gate_e, KC), (wu[b], w_up_e, KC), (wd[b], w_down_e, FCH)):
                                srcv = src[l, e_].rearrange("(k p) n -> p k n", p=128)
                                for (k0, k1) in ((0, nk // 2), (nk // 2, nk)):
                                    dma("pool", lambda e: e.dma_start(out=wt.t[:, k0:k1, :], in_=srcv[:, k0:k1, :]), writes=[wt.sub[k] for k in range(k0, k1)])
                        def tiles_of(kind, g0, gn):
                            if kind == "c":
                                return [(NSLT - 1, gn)]
                            return [((g0 + o) // 128, min(128, gn - o)) for o in range(0, gn, 128)]
                        msteps = [(e2, grp) for e2 in range(NE) for grp in groups]

                        def gathers(si):
                            if si >= len(msteps):
                                return
                            e2, (kind2, g02, gn2) = msteps[si]
                            for ji2, (j2, n2) in enumerate(tiles_of(kind2, g02, gn2)):
                                gg = xg[(si % 2) * 4 + ji2]
                                dma("pool", lambda e: e.indirect_dma_start(
                                    out=gg.t[0:n2, :], out_offset=None, in_=H2[:, :], element_offset=(0 if kind2 == "c" else NCTX * D),
                                    in_offset=bass.IndirectOffsetOnAxis(ap=IDX.t[0:n2, j2, e2:e2 + 1], axis=0)), reads=[IDX.tl, tD], writes=[gg.tl])
                        load_w(0)
                        gathers(0)
                        ng = 0
                        ny = 0
                        si = -1
                        for e_ in range(NE):
                            b = e_ % 2
                            for gi_, (kind, g0, gn) in enumerate(groups):
                                si += 1
                                if gi_ == 0 and e_ + 1 < NE:
                                    load_w(e_ + 1)
                                s = 1 if kind == "c" else 0
                                xeTg = xeT2[ngrp % 2]
                                ngrp += 1
                                tl_ = tiles_of(kind, g0, gn)
                                for ji, (j, n) in enumerate(tl_):
                                    g_ = xg[(si % 2) * 4 + ji]
                                    px = pxt[ng % 2]
                                    ng += 1
                                    for kc in range(KC):
                                        op("pe", lambda e, g_=g_, px=px, kc=kc, n=n: e.transpose(out=px.t[:, kc, 0:n], in_=g_.t[0:n, kc * 128:(kc + 1) * 128],
                                                                                                identity=identb.t[0:n, 0:n]),
                                           reads=[g_.tl, identb.tl], writes=[px.tl])
                                    op("act", lambda e, px=px, ji=ji, n=n: e.copy(out=xeTg.t[:, :, ji * 128:ji * 128 + n], in_=px.t[:, :, 0:n]), reads=[px.tl], writes=[xeTg.tl])
                                for fc in range(FCH):
                                    pa1 = pa_[fc % 2]
                                    pu1 = pu_[0]
                                    s1 = sa[fc % 2]
                                    for kc in range(KC):
                                        op("pe", lambda e, pa1=pa1, kc=kc, fc=fc: e.matmul(pa1.t[:, 0:gn], lhsT=wg[b].t[:, kc, fc * 128:(fc + 1) * 128], rhs=xeTg.t[:, kc, 0:gn],
                                                                                          start=(kc == 0), stop=(kc == KC - 1)), reads=[wg[b].sub[kc], xeTg.tl], writes=[pa1.tl])
                                    for kc in range(KC):
                                        op("pe", lambda e, pu1=pu1, kc=kc, fc=fc: e.matmul(pu1.t[:, 0:gn], lhsT=wu[b].t[:, kc, fc * 128:(fc + 1) * 128], rhs=xeTg.t[:, kc, 0:gn],
                                                                                          start=(kc == 0), stop=(kc == KC - 1)), reads=[wu[b].sub[kc], xeTg.tl], writes=[pu1.tl])
                                    op("act", lambda e, pa1=pa1, s1=s1: e.activation(out=s1.t[:, 0:gn], in_=pa1.t[:, 0:gn], func=AF.Silu), reads=[pa1.tl], writes=[s1.tl])
                                    op("dve", lambda e, pu1=pu1, s1=s1, fc=fc: e.tensor_tensor(out=actT.t[:, fc, 0:gn], in0=pu1.t[:, 0:gn], in1=s1.t[:, 0:gn], op=ALU.mult),
                                       reads=[pu1.tl, s1.tl], writes=[actT.tl])
                                gathers(si + 1)
                                for ji, (j, n) in enumerate(tl_):
                                    y_ = ye[ny % 2]
                                    ny += 1
                                    for half in range(2):
                                        p = py[half]
                                        for fc in range(FCH):
                                            op("pe", lambda e, p=p, fc=fc, ji=ji, n=n, half=half: e.matmul(p.t[0:n, :], lhsT=actT.t[:, fc, ji * 128:ji * 128 + n],
                                                                                                          rhs=wd[b].t[:, fc, half * 512:(half + 1) * 512],
                                                                                                          start=(fc == 0), stop=(fc == FCH - 1)),
                                               reads=[actT.tl, wd[b].sub[fc]], writes=[p.tl])
                                        op("dve", lambda e, p=p, y_=y_, j=j, n=n, half=half, s=s, e_=e_: e.scalar_tensor_tensor(
                                            out=y_.t[0:n, half * 512:(half + 1) * 512], in0=p.t[0:n, :], scalar=GV.t[0:n, j, e_:e_ + 1],
                                            in1=G2[s].t[0:n, half * 512:(half + 1) * 512], op0=ALU.mult, op1=ALU.mult),
                                           reads=[p.tl, GV.tl, G2[s].tl], writes=[y_.tl])
                                    dma("pool", lambda e, y_=y_, j=j, n=n, e_=e_: e.indirect_dma_start(
                                        element_offset=(0 if kind == "c" else NCTX * D), out=X[:, :], out_offset=bass.IndirectOffsetOnAxis(ap=IDX.t[0:n, j, e_:e_ + 1], axis=0),
                                        in_=y_.t[0:n, :], in_offset=None, compute_op=ALU.add), reads=[IDX.tl, y_.tl], writes=[tX])
                        fw.barrier()
                        fw.flush()

        with ExitStack() as ph:
            gf = sb(ph, "gf", [128, D])
            dma("sp", lambda e: e.dma_start(out=gf.t[:], in_=g_final.partition_broadcast(128)), writes=[gf.tl])
            xt = [sb(ph, "xf%d" % i, [128, D]) for i in range(2)]
            yo = [sb(ph, "yo%d" % i, [128, D]) for i in range(2)]
            junk = sb(ph, "junk3", [128, D], BF16)
            ss = [sb(ph, "ssf%d" % i, [128, 4]) for i in range(2)]
            tO = Tl()
            def pf_load(i):
                if i < NLAT // 128:
                    rr = NCTX + i * 128
                    dma("sp", lambda e: e.dma_start(out=xt[i % 2].t[:], in_=X[rr:rr + 128, :]), reads=[tX], writes=[xt[i % 2].tl])
            pf_load(0)
            for i in range(NLAT // 128):
                b = i % 2
                r0 = NCTX + i * 128
                sq = ss[b]
                pf_load(i + 1)
                op("act", lambda e, b=b, sq=sq: e.activation(out=junk.t[:], in_=xt[b].t[:], func=AF.Square, accum_out=sq.t[:, 0:1]), reads=[xt[b].tl], writes=[junk.tl, sq.tl])
                op("act", lambda e, sq=sq: e.activation(out=sq.t[:, 1:2], in_=sq.t[:, 0:1], func=AF.Sqrt, scale=1.0 / D, bias=EPS), reads=[sq.tl], writes=[sq.tl])
                op("dve", lambda e, sq=sq: e.reciprocal(out=sq.t[:, 2:3], in_=sq.t[:, 1:2]), reads=[sq.tl], writes=[sq.tl])
                op("dve", lambda e, b=b, sq=sq: e.scalar_tensor_tensor(out=yo[b].t[:], in0=xt[b].t[:], scalar=sq.t[:, 2:3], in1=gf.t[:], op0=ALU.mult, op1=ALU.mult),
                   reads=[xt[b].tl, sq.tl, gf.tl], writes=[yo[b].tl])
                dma("sp", lambda e, b=b, i=i: e.dma_start(out=out[i * 128:(i + 1) * 128, :], in_=yo[b].t[:]), reads=[yo[b].tl], writes=[tO])
            if dbg:
                dma("sp", lambda e: e.dma_start(out=dbgout["dX"][:, :], in_=X[:, :]), reads=[tX], writes=[tD])
                for nm in ("QT", "KT", "KTOK", "VTOK", "OTOK", "YT", "H2", "MODROW"):
                    dst, src = dbgout["d" + nm]
                    if len(src.shape) == 3:
                        dma("sp", lambda e, dst=dst, src=src: e.dma_start(out=dst[:, :, :], in_=src[:, :, :]), reads=[tD], writes=[tD])
                    else:
                        dma("sp", lambda e, dst=dst, src=src: e.dma_start(out=dst[:, :], in_=src[:, :]), reads=[tD], writes=[tD])
            fw.barrier()
            fw.flush()
        print("instructions (incl waits):", fw.ninst)
    return nc


def host_inputs(inputs, b, nlat=None):
    f = lambda a: np.ascontiguousarray(np.asarray(a, dtype=np.float32))
    L = inputs["w_mod"].shape[0]
    c = np.asarray(inputs["c"], np.float32)[b]
    cc = np.asarray(inputs["c_ctx"], np.float32)
    cT = np.stack([c.reshape(8, 128).T, cc.reshape(8, 128).T], axis=1)
    cw = np.asarray(inputs["conv_w"], np.float32).reshape(L, 3, 4, 128).transpose(0, 3, 1, 2)
    cb = np.asarray(inputs["conv_b"], np.float32).reshape(L, 4, 128).transpose(0, 2, 1)
    m = {
        "x": f(inputs["x"][b]), "ctx": f(inputs["ctx"][b]), "cT": f(cT),
        "w_mod": f(inputs["w_mod"]), "b_mod": f(inputs["b_mod"]),
        "g_norm1": f(inputs["g_norm1"]), "g_norm2": f(inputs["g_norm2"]),
        "w_in": f(inputs["w_in"]), "w_out": f(inputs["w_out"]),
        "conv_w": f(cw), "conv_b": f(cb),
        "gate_b": f(np.asarray(inputs["gate_b"], np.float32).reshape(L, 16, 1)),
        "g_head": f(np.asarray(inputs["g_head"], np.float32).reshape(L, 512)),
        "w_router": f(inputs["w_router"]),
        "w_gate_e": f(inputs["w_gate_e"]), "w_up_e": f(inputs["w_up_e"]), "w_down_e": f(inputs["w_down_e"]),
        "g_final": f(inputs["g_final"]), "consts": make_consts(),
    }
    return m


def kernel(**inputs):
    x = np.asarray(inputs["x"])
    nb, nlat, _ = x.shape
    depth = np.asarray(inputs["w_mod"]).shape[0]
    nc = build(nlat, depth)
    shared = host_inputs(inputs, 0)
    in_maps = []
    for b in range(nb):
        m = dict(shared)
        mb = host_inputs({**inputs, "w_mod": inputs["w_mod"]}, b) if False else None
        c = np.asarray(inputs["c"], np.float32)[b]
        cc = np.asarray(inputs["c_ctx"], np.float32)
        m["x"] = np.ascontiguousarray(np.asarray(inputs["x"][b], np.float32))
        m["ctx"] = np.ascontiguousarray(np.asarray(inputs["ctx"][b], np.float32))
        m["cT"] = np.ascontiguousarray(np.stack([c.reshape(8, 128).T, cc.reshape(8, 128).T], axis=1))
        in_maps.append(m)
    res = run_bass_kernel_spmd(nc, in_maps, core_ids=list(range(nb)))
    return np.stack([np.asarray(r["out"], np.float32) for r in res.results], axis=0)
```
